# Optimizing a Trainium2 kernel written in Bass

```python
import math
import jax, jax.numpy as jnp
from jax import lax
import numpy as np

D_MODEL = 2048
BATCH = 4
SEQ = 4096
DEPTH = 1

NSA_HEADS = 16
NSA_KV_HEADS = 4
NSA_GROUP = NSA_HEADS // NSA_KV_HEADS
HEAD_DIM = 64
NSA_WIDTH = NSA_HEADS * HEAD_DIM
KV_WIDTH = NSA_KV_HEADS * HEAD_DIM
CMP_LEN = 32
CMP_STRIDE = 16
CMP_HIDDEN = 256
SEL_BLOCK = 64
SEL_TOP = 16
SEL_Q_BLOCK = 64
WINDOW = 512
WIN_Q_BLOCK = 128
S5_WIDTH = 1024
S5_GROUP = 16
S5_GROUPS = S5_WIDTH // S5_GROUP
S5_STATE = 64
DT_MIN = 1e-3
DT_MAX = 1e-1
REL_BUCKETS = 32
REL_MAX_DIST = 128
PEER_KEYS = 128
PEER_EXPERTS = PEER_KEYS * PEER_KEYS
PEER_HEADS = 8
PEER_TOPK = 16
PEER_QDIM = 256
PEER_CHUNK = 128
N_BRANCHES = 2
IN_WIDTH = NSA_WIDTH + 6 * KV_WIDTH + 3 * NSA_HEADS + S5_WIDTH + N_BRANCHES * D_MODEL
EPS = 1e-6
NEG_INF = -1e30
FORCE_BONUS = 1e4

kernel_name = "hybrid_nsa_s5_peer_block"


def _split_points():
    widths = [NSA_WIDTH] + [KV_WIDTH] * 6 + [3 * NSA_HEADS, S5_WIDTH, N_BRANCHES * D_MODEL]
    pts, acc = [], 0
    for w in widths[:-1]:
        acc += w
        pts.append(acc)
    return pts


def rms_norm(x, g):
    xf = x.astype(jnp.float32)
    xf = xf * lax.rsqrt(jnp.mean(xf * xf, axis=-1, keepdims=True) + EPS)
    return xf.astype(x.dtype) * g


def t5_bucket(dist):
    dist = jnp.maximum(dist, 0)
    max_exact = REL_BUCKETS // 2
    log_ratio = jnp.log(jnp.maximum(dist, 1).astype(jnp.float32) / max_exact) / math.log(REL_MAX_DIST / max_exact)
    large = jnp.minimum(max_exact + (log_ratio * (REL_BUCKETS - max_exact)).astype(jnp.int32), REL_BUCKETS - 1)
    return jnp.where(dist < max_exact, dist, large)


def compress_kv(k, pos, w1, b1, w2, b2):
    b, l = k.shape[:2]
    kb = k.reshape(b, l // CMP_STRIDE, CMP_STRIDE, NSA_KV_HEADS, HEAD_DIM)
    blocks = jnp.concatenate([kb[:, :-1], kb[:, 1:]], axis=2)
    blocks = blocks + pos[None, None, :, None, :]
    flat = blocks.transpose(0, 1, 3, 2, 4).reshape(b, -1, NSA_KV_HEADS, CMP_LEN * HEAD_DIM)
    return jax.nn.gelu(flat @ w1 + b1) @ w2 + b2


def cmp_to_sel_matrix(n_cmp, n_sel):
    cs = np.arange(n_cmp)[:, None] * CMP_STRIDE
    ss = np.arange(n_sel)[None, :] * SEL_BLOCK
    ov = np.clip(np.minimum(cs + CMP_LEN, ss + SEL_BLOCK) - np.maximum(cs, ss), 0, None)
    return jnp.asarray(ov / CMP_LEN, dtype=jnp.float32)


def nsa(q, kc, vc, ks, vs, kw, vw, gate_logits, rel_bias):
    b, l = q.shape[:2]
    scale = HEAD_DIM ** -0.5
    qg = q.reshape(b, l, NSA_KV_HEADS, NSA_GROUP, HEAD_DIM)
    t = jnp.arange(l)

    n_cmp = kc.shape[1]
    cmp_end = jnp.arange(n_cmp) * CMP_STRIDE + CMP_LEN - 1
    dist_c = t[:, None] - cmp_end[None, :]
    valid_c = dist_c >= 0
    bias_c = rel_bias[t5_bucket(dist_c)].transpose(2, 0, 1).reshape(NSA_KV_HEADS, NSA_GROUP, l, n_cmp)
    s_c = jnp.einsum('blkgd,bnkd->bkgln', qg, kc).astype(jnp.float32) * scale + bias_c
    p_c = jax.nn.softmax(jnp.where(valid_c, s_c, NEG_INF), axis=-1) * valid_c
    o_c = jnp.einsum('bkgln,bnkd->blkgd', p_c.astype(vc.dtype), vc)

    n_sel = l // SEL_BLOCK
    n_top = min(SEL_TOP, n_sel)
    imp = jnp.einsum('bkgln,nj->bklj', p_c, cmp_to_sel_matrix(n_cmp, n_sel))
    blk = jnp.arange(n_sel)[None, :]
    cur = (t // SEL_BLOCK)[:, None]
    forced = (blk == 0) | (blk == cur) | (blk == cur - 1)
    imp = jnp.where(blk <= cur, imp + FORCE_BONUS * forced, NEG_INF)
    _, sel_idx = lax.top_k(imp, n_top)

    ks_blocks = ks.reshape(b, n_sel, SEL_BLOCK, NSA_KV_HEADS, HEAD_DIM).transpose(0, 3, 1, 2, 4)
    vs_blocks = vs.reshape(b, n_sel, SEL_BLOCK, NSA_KV_HEADS, HEAD_DIM).transpose(0, 3, 1, 2, 4)
    gather = jax.vmap(jax.vmap(lambda blocks, idx: blocks[idx]))
    tbl = rel_bias.reshape(REL_BUCKETS, NSA_KV_HEADS, NSA_GROUP).transpose(1, 0, 2)
    lookup = jax.vmap(lambda tb, bk: tb[bk], in_axes=(0, 1), out_axes=1)

    def sel_block(args):
        q_b, idx_b, t_b = args
        k_g = gather(ks_blocks, idx_b).reshape(b, NSA_KV_HEADS, SEL_Q_BLOCK, -1, HEAD_DIM)
        v_g = gather(vs_blocks, idx_b).reshape(b, NSA_KV_HEADS, SEL_Q_BLOCK, -1, HEAD_DIM)
        pos = (idx_b[..., None] * SEL_BLOCK + jnp.arange(SEL_BLOCK)).reshape(b, NSA_KV_HEADS, SEL_Q_BLOCK, -1)
        dist = t_b[:, None] - pos
        bias = lookup(tbl, t5_bucket(dist)).transpose(0, 1, 4, 2, 3)
        s = jnp.einsum('btkgd,bktsd->bkgts', q_b, k_g).astype(jnp.float32) * scale + bias
        s = jnp.where((dist >= 0)[:, :, None], s, NEG_INF)
        p = jax.nn.softmax(s, axis=-1).astype(v_g.dtype)
        return jnp.einsum('bkgts,bktsd->btkgd', p, v_g)

    n_qb = l // SEL_Q_BLOCK
    q_blocks = qg.reshape(b, n_qb, SEL_Q_BLOCK, NSA_KV_HEADS, NSA_GROUP, HEAD_DIM).swapaxes(0, 1)
    idx_blocks = sel_idx.reshape(b, NSA_KV_HEADS, n_qb, SEL_Q_BLOCK, n_top).transpose(2, 0, 1, 3, 4)
    t_blocks = t.reshape(n_qb, SEL_Q_BLOCK)
    o_s = lax.map(sel_block, (q_blocks, idx_blocks, t_blocks))
    o_s = o_s.swapaxes(0, 1).reshape(b, l, NSA_KV_HEADS, NSA_GROUP, HEAD_DIM)

    n_wb = l // WIN_Q_BLOCK
    nw = WINDOW // WIN_Q_BLOCK

    def band(k):
        kb = k.reshape(b, n_wb, WIN_Q_BLOCK, NSA_KV_HEADS, HEAD_DIM)
        kp = jnp.pad(kb, ((0, 0), (nw, 0), (0, 0), (0, 0), (0, 0)))
        return jnp.concatenate([kp[:, i:i + n_wb] for i in range(nw + 1)], axis=2)

    k_band, v_band = band(kw), band(vw)
    qi = jnp.arange(WIN_Q_BLOCK)
    kj = jnp.arange((nw + 1) * WIN_Q_BLOCK)
    dist_w = qi[:, None] + nw * WIN_Q_BLOCK - kj[None, :]
    in_win = (dist_w >= 0) & (dist_w < WINDOW)
    kpos = jnp.arange(n_wb)[:, None] * WIN_Q_BLOCK + kj[None, :] - nw * WIN_Q_BLOCK
    mask_w = in_win[None] & (kpos >= 0)[:, None, :]
    bias_w = rel_bias[t5_bucket(dist_w)].transpose(2, 0, 1).reshape(NSA_KV_HEADS, NSA_GROUP, WIN_Q_BLOCK, -1)
    qw = qg.reshape(b, n_wb, WIN_Q_BLOCK, NSA_KV_HEADS, NSA_GROUP, HEAD_DIM)
    s_w = jnp.einsum('bnqkgd,bnskd->bnkgqs', qw, k_band).astype(jnp.float32) * scale + bias_w
    s_w = jnp.where(mask_w[None, :, None, None], s_w, NEG_INF)
    p_w = jax.nn.softmax(s_w, axis=-1).astype(v_band.dtype)
    o_w = jnp.einsum('bnkgqs,bnskd->bnqkgd', p_w, v_band).reshape(b, l, NSA_KV_HEADS, NSA_GROUP, HEAD_DIM)

    g = jax.nn.sigmoid(gate_logits).reshape(b, l, NSA_KV_HEADS, NSA_GROUP, 3, 1)
    o = g[..., 0, :] * o_c + g[..., 1, :] * o_s + g[..., 2, :] * o_w
    return o.reshape(b, l, NSA_WIDTH)


def s5(u, lam_re, lam_im, log_step, b_re, b_im, c_re, c_im, d_skip):
    b, l, _ = u.shape
    f32 = jnp.float32
    ug = u.astype(f32).reshape(b, l, S5_GROUPS, S5_GROUP).transpose(1, 0, 2, 3)
    lam_re, lam_im = lam_re.astype(f32), lam_im.astype(f32)
    step = jnp.exp(log_step.astype(f32))[:, None]
    mag = jnp.exp(lam_re * step)
    lb_re, lb_im = mag * jnp.cos(lam_im * step), mag * jnp.sin(lam_im * step)
    den = lam_re * lam_re + lam_im * lam_im
    n_re = lb_re - 1.0
    f_re = (n_re * lam_re + lb_im * lam_im) / den
    f_im = (lb_im * lam_re - n_re * lam_im) / den
    b_re, b_im = b_re.astype(f32), b_im.astype(f32)
    bb_re = f_re[..., None] * b_re - f_im[..., None] * b_im
    bb_im = f_re[..., None] * b_im + f_im[..., None] * b_re
    bu_re = jnp.einsum('lbgh,gph->lbgp', ug, bb_re)
    bu_im = jnp.einsum('lbgh,gph->lbgp', ug, bb_im)
    a_re = jnp.broadcast_to(lb_re, bu_re.shape)
    a_im = jnp.broadcast_to(lb_im, bu_re.shape)

    def combine(e1, e2):
        a1r, a1i, b1r, b1i = e1
        a2r, a2i, b2r, b2i = e2
        return (a2r * a1r - a2i * a1i, a2r * a1i + a2i * a1r,
                a2r * b1r - a2i * b1i + b2r, a2r * b1i + a2i * b1r + b2i)

    _, _, x_re, x_im = lax.associative_scan(combine, (a_re, a_im, bu_re, bu_im), axis=0)
    y = jnp.einsum('lbgp,ghp->lbgh', x_re, c_re.astype(f32)) - jnp.einsum('lbgp,ghp->lbgh', x_im, c_im.astype(f32))
    y = y + d_skip.astype(f32).reshape(S5_GROUPS, S5_GROUP) * ug
    return y.transpose(1, 0, 2, 3).reshape(b, l, S5_WIDTH).astype(u.dtype)


def peer(h, w_q, sub_keys, u_tab, v_tab):
    b, l, d = h.shape
    n_tok = b * l
    hf = h.reshape(n_tok, d)
    q = (hf @ w_q).reshape(n_tok, PEER_HEADS, 2, PEER_QDIM // 2)
    s = jnp.einsum('thcd,hcnd->thcn', q, sub_keys).astype(jnp.float32)
    v1, i1 = lax.top_k(s[:, :, 0], PEER_TOPK)
    v2, i2 = lax.top_k(s[:, :, 1], PEER_TOPK)
    cand_s = (v1[..., :, None] + v2[..., None, :]).reshape(n_tok, PEER_HEADS, -1)
    cand_i = (i1[..., :, None] * PEER_KEYS + i2[..., None, :]).reshape(n_tok, PEER_HEADS, -1)
    top_s, top_p = lax.top_k(cand_s, PEER_TOPK)
    expert = jnp.take_along_axis(cand_i, top_p, axis=-1)
    gate = jax.nn.softmax(top_s, axis=-1)
    n_ch = n_tok // PEER_CHUNK

    def chunk(args):
        x_c, e_c, g_c = args
        a = jnp.einsum('td,thkd->thk', x_c, u_tab[e_c]).astype(jnp.float32)
        w = (g_c * jax.nn.gelu(a)).astype(x_c.dtype)
        return jnp.einsum('thk,thkd->td', w, v_tab[e_c])

    out = lax.map(chunk, (hf.reshape(n_ch, PEER_CHUNK, d),
                          expert.reshape(n_ch, PEER_CHUNK, PEER_HEADS, PEER_TOPK),
                          gate.reshape(n_ch, PEER_CHUNK, PEER_HEADS, PEER_TOPK)))
    return out.reshape(b, l, d)


def setup_inputs(seed: int = 0) -> dict:
    key = jax.random.key(seed)
    ks = jax.random.split(key, 40)
    f32 = jnp.float32

    def nrm(k, shape, s):
        return jax.random.normal(k, shape, f32) * s

    L = DEPTH
    lam_im = jnp.broadcast_to(math.pi * jnp.arange(S5_STATE, dtype=f32), (L, S5_GROUPS, S5_STATE))
    return {
        "x": nrm(ks[0], (BATCH, SEQ, D_MODEL), 1.0),
        "c": nrm(ks[1], (BATCH, D_MODEL), 1.0),
        "ada_w": nrm(ks[2], (L, D_MODEL, 6 * D_MODEL), 0.5 * D_MODEL ** -0.5),
        "ada_b": nrm(ks[3], (L, 6 * D_MODEL), 0.02),
        "norm_mix_g": 1.0 + nrm(ks[4], (L, D_MODEL), 0.02),
        "norm_ffn_g": 1.0 + nrm(ks[5], (L, D_MODEL), 0.02),
        "w_in": nrm(ks[6], (L, D_MODEL, IN_WIDTH), D_MODEL ** -0.5),
        "cmp_pos": nrm(ks[7], (L, 2, CMP_LEN, HEAD_DIM), 0.1),
        "cmp_w1": nrm(ks[8], (L, 2, CMP_LEN * HEAD_DIM, CMP_HIDDEN), (CMP_LEN * HEAD_DIM) ** -0.5),
        "cmp_b1": nrm(ks[9], (L, 2, CMP_HIDDEN), 0.02),
        "cmp_w2": nrm(ks[10], (L, 2, CMP_HIDDEN, HEAD_DIM), CMP_HIDDEN ** -0.5),
        "cmp_b2": nrm(ks[11], (L, 2, HEAD_DIM), 0.02),
        "rel_bias": nrm(ks[12], (REL_BUCKETS, NSA_HEADS), 0.3),
        "s5_lam_re": -0.5 + nrm(ks[13], (L, S5_GROUPS, S5_STATE), 0.01),
        "s5_lam_im": lam_im + nrm(ks[14], (L, S5_GROUPS, S5_STATE), 0.01),
        "s5_log_step": jax.random.uniform(ks[15], (L, S5_GROUPS), f32, math.log(DT_MIN), math.log(DT_MAX)),
        "s5_b_re": nrm(ks[16], (L, S5_GROUPS, S5_STATE, S5_GROUP), (2 * S5_GROUP) ** -0.5),
        "s5_b_im": nrm(ks[17], (L, S5_GROUPS, S5_STATE, S5_GROUP), (2 * S5_GROUP) ** -0.5),
        "s5_c_re": nrm(ks[18], (L, S5_GROUPS, S5_GROUP, S5_STATE), 0.5),
        "s5_c_im": nrm(ks[19], (L, S5_GROUPS, S5_GROUP, S5_STATE), 0.5),
        "s5_d": nrm(ks[20], (L, S5_WIDTH), 1.0),
        "glu_w": nrm(ks[21], (L, S5_WIDTH, S5_WIDTH), S5_WIDTH ** -0.5),
        "glu_b": nrm(ks[22], (L, S5_WIDTH), 0.02),
        "w_up_attn": nrm(ks[23], (L, NSA_WIDTH, D_MODEL), NSA_WIDTH ** -0.5),
        "w_up_ssm": nrm(ks[24], (L, S5_WIDTH, D_MODEL), S5_WIDTH ** -0.5),
        "w_out": nrm(ks[25], (L, D_MODEL, D_MODEL), D_MODEL ** -0.5),
        "peer_w_q": nrm(ks[26], (L, D_MODEL, PEER_HEADS * PEER_QDIM), D_MODEL ** -0.5),
        "peer_sub_keys": nrm(ks[27], (L, PEER_HEADS, 2, PEER_KEYS, PEER_QDIM // 2), (PEER_QDIM // 2) ** -0.5),
        "peer_u": nrm(ks[28], (L, PEER_EXPERTS, D_MODEL), D_MODEL ** -0.5),
        "peer_v": nrm(ks[29], (L, PEER_EXPERTS, D_MODEL), 0.5),
        "final_g": 1.0 + nrm(ks[30], (D_MODEL,), 0.02),
    }


def reference(x, c, ada_w, ada_b, norm_mix_g, norm_ffn_g, w_in, cmp_pos, cmp_w1, cmp_b1, cmp_w2, cmp_b2,
              rel_bias, s5_lam_re, s5_lam_im, s5_log_step, s5_b_re, s5_b_im, s5_c_re, s5_c_im, s5_d,
              glu_w, glu_b, w_up_attn, w_up_ssm, w_out, peer_w_q, peer_sub_keys, peer_u, peer_v, final_g):
    b, l, _ = x.shape
    points = _split_points()
    for i in range(DEPTH):
        mod = jax.nn.silu(c) @ ada_w[i] + ada_b[i]
        sh1, sc1, g1, sh2, sc2, g2 = jnp.split(mod[:, None, :], 6, axis=-1)

        h = rms_norm(x, norm_mix_g[i]) * (1.0 + sc1) + sh1
        proj = h @ w_in[i]
        q, kc, vc, ks_, vs_, kw, vw, nsa_gates, u, merge_g = jnp.split(proj, points, axis=-1)
        kv = lambda t_: t_.reshape(b, l, NSA_KV_HEADS, HEAD_DIM)
        kc_c = compress_kv(kv(kc), cmp_pos[i, 0], cmp_w1[i, 0], cmp_b1[i, 0], cmp_w2[i, 0], cmp_b2[i, 0])
        vc_c = compress_kv(kv(vc), cmp_pos[i, 1], cmp_w1[i, 1], cmp_b1[i, 1], cmp_w2[i, 1], cmp_b2[i, 1])
        o_attn = nsa(q.reshape(b, l, NSA_HEADS, HEAD_DIM), kc_c, vc_c, kv(ks_), kv(vs_), kv(kw), kv(vw),
                     nsa_gates, rel_bias)

        y = s5(u, s5_lam_re[i], s5_lam_im[i], s5_log_step[i], s5_b_re[i], s5_b_im[i],
               s5_c_re[i], s5_c_im[i], s5_d[i])
        y = jax.nn.gelu(y)
        y = y * jax.nn.sigmoid(y @ glu_w[i] + glu_b[i])

        ga, gb = jnp.split(jax.nn.sigmoid(merge_g), N_BRANCHES, axis=-1)
        mixed = ga * (o_attn @ w_up_attn[i]) + gb * (y @ w_up_ssm[i])
        x = x + g1 * (mixed @ w_out[i])

        h2 = rms_norm(x, norm_ffn_g[i]) * (1.0 + sc2) + sh2
        x = x + g2 * peer(h2, peer_w_q[i], peer_sub_keys[i], peer_u[i], peer_v[i])
    return rms_norm(x, final_g)
```

```python
import contextlib
import math
import os
import numpy as np
import concourse.bass as bass
import concourse.mybir as mybir
from concourse.bass_utils import run_bass_kernel_spmd

F32 = mybir.dt.float32
BF16 = mybir.dt.bfloat16
U32 = mybir.dt.uint32
AF = mybir.ActivationFunctionType
ALU = mybir.AluOpType
AX = mybir.AxisListType

D = 2048
TOWN = 2048
TALL = 4096
KC = 16
INW = 7728
EPS = 1e-6


class _Stop(Exception):
    pass


CUT = int(os.environ.get("KCUT", "0"))


class Buf:
    def __init__(self, name, dsem=None):
        self.name = name
        self.dma = False
        self.kind = None
        self.w = None
        self.r = {}
        self.dsem = dsem
        self.dcount = 0


class Sched:
    LIMIT = 4000

    def __init__(self, nc, es):
        self.nc = nc
        self.es = es
        self.eng = {"pe": nc.tensor, "dve": nc.vector, "act": nc.scalar, "pool": nc.gpsimd, "sp": nc.sync}
        self.sem = {}
        self.cnt = {}
        self.semid = {}
        self.nsem = 0
        self.waited = {e: {} for e in self.eng}
        self.dmabufs = []
        self.freesems = {}
        self.ninst = 0
        for e in self.eng:
            self._newsem(e)

    def _newsem(self, e):
        s = self.es.enter_context(self.nc.semaphore(f"s{self.nsem}_{e}"))
        self.nsem += 1
        self.sem[e] = (s, self.nsem)
        self.cnt[e] = 0

    def buf(self, name, dma=False):
        b = Buf(name)
        b.dma = dma
        b.kind = None
        return b

    def _dsem(self, b, kind):
        if b.dsem is None:
            assert b.dma, b.name
            fl = self.freesems.setdefault(kind, [])
            if fl:
                b.dsem, b.dcount = fl.pop()
            else:
                b.dsem = (self.es.enter_context(self.nc.semaphore(f"d{self.nsem}")), self.nsem + 1)
                self.nsem += 1
                b.dcount = 0
            b.kind = kind
            self.dmabufs.append(b)
        assert b.kind == kind, (b.name, b.kind, kind)

    def retire(self, bufs):
        for b in bufs:
            if b.dsem is not None and b in self.dmabufs:
                self.dmabufs.remove(b)
                self.freesems.setdefault(b.kind, []).append((b.dsem, b.dcount))
                b.dsem = None
                b.dma = False

    def _wait(self, X, tok):
        if tok[0] == "dma":
            b = tok[1]
            if b.dsem is None or b.dcount == 0:
                return
            sem, sid = b.dsem
            val = 16 * b.dcount
            E = None
        else:
            (sem, sid), val, E = tok
        if E == X and X in ("pe", "sp"):
            return
        if self.waited[X].get(sid, 0) >= val:
            return
        self.eng[X].wait_ge(sem, val)
        self.waited[X][sid] = val
        self.ninst += 1

    def _deps(self, X, reads, writes):
        for b in reads:
            if b.w is not None:
                self._wait(X, b.w)
        for b in writes:
            if b.w is not None:
                self._wait(X, b.w)
            for t in b.r.values():
                self._wait(X, t)

    def op(self, X, fn, reads=(), writes=()):
        ex = [b for b in reads if getattr(b, "excl", False) and b not in writes]
        if ex:
            writes = list(writes) + ex
            reads = [b for b in reads if b not in ex]
        self._deps(X, reads, writes)
        inst = fn(self.eng[X])
        if self.cnt[X] >= self.LIMIT:
            self._newsem(X)
        self.cnt[X] += 1
        inst.then_inc(self.sem[X][0], 1)
        tok = (self.sem[X], self.cnt[X], X)
        for b in reads:
            b.r[X] = tok
        for b in writes:
            b.w = tok
            b.r = {}
        self.ninst += 1
        return tok

    def dma(self, X, out, in_, reads, wbuf, **kw):
        self._dsem(wbuf, "sw" if X == "pool" else "hw")
        self._deps(X, reads, [wbuf])
        inst = self.eng[X].dma_start(out=out, in_=in_, **kw)
        inst.then_inc(wbuf.dsem[0], 16)
        wbuf.dcount += 1
        wbuf.w = ("dma", wbuf)
        wbuf.r = {}
        for b in reads:
            b.r[("dma", id(wbuf))] = ("dma", wbuf)
        self.ninst += 1

    def barrier(self):
        for X in self.eng:
            for E in self.eng:
                if E != X and self.cnt[E] > 0:
                    self._wait(X, (self.sem[E], self.cnt[E], E))
            for b in self.dmabufs:
                if b.dsem is not None and b.dcount > 0:
                    self._wait(X, ("dma", b))


class KB:
    def __init__(self, stages=99, debug=()):
        self.stages = stages
        self.debug = debug
        self.nc = bass.Bass("TRN2", target_bir_lowering=False)
        self.es = contextlib.ExitStack()
        self.S = Sched(self.nc, self.es)
        self.inputs = {}
        self.psn = 0
        self.ctxbufs = {}

    def din(self, name, shape, dtype=F32):
        t = self.nc.dram_tensor(name, list(shape), dtype, kind="ExternalInput").ap()
        self.inputs[name] = t
        return t

    def dscratch(self, name, shape, dtype):
        t = self.nc.dram_tensor(name, list(shape), dtype, kind="Internal").ap()
        return t, self.S.buf(name, dma=True)

    def sb(self, ctx, name, shape, dtype, dma=False):
        t = ctx.enter_context(self.nc.sbuf_tensor(name, list(shape), dtype))
        b = self.S.buf(name, dma=dma)
        self.ctxbufs.setdefault(id(ctx), []).append(b)
        return t, b

    def end(self, ctx):
        self.S.barrier()
        self.S.retire(self.ctxbufs.pop(id(ctx), []))

    def psum_init(self):
        self.ps = []
        for i in range(7):
            t = self.es.enter_context(self.nc.psum_tensor(f"ps{i}", [128, 512], F32))
            self.ps.append((t, self.S.buf(f"ps{i}")))
            self.ps[-1][1].excl = True
        t = self.es.enter_context(self.nc.psum_tensor("psbf", [128, 1024], BF16))
        self.psbf = (t, self.S.buf("psbf"))
        self.psbf[1].excl = True

    def psum(self, nrot=4):
        r = self.ps[self.psn % nrot]
        self.psn += 1
        return r


def build(stages=99, debug=(), skip=(), sub=99):
    kb = KB(stages, debug)
    nc, S = kb.nc, kb.S
    kb.psum_init()
    es = kb.es

    xT = kb.din("xT", [D, TALL])
    xTr = kb.din("xTr", [D, TALL])
    cT = kb.din("cT", [128, KC])
    ada_w = kb.din("ada_w", [D, 6 * D])
    ada_bT = kb.din("ada_bT", [128, 96])
    gmixT = kb.din("gmixT", [128, KC])
    gffnT = kb.din("gffnT", [128, KC])
    gfinT = kb.din("gfinT", [128, KC])
    w_in = kb.din("w_in", [D, INW])
    ctxflag = kb.din("ctxflag", [128, 1])

    outT = nc.dram_tensor("outT", [D, TOWN], F32, kind="ExternalOutput").ap()
    outT_b = S.buf("outT", dma=True)

    qT_d, qT_b = kb.dscratch("qT_d", [1024, TOWN], BF16)
    kcT_d, kcT_b = kb.dscratch("kcT_d", [256, TALL], BF16)
    vcT_d, vcT_b = kb.dscratch("vcT_d", [256, TALL], BF16)
    ksT_d, ksT_b = kb.dscratch("ksT_d", [256, TALL], BF16)
    kwT_d, kwT_b = kb.dscratch("kwT_d", [256, TALL], BF16)
    vs_d, vs_b = kb.dscratch("vs_d", [TALL, 256], BF16)
    vw_d, vw_b = kb.dscratch("vw_d", [TALL, 256], BF16)
    gT_d, gT_b = kb.dscratch("gT_d", [48, TOWN], BF16)
    uT_d, uT_b = kb.dscratch("uT_d", [1024, TALL], BF16)
    mgT_d, mgT_b = kb.dscratch("mgT_d", [4096, TOWN], BF16)

    pc = es
    ones_bf, ones_bf_b = kb.sb(pc, "ones_bf", [128, 128], BF16)
    eps_t, eps_b = kb.sb(pc, "eps_t", [128, 1], F32)
    mod, mod_b = kb.sb(pc, "mod", [128, 96], F32)
    gm1, gm1_b = kb.sb(pc, "gm1", [128, KC], F32)
    gm2, gm2_b = kb.sb(pc, "gm2", [128, KC], F32)
    gfin, gfin_b = kb.sb(pc, "gfin", [128, KC], F32, dma=True)
    flag_t, flag_b = kb.sb(pc, "flag_t", [128, 1], F32, dma=True)
    S.op("dve", lambda e: e.memset(ones_bf[:], 1.0), writes=[ones_bf_b])
    S.op("dve", lambda e: e.memset(eps_t[:], EPS), writes=[eps_b])
    S.dma("sp", gfin[:], gfinT[:, :], [], gfin_b)
    S.dma("sp", flag_t[:], ctxflag[:, :], [], flag_b)

    with contextlib.ExitStack() as ph:
        c_sb, c_b = kb.sb(ph, "c_sb", [128, KC], F32, dma=True)
        sc_sb, sc_b = kb.sb(ph, "sc_sb", [128, KC], F32)
        ab_sb, ab_b = kb.sb(ph, "ab_sb", [128, 96], F32, dma=True)
        gx_sb, gx_b = kb.sb(ph, "gx_sb", [128, 2 * KC], F32, dma=True)
        wts = [kb.sb(ph, f"adaw{i}", [128, KC, 512], F32, dma=True) for i in range(2)]
        S.dma("sp", c_sb[:], cT[:, :], [], c_b)
        S.dma("sp", ab_sb[:], ada_bT[:, :], [], ab_b)
        S.dma("sp", gx_sb[:, 0:KC], gmixT[:, :], [], gx_b)
        S.dma("sp", gx_sb[:, KC:2 * KC], gffnT[:, :], [], gx_b)
        S.op("act", lambda e: e.activation(out=sc_sb[:], in_=c_sb[:], func=AF.Silu), reads=[c_b], writes=[sc_b])
        pst, psb = kb.psum()
        awv = ada_w.rearrange("(k p) f -> p k f", p=128)
        for fg in range(24):
            wt, wb = wts[fg % 2]
            S.dma("sp" if fg % 2 == 0 else "act", wt[:], awv[:, :, fg * 512:(fg + 1) * 512], [], wb)
            for fc in range(4):
                col = fg * 4 + fc
                for k in range(KC):
                    S.op("pe", lambda e, k=k, fc=fc, col=col, wt=wt: e.matmul(
                        pst[:, col:col + 1], lhsT=wt[:, k, fc * 128:(fc + 1) * 128], rhs=sc_sb[:, k:k + 1],
                        start=(k == 0), stop=(k == KC - 1)), reads=[wb, sc_b], writes=[psb])
        S.op("dve", lambda e: e.tensor_tensor(out=mod[:], in0=pst[:, 0:96], in1=ab_sb[:], op=ALU.add),
             reads=[psb, ab_b], writes=[mod_b])
        S.op("dve", lambda e: e.scalar_tensor_tensor(out=gm1[:], in0=mod[:, 16:32], scalar=1.0, in1=gx_sb[:, 0:KC],
                                                     op0=ALU.add, op1=ALU.mult), reads=[mod_b, gx_b], writes=[gm1_b])
        S.op("dve", lambda e: e.scalar_tensor_tensor(out=gm2[:], in0=mod[:, 64:80], scalar=1.0, in1=gx_sb[:, KC:2 * KC],
                                                     op0=ALU.add, op1=ALU.mult), reads=[mod_b, gx_b], writes=[gm2_b])
        kb.end(ph)

    SH1, G1, SH2, G2 = 0, 32, 48, 80

    def norm_block(ph, src_ap_fn, t0, nt, hT, hT_b, gm, gm_b, shcol, xbufs, tmpbufs, sqbuf, rbuf):
        TT = 256
        for ti in range(nt // TT):
            xt, xb = xbufs[ti % 2]
            S.dma("sp", xt[:], src_ap_fn(t0 + ti * TT, TT), [], xb)
            sq, sqb = sqbuf
            S.op("act", lambda e, xt=xt: e.activation(out=sq[:], in_=xt[:], func=AF.Square), reads=[xb], writes=[sqb])
            pst, psb = kb.psum()
            for k in range(KC):
                S.op("pe", lambda e, k=k: e.matmul(pst[:, 0:TT], lhsT=ones_bf[:], rhs=sq[:, k, :],
                                                   start=(k == 0), stop=(k == KC - 1)),
                     reads=[ones_bf_b, sqb], writes=[psb])
            rt, rb = rbuf
            S.op("act", lambda e: e.activation(out=rt[:], in_=pst[:, 0:TT], func=AF.Sqrt, bias=eps_t[:, 0:1],
                                               scale=1.0 / D), reads=[psb, eps_b], writes=[rb])
            S.op("dve", lambda e: e.reciprocal(out=rt[:], in_=rt[:]), reads=[rb], writes=[rb])
            for k in range(KC):
                tt, tb = tmpbufs[k % 2]
                S.op("dve", lambda e, k=k, tt=tt, xt=xt: e.scalar_tensor_tensor(
                    out=tt[:], in0=xt[:, k, :], scalar=gm[:, k:k + 1], in1=rt[:], op0=ALU.mult, op1=ALU.mult),
                    reads=[xb, gm_b, rb], writes=[tb])
                S.op("act", lambda e, k=k, tt=tt, ti=ti: e.activation(
                    out=hT[:, k, ti * TT:(ti + 1) * TT], in_=tt[:], func=AF.Identity,
                    bias=mod[:, shcol + k:shcol + k + 1], scale=1.0), reads=[tb, mod_b], writes=[hT_b])

    if stages >= 2:
        with contextlib.ExitStack() as ph:
            TB = 1024
            hT, hT_b = kb.sb(ph, "hT", [128, KC, TB], BF16)
            xbufs = [kb.sb(ph, f"xb{i}", [128, KC, 256], F32, dma=True) for i in range(2)]
            tmpbufs = [kb.sb(ph, f"ntmp{i}", [128, 256], F32) for i in range(2)]
            sqbuf = kb.sb(ph, "sqb", [128, KC, 256], BF16)
            rbuf = kb.sb(ph, "rstd", [128, 256], F32)
            wbufs = [kb.sb(ph, f"wbuf{i}", [128, KC, 512], BF16, dma=True) for i in range(2)]
            obufs = [kb.sb(ph, f"obuf{i}", [128, 512], BF16) for i in range(4)]
            xTv = xT.rearrange("(k p) t -> p k t", p=128)
            winv = w_in.rearrange("(k p) c -> p k c", p=128)
            wcount = [0]
            ocount = [0]

            def load_w(col0, ncols):
                wt, wb = wbufs[wcount[0] % 2]
                wcount[0] += 1
                S.dma("pool", wt[:, :, 0:ncols], winv[:, :, col0:col0 + ncols], [], wb)
                return wt, wb

            def fm_cols(col0, ncols, tb0, epi):
                wt, wb = load_w(col0, ncols)
                for cc in range((ncols + 127) // 128):
                    cw = min(128, ncols - cc * 128)
                    for tt in range(TB // 512):
                        pst, psb = kb.psum()
                        for k in range(KC):
                            S.op("pe", lambda e, k=k, cc=cc, cw=cw, tt=tt, pst=pst, wt=wt: e.matmul(
                                pst[0:cw, :], lhsT=wt[:, k, cc * 128:cc * 128 + cw], rhs=hT[:, k, tt * 512:(tt + 1) * 512],
                                start=(k == 0), stop=(k == KC - 1)), reads=[wb, hT_b], writes=[psb])
                        epi(col0 + cc * 128, cw, tb0 + tt * 512, pst, psb)

            def store_epi(dst_d, dst_b, rowbase, tbase, func=AF.Copy, scale=1.0, flag=False):
                def epi(col, cw, t, pst, psb):
                    ot, ob = obufs[ocount[0] % 4]
                    ocount[0] += 1
                    if flag:
                        S.op("act", lambda e: e.activation(out=ot[0:cw, :], in_=pst[0:cw, :], func=AF.Copy,
                                                           scale=flag_t[0:cw, 0:1]), reads=[psb, flag_b], writes=[ob])
                    else:
                        S.op("act", lambda e: e.activation(out=ot[0:cw, :], in_=pst[0:cw, :], func=func, scale=scale),
                             reads=[psb], writes=[ob])
                    r0 = col - rowbase
                    S.dma("sp", dst_d[r0:r0 + cw, t - tbase:t - tbase + 512], ot[0:cw, :], [ob], dst_b)
                return epi

            def tm_cols(col0, ncols, tb0, dst_d, dst_b):
                wt, wb = load_w(col0, ncols)
                for sc in range(TB // 128):
                    pst, psb = kb.psum()
                    for k in range(KC):
                        S.op("pe", lambda e, k=k, sc=sc, pst=pst, wt=wt: e.matmul(
                            pst[:, 0:ncols], lhsT=hT[:, k, sc * 128:(sc + 1) * 128], rhs=wt[:, k, 0:ncols],
                            start=(k == 0), stop=(k == KC - 1)), reads=[wb, hT_b], writes=[psb])
                    ot, ob = obufs[ocount[0] % 4]
                    ocount[0] += 1
                    S.op("act", lambda e, pst=pst, ot=ot: e.activation(out=ot[:, 0:ncols], in_=pst[:, 0:ncols], func=AF.Copy),
                         reads=[psb], writes=[ob])
                    S.dma("sp", dst_d[tb0 + sc * 128:tb0 + (sc + 1) * 128, :], ot[:, 0:ncols], [ob], dst_b)

            xTrv = xTr.rearrange("(k p) t -> p k t", p=128)
            for rev in (False, True):
                srcv = xTrv if rev else xTv
                for blk in range(TALL // TB):
                    tb0 = blk * TB
                    own = tb0 >= TALL - TOWN
                    norm_block(ph, lambda t, n, srcv=srcv: srcv[:, :, t:t + n], tb0, TB, hT, hT_b, gm1, gm1_b, SH1,
                               xbufs, tmpbufs, sqbuf, rbuf)
                    if rev:
                        fm_cols(1536, 256, tb0, store_epi(ksT_d, ksT_b, 1536, 0))
                        fm_cols(2048, 256, tb0, store_epi(kwT_d, kwT_b, 2048, 0))
                        tm_cols(1792, 256, tb0, vs_d, vs_b)
                        tm_cols(2304, 256, tb0, vw_d, vw_b)
                        continue
                    fm_cols(1024, 256, tb0, store_epi(kcT_d, kcT_b, 1024, 0))
                    fm_cols(1280, 256, tb0, store_epi(vcT_d, vcT_b, 1280, 0))
                    fm_cols(2608, 512, tb0, store_epi(uT_d, uT_b, 2608, 0, flag=not own))
                    fm_cols(3120, 512, tb0, store_epi(uT_d, uT_b, 2608, 0, flag=not own))
                    if own:
                        tq = TALL - TOWN
                        fm_cols(0, 512, tb0, store_epi(qT_d, qT_b, 0, tq, scale=0.125))
                        fm_cols(512, 512, tb0, store_epi(qT_d, qT_b, 0, tq, scale=0.125))
                        fm_cols(2560, 48, tb0, store_epi(gT_d, gT_b, 2560, tq, func=AF.Sigmoid))
                        for g in range(8):
                            fm_cols(3632 + g * 512, 512, tb0, store_epi(mgT_d, mgT_b, 3632, tq, func=AF.Sigmoid))
            kb.end(ph)


    oT_d, oT_b = kb.dscratch("oT_d", [1024, TOWN], BF16)
    LB, OFFB, LC, OFFC = 4096, 1024, 6400, 2064
    if stages >= 3 and 3 not in skip:
        cmp_w1 = kb.din("cmp_w1", [2, 2048, 256])
        cmp_posT = kb.din("cmp_posT", [2, 64, 32])
        cmp_b1T = kb.din("cmp_b1T", [2, 128, 2])
        cmp_w2 = kb.din("cmp_w2", [2, 256, 64])
        cmp_b2 = kb.din("cmp_b2", [2, 64])
        rel_bias = kb.din("rel_bias", [32, 16])
        ident_in = kb.din("ident", [128, 128])
        jmat_in = kb.din("jmat", [128, 128])
        selg_in = kb.din("selg", [64, 48 * 64])
        negE_in = kb.din("negE", [128, 32 * 128])
        maug_in = kb.din("maug", [128, 2 * 65])
        selbias_in = kb.din("selbias", [128, 16 * 64])
        selinv_in = kb.din("selinv", [128, 16 * 64])
        Vb_in = kb.din("Vb", [16, LB])
        Vw_in = kb.din("Vw", [16, LB])
        Vwc_in = kb.din("Vwc", [16, LB])
        Vc_in = kb.din("Vc", [16, LC])
        Vc0_in = kb.din("Vc0", [16, LC])
        with contextlib.ExitStack() as ph:
          try:
              identb, identb_b = kb.sb(ph, "identb", [128, 128], BF16, dma=True)
              Jb, Jb_b = kb.sb(ph, "Jb", [128, 128], BF16, dma=True)
              selg, selg_b = kb.sb(ph, "selg_sb", [64, 48 * 64], BF16, dma=True)
              negE, negE_b = kb.sb(ph, "negE_sb", [128, 32 * 128], BF16, dma=True)
              maug, maug_b = kb.sb(ph, "maug_sb", [128, 2 * 65], BF16, dma=True)
              selbias, selbias_b = kb.sb(ph, "selbias_sb", [128, 16 * 64], F32, dma=True)
              rb31, rb31_b = kb.sb(ph, "rb31", [128, 16], F32, dma=True)
              qT_sb, qT_sbb = kb.sb(ph, "qT_sb", [128, 8, TOWN], BF16, dma=True)
              gT_sb, gT_sbb = kb.sb(ph, "gT_sb", [64, TOWN], BF16, dma=True)
              kccT = [kb.sb(ph, f"kccT{i}", [128, 256], BF16) for i in range(4)]
              vcc = [kb.sb(ph, f"vcc{i}", [128, 2, 64], BF16) for i in range(4)]
              ones64, ones64_b = kb.sb(ph, "ones64", [128, 64], BF16)
              S.op("dve", lambda e: e.memset(ones64[:], 1.0), writes=[ones64_b])
              S.dma("pool", identb[:], ident_in[:, :], [], identb_b)
              S.dma("pool", Jb[:], jmat_in[:, :], [], Jb_b)
              S.dma("pool", selg[:].rearrange("p (a b) -> p a b", b=1024), selg_in.rearrange("p (a b) -> p a b", b=1024), [], selg_b)
              S.dma("pool", negE[:].rearrange("p (a b) -> p a b", b=1024), negE_in.rearrange("p (a b) -> p a b", b=1024), [], negE_b)
              S.dma("pool", maug[:], maug_in[:, :], [], maug_b)
              S.dma("sp", selbias[:], selbias_in[:, :], [], selbias_b)
              selinv, selinv_b = kb.sb(ph, "selinv_sb", [128, 16 * 64], F32, dma=True)
              S.dma("sp", selinv[:], selinv_in[:, :], [], selinv_b)
              S.dma("sp", rb31[:], rel_bias[31:32, :].partition_broadcast(128), [], rb31_b)
              S.dma("sp", qT_sb[:], qT_d.rearrange("(c p) t -> p c t", p=128), [qT_b], qT_sbb)
              S.op("dve", lambda e: e.memset(gT_sb[:], 0.0), writes=[gT_sbb])
              S.dma("sp", gT_sb[0:48, :], gT_d[:, :], [gT_b], gT_sbb)

              with contextlib.ExitStack() as pd:
                  w1r, w1r_b = kb.sb(pd, "w1r", [64, 32, 256], BF16, dma=True)
                  posT, posT_b = kb.sb(pd, "posT", [64, 32], BF16, dma=True)
                  b1T, b1T_b = kb.sb(pd, "b1T", [128, 2], F32, dma=True)
                  w2d, w2d_b = kb.sb(pd, "w2d", [128, 2, 64], BF16, dma=True)
                  b2row, b2row_b = kb.sb(pd, "b2row", [128, 64], F32, dma=True)
                  biasH, biasH_b = kb.sb(pd, "biasH", [128, 2], F32)
                  kch, kch_b = kb.sb(pd, "kch", [64, TALL], BF16, dma=True)
                  hg = [kb.sb(pd, f"hg{i}", [128, 256], BF16) for i in range(2)]
                  kdup, kdup_b = kb.sb(pd, "kdup", [128, 128], BF16)
                  vtm, vtm_b = kb.sb(pd, "vtm", [128, 64], BF16)
                  for i in range(2):
                      S.op("dve", lambda e, i=i: e.memset(hg[i][0][:], 0.0), writes=[hg[i][1]])
                  for j in range(2 if CUT != 1 else 0):
                      S.dma("pool", w1r[:], cmp_w1[j].rearrange("(pos dh) hid -> dh pos hid", dh=64), [], w1r_b)
                      S.dma("pool", posT[:], cmp_posT[j], [], posT_b)
                      S.dma("sp", b1T[:], cmp_b1T[j], [], b1T_b)
                      S.dma("pool", w2d[:], cmp_w2[j].rearrange("(c p) d -> p c d", p=128), [], w2d_b)
                      S.dma("sp", b2row[:], cmp_b2[j:j + 1, :].partition_broadcast(128), [], b2row_b)
                      pst, psb = kb.ps[4]
                      for hc in range(2):
                          for pos in range(32):
                              S.op("pe", lambda e, hc=hc, pos=pos: e.matmul(
                                  pst[:, hc:hc + 1], lhsT=w1r[0:64, pos, hc * 128:(hc + 1) * 128], rhs=posT[0:64, pos:pos + 1],
                                  start=(pos == 0), stop=(pos == 31)), reads=[w1r_b, posT_b], writes=[psb])
                      S.op("dve", lambda e: e.tensor_tensor(out=biasH[:], in0=pst[:, 0:2], in1=b1T[:], op=ALU.add),
                           reads=[psb, b1T_b], writes=[biasH_b])
                      src_d, src_b = (kcT_d, kcT_b) if j == 0 else (vcT_d, vcT_b)
                      for kvh in range(4 if CUT != 2 else 0):
                          S.dma("sp", kch[:], src_d[kvh * 64:(kvh + 1) * 64, :], [src_b], kch_b)
                          for hc in range(2):
                              pst, psb = kb.psum()
                              for pos in range(32):
                                  S.op("pe", lambda e, hc=hc, pos=pos, pst=pst: e.matmul(
                                      pst[:, 0:255], lhsT=w1r[0:64, pos, hc * 128:(hc + 1) * 128],
                                      rhs=kch[0:64, pos:pos + 4065:16], start=(pos == 0), stop=(pos == 31)),
                                      reads=[w1r_b, kch_b], writes=[psb])
                              S.op("act", lambda e, hc=hc, pst=pst: e.activation(
                                  out=hg[hc][0][:, 0:255], in_=pst[:, 0:255], func=AF.Gelu_apprx_tanh,
                                  bias=biasH[:, hc:hc + 1], scale=1.0), reads=[psb, biasH_b], writes=[hg[hc][1]])
                          for nch in range(0 if (CUT == 3 or (CUT == 4 and j == 1) or (CUT == 5 and j == 0)) else 2):
                              pst, psb = kb.psum()
                              for hc in range(2):
                                  S.op("pe", lambda e, hc=hc, nch=nch, pst=pst: e.matmul(
                                      pst[:, 0:64], lhsT=hg[hc][0][:, nch * 128:(nch + 1) * 128], rhs=w2d[:, hc, :],
                                      start=(hc == 0), stop=(hc == 1)), reads=[hg[hc][1], w2d_b], writes=[psb])
                              pst3, psb3 = kb.psum()
                              if j == 0:
                                  for hh in range(2):
                                      S.op("dve", lambda e, hh=hh, pst=pst: e.tensor_tensor(
                                          out=kdup[:, hh * 64:(hh + 1) * 64], in0=pst[:, 0:64], in1=b2row[:], op=ALU.add),
                                          reads=[psb, b2row_b], writes=[kdup_b])
                                  S.op("pe", lambda e, pst3=pst3: e.matmul(pst3[:, 0:128], lhsT=kdup[:], rhs=Jb[:], start=True, stop=True),
                                       reads=[kdup_b, Jb_b], writes=[psb3])
                                  S.op("act", lambda e, kvh=kvh, nch=nch, pst3=pst3: e.activation(
                                      out=kccT[kvh][0][:, nch * 128:(nch + 1) * 128], in_=pst3[:, 0:128], func=AF.Copy),
                                      reads=[psb3], writes=[kccT[kvh][1]])
                              else:
                                  S.op("dve", lambda e, pst=pst: e.tensor_tensor(out=vtm[:], in0=pst[:, 0:64], in1=b2row[:], op=ALU.add),
                                       reads=[psb, b2row_b], writes=[vtm_b])
                                  S.op("pe", lambda e, pst3=pst3: e.matmul(pst3[:, 0:64], lhsT=Jb[:], rhs=vtm[:], start=True, stop=True),
                                       reads=[vtm_b, Jb_b], writes=[psb3])
                                  S.op("act", lambda e, kvh=kvh, nch=nch, pst3=pst3: e.activation(
                                      out=vcc[kvh][0][:, nch, :], in_=pst3[:, 0:64], func=AF.Copy),
                                      reads=[psb3], writes=[vcc[kvh][1]])
                  kb.end(pd)

              ksT2, ksT2_b = kb.sb(ph, "ksT2", [128, TALL], BF16, dma=True)
              kwT2, kwT2_b = kb.sb(ph, "kwT2", [128, TALL], BF16, dma=True)
              vs_all, vs_all_b = kb.sb(ph, "vs_all", [128, 32, 256], BF16, dma=True)
              vw_all, vw_all_b = kb.sb(ph, "vw_all", [128, 32, 256], BF16, dma=True)
              notselT, notselT_b = kb.sb(ph, "notselT", [128, TOWN], BF16)
              oacc, oacc_b = kb.sb(ph, "oacc", [64, 4, TOWN], F32)
              impacc, impacc_b = kb.sb(ph, "impacc", [128, 16, 64], F32)
              btiles = [kb.sb(ph, f"btile{i}", [128, 512], F32, dma=True) for i in range(6)]
              sfp = [kb.sb(ph, f"sfp{i}", [128, 512], F32) for i in range(4)]
              pus = [kb.sb(ph, f"pu{i}", [128, 512], BF16) for i in range(8)]
              rec, rec_b = kb.sb(ph, "rec", [128, 512], F32)
              fac, fac_b = kb.sb(ph, "fac", [128, 512], F32)
              otmp, otmp_b = kb.sb(ph, "otmp", [128, 512], F32)
              rec2, rec2_b = kb.sb(ph, "rec2", [128, 1], F32)
              simp, simp_b = kb.sb(ph, "simp", [128, 64], F32)
              simp2, simp2_b = kb.sb(ph, "simp2", [128, 64], F32)
              m8a, m8a_b = kb.sb(ph, "m8a", [128, 8], F32)
              m8b, m8b_b = kb.sb(ph, "m8b", [128, 8], F32)
              nsel, nsel_b = kb.sb(ph, "nsel", [128, 128], BF16)
              obf, obf_b = kb.sb(ph, "obf", [64, 4, TOWN], BF16)
              cnt = {"bt": 0, "sf": 0, "pu": 0}
              pso, pso_b = kb.ps[6]
              psg, psg_b = kb.ps[5]
              psi, psi_b = kb.ps[4]
              psd, psd_b = kb.ps[3]
              psbf, psbf_b = kb.psbf

              def hankel(src_in, h, c0, pstep):
                  bt, bb = btiles[cnt["bt"] % 6]
                  cnt["bt"] += 1
                  src = bass.AP(tensor=src_in.tensor, offset=h * src_in.shape[1] + c0, ap=[[pstep, 128], [1, 512]])
                  S.dma("sp" if cnt["bt"] % 2 else "act", bt[:], src, [], bb)
                  return bt, bb

              def finish_branch(hq, br, tt, first):
                  g_ = hq % 4
                  tsl = slice(tt * 512, (tt + 1) * 512)
                  S.op("dve", lambda e: e.tensor_scalar(out=rec[0:64, :], in0=psd[0:64, :], scalar1=1e-30,
                                                        scalar2=None, op0=ALU.add), reads=[psd_b], writes=[rec_b])
                  S.op("dve", lambda e: e.reciprocal(out=rec[0:64, :], in_=rec[0:64, :]), reads=[rec_b], writes=[rec_b])
                  gi = hq * 3 + br
                  S.op("pe", lambda e: e.matmul(psg[0:64, :], lhsT=selg[0:64, gi * 64:(gi + 1) * 64], rhs=gT_sb[0:64, tsl],
                                                start=True, stop=True), reads=[selg_b, gT_sbb], writes=[psg_b])
                  S.op("dve", lambda e: e.tensor_tensor(out=fac[0:64, :], in0=psg[0:64, :], in1=rec[0:64, :], op=ALU.mult),
                       reads=[psg_b, rec_b], writes=[fac_b])
                  if first:
                      S.op("dve", lambda e: e.tensor_tensor(out=oacc[:, g_, tsl], in0=pso[0:64, :], in1=fac[0:64, :], op=ALU.mult),
                           reads=[pso_b, fac_b], writes=[oacc_b])
                  else:
                      S.op("dve", lambda e: e.tensor_tensor(out=otmp[0:64, :], in0=pso[0:64, :], in1=fac[0:64, :], op=ALU.mult),
                           reads=[pso_b, fac_b], writes=[otmp_b])
                      S.op("pool", lambda e: e.tensor_tensor(out=oacc[:, g_, tsl], in0=oacc[:, g_, tsl], in1=otmp[0:64, :], op=ALU.add),
                           reads=[otmp_b, oacc_b], writes=[oacc_b])

              for kvh in range(4 if sub >= 2 else 0):
                  for g in range(4):
                      hq = kvh * 4 + g
                      base = 64 * (hq % 2)
                      qc = hq // 2
                      for tt in range(4):
                          tsl = slice(tt * 512, (tt + 1) * 512)
                          pul = []
                          for nch in range(2):
                              c0 = OFFC + (2048 + 512 * tt - 2048 * nch - 2063)
                              bt, bb = hankel(Vc0_in if nch == 0 else Vc_in, hq, c0, 16)
                              pst, psb = kb.psum(3)
                              S.op("pe", lambda e, pst=pst, nch=nch: e.matmul(
                                  pst[:, :], lhsT=kccT[kvh][0][base:base + 64, nch * 128:(nch + 1) * 128],
                                  rhs=qT_sb[base:base + 64, qc, tsl], start=True, stop=True),
                                  reads=[kccT[kvh][1], qT_sbb], writes=[psb])
                              sf, sfb = sfp[cnt["sf"] % 4]
                              cnt["sf"] += 1
                              S.op("dve", lambda e, pst=pst, sf=sf, bt=bt: e.tensor_tensor(out=sf[:], in0=pst[:, :], in1=bt[:], op=ALU.add),
                                   reads=[psb, bb], writes=[sfb])
                              pu, pub = pus[cnt["pu"] % 8]
                              cnt["pu"] += 1
                              S.op("act", lambda e, sf=sf, pu=pu: e.activation(out=pu[:], in_=sf[:], func=AF.Exp),
                                   reads=[sfb], writes=[pub])
                              pul.append((pu, pub))
                          for nch in range(2):
                              S.op("pe", lambda e, nch=nch: e.matmul(pso[0:64, :], lhsT=vcc[kvh][0][:, nch, :], rhs=pul[nch][0][:],
                                                                     start=(nch == 0), stop=(nch == 1)),
                                   reads=[vcc[kvh][1], pul[nch][1]], writes=[pso_b])
                          for nch in range(2):
                              S.op("pe", lambda e, nch=nch: e.matmul(psd[0:64, :], lhsT=ones64[:], rhs=pul[nch][0][:],
                                                                     start=(nch == 0), stop=(nch == 1)),
                                   reads=[ones64_b, pul[nch][1]], writes=[psd_b])
                          finish_branch(hq, 0, tt, True)
                          for tsub in range(4):
                              ts = tt * 4 + tsub
                              for nch in range(2):
                                  S.op("pe", lambda e, nch=nch, tsub=tsub: e.matmul(
                                      psi[:, 0:65], lhsT=pul[nch][0][:, tsub * 128:(tsub + 1) * 128],
                                      rhs=maug[:, nch * 65:(nch + 1) * 65], start=(nch == 0), stop=(nch == 1)),
                                      reads=[pul[nch][1], maug_b], writes=[psi_b])
                              S.op("dve", lambda e: e.tensor_scalar(out=rec2[:], in0=psi[:, 64:65], scalar1=1e-30, scalar2=None,
                                                                    op0=ALU.add), reads=[psi_b], writes=[rec2_b])
                              S.op("dve", lambda e: e.reciprocal(out=rec2[:], in_=rec2[:]), reads=[rec2_b], writes=[rec2_b])
                              if g == 0:
                                  S.op("dve", lambda e, ts=ts: e.tensor_scalar(out=impacc[:, ts, :], in0=psi[:, 0:64], scalar1=rec2[:, 0:1],
                                                                               scalar2=None, op0=ALU.mult),
                                       reads=[psi_b, rec2_b], writes=[impacc_b])
                              else:
                                  S.op("dve", lambda e, ts=ts: e.scalar_tensor_tensor(
                                      out=impacc[:, ts, :], in0=psi[:, 0:64], scalar=rec2[:, 0:1], in1=impacc[:, ts, :],
                                      op0=ALU.mult, op1=ALU.add), reads=[psi_b, rec2_b, impacc_b], writes=[impacc_b])
                  for ts in range(16 if sub >= 3 else 0):
                      S.op("dve", lambda e, ts=ts: e.tensor_tensor(out=simp[:], in0=impacc[:, ts, :], in1=selbias[:, ts * 64:(ts + 1) * 64],
                                                                   op=ALU.add), reads=[impacc_b, selbias_b], writes=[simp_b])
                      S.op("dve", lambda e: e.max(out=m8a[:], in_=simp[:]), reads=[simp_b], writes=[m8a_b])
                      S.op("dve", lambda e: e.match_replace(out=simp2[:], in_to_replace=m8a[:], in_values=simp[:], imm_value=-3.0e38),
                           reads=[simp_b, m8a_b], writes=[simp2_b])
                      S.op("dve", lambda e: e.max(out=m8b[:], in_=simp2[:]), reads=[simp2_b], writes=[m8b_b])
                      for hh in range(2):
                          S.op("dve", lambda e, ts=ts, hh=hh: e.scalar_tensor_tensor(out=nsel[:, hh * 64:(hh + 1) * 64], in0=simp[:], scalar=m8b[:, 7:8],
                                                                                     in1=selinv[:, ts * 64:(ts + 1) * 64], op0=ALU.is_lt, op1=ALU.max),
                               reads=[simp_b, m8b_b, selinv_b], writes=[nsel_b])
                      S.op("pe", lambda e: e.transpose(out=psbf[:, 0:128], in_=nsel[:, :], identity=identb[:]),
                           reads=[nsel_b, identb_b], writes=[psbf_b])
                      S.op("act", lambda e, ts=ts: e.activation(out=notselT[:, ts * 128:(ts + 1) * 128], in_=psbf[:, 0:128], func=AF.Copy),
                           reads=[psbf_b], writes=[notselT_b])
                  for hh in range(2):
                      S.dma("sp", ksT2[hh * 64:(hh + 1) * 64, :], ksT_d[kvh * 64:(kvh + 1) * 64, :], [ksT_b], ksT2_b)
                      S.dma("act", kwT2[hh * 64:(hh + 1) * 64, :], kwT_d[kvh * 64:(kvh + 1) * 64, :], [kwT_b], kwT2_b)
                  if kvh == 0:
                      S.dma("sp", vs_all[:], vs_d.rearrange("(c p) d -> p c d", p=128), [vs_b], vs_all_b)
                      S.dma("act", vw_all[:], vw_d.rearrange("(c p) d -> p c d", p=128), [vw_b], vw_all_b)
                  for g in range(4 if sub >= 4 else 0):
                      hq = kvh * 4 + g
                      base = 64 * (hq % 2)
                      qc = hq // 2
                      for br in (1, 2):
                          kT, kTb = (ksT2, ksT2_b) if br == 1 else (kwT2, kwT2_b)
                          va, vab = (vs_all, vs_all_b) if br == 1 else (vw_all, vw_all_b)
                          for tt in range(4):
                              tsl = slice(tt * 512, (tt + 1) * 512)
                              sc_hi = 16 + 4 * tt + 3
                              sc_lo = 0 if br == 1 else 16 + 4 * tt - 4
                              for sc in range(sc_lo, sc_hi + 1):
                                  r = sc - (16 + 4 * tt)
                                  near = (r >= -1) if br == 1 else True
                                  pst, psb = kb.psum(3)
                                  S.op("pe", lambda e, pst=pst, sc=sc: e.matmul(
                                      pst[:, :], lhsT=kT[base:base + 64, sc * 128:(sc + 1) * 128], rhs=qT_sb[base:base + 64, qc, tsl],
                                      start=True, stop=(br == 2)), reads=[kTb, qT_sbb], writes=[psb])
                                  if br == 1:
                                      S.op("pe", lambda e, pst=pst, sc=sc: e.matmul(
                                          pst[:, :], lhsT=negE[base:base + 64, sc * 128:(sc + 1) * 128], rhs=notselT[base:base + 64, tsl],
                                          start=False, stop=True), reads=[negE_b, notselT_b], writes=[psb])
                                  pu, pub = pus[cnt["pu"] % 8]
                                  cnt["pu"] += 1
                                  if near:
                                      if br == 1:
                                          vin = Vb_in
                                      elif r >= 0:
                                          vin = Vb_in
                                      else:
                                          vin = Vwc_in if (tt == 0) else Vw_in
                                      c0 = OFFB + (2048 + 512 * tt - 128 * sc - 127)
                                      bt, bb = hankel(vin, hq, c0, 1)
                                      sf, sfb = sfp[cnt["sf"] % 4]
                                      cnt["sf"] += 1
                                      S.op("dve", lambda e, pst=pst, sf=sf, bt=bt: e.tensor_tensor(out=sf[:], in0=pst[:, :], in1=bt[:], op=ALU.add),
                                           reads=[psb, bb], writes=[sfb])
                                      S.op("act", lambda e, sf=sf, pu=pu: e.activation(out=pu[:], in_=sf[:], func=AF.Exp),
                                           reads=[sfb], writes=[pub])
                                  else:
                                      S.op("act", lambda e, pst=pst, pu=pu: e.activation(out=pu[:], in_=pst[:, :], func=AF.Exp,
                                                                                         bias=rb31[:, hq:hq + 1], scale=1.0),
                                           reads=[psb, rb31_b], writes=[pub])
                                  S.op("pe", lambda e, sc=sc, pu=pu: e.matmul(pso[0:64, :], lhsT=va[:, sc, kvh * 64:(kvh + 1) * 64], rhs=pu[:],
                                                                              start=(sc == sc_lo), stop=(sc == sc_hi)),
                                       reads=[vab, pub], writes=[pso_b])
                                  S.op("pe", lambda e, sc=sc, pu=pu: e.matmul(psd[0:64, :], lhsT=ones64[:], rhs=pu[:],
                                                                              start=(sc == sc_lo), stop=(sc == sc_hi)),
                                       reads=[ones64_b, pub], writes=[psd_b])
                              finish_branch(hq, br, tt, False)
                  S.op("act", lambda e: e.activation(out=obf[:], in_=oacc[:], func=AF.Copy), reads=[oacc_b], writes=[obf_b])
                  S.dma("sp", oT_d[kvh * 256:(kvh + 1) * 256, :].rearrange("(g p) t -> p g t", p=64), obf[:], [obf_b], oT_b)
              kb.end(ph)
          except _Stop:
              kb.end(ph)


    y2T_d, y2T_b = kb.dscratch("y2T_d", [1024, TOWN], BF16)
    TWO_PI = 2.0 * math.pi
    if stages >= 4 and 4 not in skip:
        lre_f = kb.din("lre_f", [1, 4096]); lim_f = kb.din("lim_f", [1, 4096]); lst_f = kb.din("lst_f", [1, 4096])
        lreT_in = kb.din("lreT", [128, 32]); limT_in = kb.din("limT", [128, 32]); lstT_in = kb.din("lstT", [128, 32])
        Bpre_in = kb.din("Bpad_re", [128, 4096]); Bpim_in = kb.din("Bpad_im", [128, 4096])
        Cpre_in = kb.din("Cpad_re", [128, 4096]); Cpim_in = kb.din("Cpad_im", [128, 4096])
        tau_in = kb.din("tau", [1, 512])
        s5dT_in = kb.din("s5dT", [128, 8])
        glu_w_in = kb.din("glu_w", [1024, 1024]); glubT_in = kb.din("glubT", [128, 8])
        with contextlib.ExitStack() as ph:
            Wre, Wre_b = kb.sb(ph, "Wre", [128, 4096], BF16)
            Wim, Wim_b = kb.sb(ph, "Wim", [128, 4096], BF16)
            Cre, Cre_b = kb.sb(ph, "Cre", [128, 4096], BF16, dma=True)
            Cim, Cim_b = kb.sb(ph, "Cim", [128, 4096], BF16, dma=True)
            S.dma("pool", Cre[:].rearrange("p (a b) -> p a b", b=1024), Cpre_in.rearrange("p (a b) -> p a b", b=1024), [], Cre_b)
            S.dma("pool", Cim[:].rearrange("p (a b) -> p a b", b=1024), Cpim_in.rearrange("p (a b) -> p a b", b=1024), [], Cim_b)
            tau, tau_b = kb.sb(ph, "tau_sb", [128, 512], F32, dma=True)
            S.dma("sp", tau[:], tau_in[0:1, :].partition_broadcast(128), [], tau_b)
            thT, thT_b = kb.sb(ph, "thT", [128, 32], F32)
            rT, rT_b = kb.sb(ph, "rT", [128, 32], F32)
            cQ, cQ_b = kb.sb(ph, "cQ", [128, 32], F32)
            sQ, sQ_b = kb.sb(ph, "sQ", [128, 32], F32)
            nsQ, nsQ_b = kb.sb(ph, "nsQ", [128, 32], F32)
            s5d, s5d_b = kb.sb(ph, "s5d", [128, 8], F32, dma=True)
            glub, glub_b = kb.sb(ph, "glub", [128, 8], F32, dma=True)
            S.dma("sp", s5d[:], s5dT_in[:, :], [], s5d_b)
            S.dma("sp", glub[:], glubT_in[:, :], [], glub_b)

            sc_cache = {}

            def sincos(ctx, A, Ab, N, nm):
                if nm not in sc_cache:
                    sc_cache[nm] = [kb.sb(ctx, nm + "ki", [128, N], mybir.dt.int32), kb.sb(ctx, nm + "kf", [128, N], F32),
                                    kb.sb(ctx, nm + "r", [128, N], F32), kb.sb(ctx, nm + "m", [128, N], F32),
                                    kb.sb(ctx, nm + "S", [128, N], F32), kb.sb(ctx, nm + "C", [128, N], F32)]
                (ki, ki_b), (kf, kf_b), (r, r_b), (mm, mm_b), (Sx, Sx_b), (Cx, Cx_b) = sc_cache[nm]
                S.op("dve", lambda e: e.tensor_scalar(out=ki[:], in0=A, scalar1=1.0 / TWO_PI, scalar2=None, op0=ALU.mult),
                     reads=[Ab], writes=[ki_b])
                S.op("dve", lambda e: e.tensor_copy(out=kf[:], in_=ki[:]), reads=[ki_b], writes=[kf_b])
                S.op("dve", lambda e: e.scalar_tensor_tensor(out=r[:], in0=kf[:], scalar=-TWO_PI, in1=A, op0=ALU.mult, op1=ALU.add),
                     reads=[kf_b, Ab], writes=[r_b])
                S.op("dve", lambda e: e.tensor_scalar(out=mm[:], in0=r[:], scalar1=math.pi, scalar2=-TWO_PI, op0=ALU.is_gt, op1=ALU.mult),
                     reads=[r_b], writes=[mm_b])
                S.op("dve", lambda e: e.tensor_tensor(out=r[:], in0=r[:], in1=mm[:], op=ALU.add), reads=[r_b, mm_b], writes=[r_b])
                S.op("dve", lambda e: e.tensor_scalar(out=mm[:], in0=r[:], scalar1=-math.pi, scalar2=TWO_PI, op0=ALU.is_lt, op1=ALU.mult),
                     reads=[r_b], writes=[mm_b])
                S.op("dve", lambda e: e.tensor_tensor(out=r[:], in0=r[:], in1=mm[:], op=ALU.add), reads=[r_b, mm_b], writes=[r_b])
                S.op("act", lambda e: e.activation(out=Sx[:], in_=r[:], func=AF.Sin), reads=[r_b], writes=[Sx_b])
                S.op("act", lambda e: e.activation(out=mm[:], in_=r[:], func=AF.Abs), reads=[r_b], writes=[mm_b])
                S.op("act", lambda e: e.activation(out=Cx[:], in_=mm[:], func=AF.Sin, scale=-1.0, bias=halfpi[:, 0:1]),
                     reads=[mm_b, halfpi_b], writes=[Cx_b])
                return Sx, Sx_b, Cx, Cx_b

            halfpi, halfpi_b = kb.sb(ph, "halfpi", [128, 1], F32)
            S.op("dve", lambda e: e.memset(halfpi[:], math.pi / 2), writes=[halfpi_b])

            with contextlib.ExitStack() as pp:
                lreT, lreT_b = kb.sb(pp, "lreT_sb", [128, 32], F32, dma=True)
                limT, limT_b = kb.sb(pp, "limT_sb", [128, 32], F32, dma=True)
                lstT, lstT_b = kb.sb(pp, "lstT_sb", [128, 32], F32, dma=True)
                aQ, aQ_b = kb.sb(pp, "aQ", [128, 32], F32)
                S.dma("sp", lreT[:], lreT_in[:, :], [], lreT_b)
                S.dma("sp", limT[:], limT_in[:, :], [], limT_b)
                S.dma("sp", lstT[:], lstT_in[:, :], [], lstT_b)
                S.op("act", lambda e: e.activation(out=lstT[:], in_=lstT[:], func=AF.Exp), reads=[lstT_b], writes=[lstT_b])
                S.op("dve", lambda e: e.tensor_tensor(out=thT[:], in0=limT[:], in1=lstT[:], op=ALU.mult), reads=[limT_b, lstT_b], writes=[thT_b])
                S.op("dve", lambda e: e.tensor_tensor(out=rT[:], in0=lreT[:], in1=lstT[:], op=ALU.mult), reads=[lreT_b, lstT_b], writes=[rT_b])
                S.op("act", lambda e: e.activation(out=rT[:], in_=rT[:], func=AF.Exp), reads=[rT_b], writes=[rT_b])
                S.op("dve", lambda e: e.tensor_scalar(out=aQ[:], in0=thT[:], scalar1=512.0, scalar2=None, op0=ALU.mult), reads=[thT_b], writes=[aQ_b])
                Sx, Sx_b, Cx, Cx_b = sincos(pp, aQ[:], aQ_b, 32, "q_")
                S.op("dve", lambda e: e.tensor_copy(out=sQ[:], in_=Sx[:]), reads=[Sx_b], writes=[sQ_b])
                S.op("dve", lambda e: e.tensor_copy(out=cQ[:], in_=Cx[:]), reads=[Cx_b], writes=[cQ_b])
                S.op("dve", lambda e: e.tensor_scalar(out=nsQ[:], in0=Sx[:], scalar1=-1.0, scalar2=None, op0=ALU.mult), reads=[Sx_b], writes=[nsQ_b])
                kb.end(pp)

            for pg in range(4):
                with contextlib.ExitStack() as pp:
                    NN = 1024
                    csl = slice(pg * NN, (pg + 1) * NN)
                    def T(nm, dma=False):
                        return kb.sb(pp, f"{nm}{pg}", [128, NN], F32, dma=dma)
                    lre, lre_b = T("lre", True); lim, lim_b = T("lim", True); st, st_b = T("st", True)
                    S.dma("sp", lre[:], lre_f[0:1, csl].partition_broadcast(128), [], lre_b)
                    S.dma("sp", lim[:], lim_f[0:1, csl].partition_broadcast(128), [], lim_b)
                    S.dma("sp", st[:], lst_f[0:1, csl].partition_broadcast(128), [], st_b)
                    bre, bre_b = T("bpre", True); bim, bim_b = T("bpim", True)
                    S.dma("act", bre[:], Bpre_in[:, csl], [], bre_b)
                    S.dma("act", bim[:], Bpim_in[:, csl], [], bim_b)
                    ar, ar_b = T("ar"); ai, ai_b = T("ai"); mag, mag_b = T("mag")
                    S.op("act", lambda e: e.activation(out=st[:], in_=st[:], func=AF.Exp), reads=[st_b], writes=[st_b])
                    S.op("dve", lambda e: e.tensor_tensor(out=ar[:], in0=lre[:], in1=st[:], op=ALU.mult), reads=[lre_b, st_b], writes=[ar_b])
                    S.op("dve", lambda e: e.tensor_tensor(out=ai[:], in0=lim[:], in1=st[:], op=ALU.mult), reads=[lim_b, st_b], writes=[ai_b])
                    S.op("act", lambda e: e.activation(out=mag[:], in_=ar[:], func=AF.Exp), reads=[ar_b], writes=[mag_b])
                    Sx, Sx_b, Cx, Cx_b = sincos(pp, ai[:], ai_b, NN, f"p{pg}_")
                    S.op("dve", lambda e: e.tensor_tensor(out=Cx[:], in0=Cx[:], in1=mag[:], op=ALU.mult), reads=[Cx_b, mag_b], writes=[Cx_b])
                    S.op("dve", lambda e: e.tensor_tensor(out=Sx[:], in0=Sx[:], in1=mag[:], op=ALU.mult), reads=[Sx_b, mag_b], writes=[Sx_b])
                    S.op("dve", lambda e: e.tensor_scalar(out=Cx[:], in0=Cx[:], scalar1=-1.0, scalar2=None, op0=ALU.add), reads=[Cx_b], writes=[Cx_b])
                    S.op("dve", lambda e: e.tensor_tensor(out=mag[:], in0=lre[:], in1=lre[:], op=ALU.mult), reads=[lre_b], writes=[mag_b])
                    S.op("dve", lambda e: e.tensor_tensor(out=ar[:], in0=lim[:], in1=lim[:], op=ALU.mult), reads=[lim_b], writes=[ar_b])
                    S.op("dve", lambda e: e.tensor_tensor(out=mag[:], in0=mag[:], in1=ar[:], op=ALU.add), reads=[mag_b, ar_b], writes=[mag_b])
                    S.op("dve", lambda e: e.reciprocal(out=mag[:], in_=mag[:]), reads=[mag_b], writes=[mag_b])
                    S.op("dve", lambda e: e.tensor_tensor(out=ar[:], in0=Cx[:], in1=lre[:], op=ALU.mult), reads=[Cx_b, lre_b], writes=[ar_b])
                    S.op("dve", lambda e: e.tensor_tensor(out=st[:], in0=Sx[:], in1=lim[:], op=ALU.mult), reads=[Sx_b, lim_b], writes=[st_b])
                    S.op("dve", lambda e: e.tensor_tensor(out=ar[:], in0=ar[:], in1=st[:], op=ALU.add), reads=[ar_b, st_b], writes=[ar_b])
                    S.op("dve", lambda e: e.tensor_tensor(out=ar[:], in0=ar[:], in1=mag[:], op=ALU.mult), reads=[ar_b, mag_b], writes=[ar_b])
                    S.op("dve", lambda e: e.tensor_tensor(out=ai[:], in0=Sx[:], in1=lre[:], op=ALU.mult), reads=[Sx_b, lre_b], writes=[ai_b])
                    S.op("dve", lambda e: e.tensor_tensor(out=st[:], in0=Cx[:], in1=lim[:], op=ALU.mult), reads=[Cx_b, lim_b], writes=[st_b])
                    S.op("dve", lambda e: e.tensor_tensor(out=ai[:], in0=ai[:], in1=st[:], op=ALU.subtract), reads=[ai_b, st_b], writes=[ai_b])
                    S.op("dve", lambda e: e.tensor_tensor(out=ai[:], in0=ai[:], in1=mag[:], op=ALU.mult), reads=[ai_b, mag_b], writes=[ai_b])
                    S.op("dve", lambda e: e.tensor_tensor(out=st[:], in0=ar[:], in1=bre[:], op=ALU.mult), reads=[ar_b, bre_b], writes=[st_b])
                    S.op("dve", lambda e: e.tensor_tensor(out=mag[:], in0=ai[:], in1=bim[:], op=ALU.mult), reads=[ai_b, bim_b], writes=[mag_b])
                    S.op("dve", lambda e: e.tensor_tensor(out=Wre[:, csl], in0=st[:], in1=mag[:], op=ALU.subtract), reads=[st_b, mag_b], writes=[Wre_b])
                    S.op("dve", lambda e: e.tensor_tensor(out=st[:], in0=ar[:], in1=bim[:], op=ALU.mult), reads=[ar_b, bim_b], writes=[st_b])
                    S.op("dve", lambda e: e.tensor_tensor(out=mag[:], in0=ai[:], in1=bre[:], op=ALU.mult), reads=[ai_b, bre_b], writes=[mag_b])
                    S.op("dve", lambda e: e.tensor_tensor(out=Wim[:, csl], in0=st[:], in1=mag[:], op=ALU.add), reads=[st_b, mag_b], writes=[Wim_b])
                    kb.end(pp)

            ang, ang_b = kb.sb(ph, "ang", [128, 512], F32)
            rt, rt_b = kb.sb(ph, "rt", [128, 512], F32)
            ones512, ones512_b = kb.sb(ph, "ones512", [128, 512], F32)
            S.op("dve", lambda e: e.memset(ones512[:], 1.0), writes=[ones512_b])
            uTs = [kb.sb(ph, f"uTs{i}", [128, TALL], BF16, dma=True) for i in range(2)]
            xre, xre_b = kb.sb(ph, "xre", [128, 4, TOWN], BF16)
            nxim, nxim_b = kb.sb(ph, "nxim", [128, 4, TOWN], BF16)
            yg, yg_b = kb.sb(ph, "yg", [128, 8, TOWN], BF16)
            wk2 = [{n: kb.sb(ph, f"wk{i}_" + n, [128, 512], F32) for n in ("t1", "t2", "t3", "t4", "bre", "bim", "o1", "o2", "o3", "o4", "ysb")} for i in range(2)]
            wk = wk2[0]
            wre2 = [kb.sb(ph, f"wre{i}", [128, 512], F32) for i in range(2)]
            wim2 = [kb.sb(ph, f"wim{i}", [128, 512], F32) for i in range(2)]
            ini_re, ini_re_b = kb.sb(ph, "ini_re", [128, 1], F32)
            ini_im, ini_im_b = kb.sb(ph, "ini_im", [128, 1], F32)
            itmp, itmp_b = kb.sb(ph, "itmp", [128, 2], F32)
            with contextlib.ExitStack() as sc_ctx:
                for cc in range(8):
                    uT, uT_sbb = uTs[cc % 2]
                    S.dma("sp", uT[:], uT_d[cc * 128:(cc + 1) * 128, :], [uT_b], uT_sbb)
                    for pl in range(4):
                        pr = cc * 4 + pl
                        if True:
                            pctx = ph
                            S.op("dve", lambda e, pr=pr: e.tensor_scalar(out=ang[:], in0=tau[:], scalar1=thT[:, pr:pr + 1], scalar2=None, op0=ALU.mult),
                                 reads=[tau_b, thT_b], writes=[ang_b])
                            Sx, Sx_b, Cx, Cx_b = sincos(pctx, ang[:], ang_b, 512, "tmain_")
                            S.op("dve", lambda e, pr=pr: e.tensor_scalar(out=rt[:], in0=ones512[:], scalar1=rT[:, pr:pr + 1], scalar2=None, op0=ALU.mult),
                                 reads=[ones512_b, rT_b], writes=[rt_b])
                            for j in range(8):
                                ps_re, ps_re_b = kb.psum()
                                ps_im, ps_im_b = kb.psum()
                                S.op("pe", lambda e, ps_re=ps_re, j=j, pr=pr: e.matmul(
                                    ps_re[:, :], lhsT=Wre[:, pr * 128:(pr + 1) * 128], rhs=uT[:, j * 512:(j + 1) * 512], start=True, stop=True),
                                    reads=[Wre_b, uT_sbb], writes=[ps_re_b])
                                S.op("pe", lambda e, ps_im=ps_im, j=j, pr=pr: e.matmul(
                                    ps_im[:, :], lhsT=Wim[:, pr * 128:(pr + 1) * 128], rhs=uT[:, j * 512:(j + 1) * 512], start=True, stop=True),
                                    reads=[Wim_b, uT_sbb], writes=[ps_im_b])
                                wkj = wk2[j % 2]
                                t1, t1b = wkj["t1"]; t2, t2b = wkj["t2"]; t3, t3b = wkj["t3"]; t4, t4b = wkj["t4"]
                                bre_, breb = wkj["bre"]; bim_, bimb = wkj["bim"]
                                S.op("dve", lambda e, ps_re=ps_re: e.tensor_tensor(out=t1[:], in0=ps_re[:, :], in1=Cx[:], op=ALU.mult), reads=[ps_re_b, Cx_b], writes=[t1b])
                                S.op("dve", lambda e, ps_im=ps_im: e.tensor_tensor(out=t2[:], in0=ps_im[:, :], in1=Sx[:], op=ALU.mult), reads=[ps_im_b, Sx_b], writes=[t2b])
                                S.op("pool", lambda e: e.tensor_tensor(out=bre_[:], in0=t1[:], in1=t2[:], op=ALU.add), reads=[t1b, t2b], writes=[breb])
                                S.op("dve", lambda e, ps_im=ps_im: e.tensor_tensor(out=t3[:], in0=ps_im[:, :], in1=Cx[:], op=ALU.mult), reads=[ps_im_b, Cx_b], writes=[t3b])
                                S.op("dve", lambda e, ps_re=ps_re: e.tensor_tensor(out=t4[:], in0=ps_re[:, :], in1=Sx[:], op=ALU.mult), reads=[ps_re_b, Sx_b], writes=[t4b])
                                S.op("pool", lambda e: e.tensor_tensor(out=bim_[:], in0=t3[:], in1=t4[:], op=ALU.subtract), reads=[t3b, t4b], writes=[bimb])
                                wre, wreb = wre2[j % 2]
                                wim, wimb = wim2[j % 2]
                                if j == 0:
                                    S.op("dve", lambda e, wre=wre: e.tensor_tensor_scan(out=wre[:], data0=rt[:], data1=bre_[:], initial=0.0, op0=ALU.mult, op1=ALU.add),
                                         reads=[rt_b, breb], writes=[wreb])
                                    S.op("dve", lambda e, wim=wim: e.tensor_tensor_scan(out=wim[:], data0=rt[:], data1=bim_[:], initial=0.0, op0=ALU.mult, op1=ALU.add),
                                         reads=[rt_b, bimb], writes=[wimb])
                                else:
                                    S.op("dve", lambda e, wre=wre: e.tensor_tensor_scan(out=wre[:], data0=rt[:], data1=bre_[:], initial=ini_re[:, 0:1], op0=ALU.mult, op1=ALU.add),
                                         reads=[rt_b, breb, ini_re_b], writes=[wreb])
                                    S.op("dve", lambda e, wim=wim: e.tensor_tensor_scan(out=wim[:], data0=rt[:], data1=bim_[:], initial=ini_im[:, 0:1], op0=ALU.mult, op1=ALU.add),
                                         reads=[rt_b, bimb, ini_im_b], writes=[wimb])
                                if j < 7:
                                    S.op("dve", lambda e, wre=wre, pr=pr: e.tensor_scalar(out=itmp[:, 0:1], in0=wre[:, 511:512], scalar1=cQ[:, pr:pr + 1], scalar2=None, op0=ALU.mult),
                                         reads=[wreb, cQ_b], writes=[itmp_b])
                                    S.op("dve", lambda e, wre=wre, pr=pr: e.tensor_scalar(out=itmp[:, 1:2], in0=wre[:, 511:512], scalar1=sQ[:, pr:pr + 1], scalar2=None, op0=ALU.mult),
                                         reads=[wreb, sQ_b], writes=[itmp_b])
                                    S.op("dve", lambda e, wim=wim, pr=pr: e.scalar_tensor_tensor(out=ini_re[:], in0=wim[:, 511:512], scalar=nsQ[:, pr:pr + 1], in1=itmp[:, 0:1], op0=ALU.mult, op1=ALU.add),
                                         reads=[wimb, nsQ_b, itmp_b], writes=[ini_re_b])
                                    S.op("dve", lambda e, wim=wim, pr=pr: e.scalar_tensor_tensor(out=ini_im[:], in0=wim[:, 511:512], scalar=cQ[:, pr:pr + 1], in1=itmp[:, 1:2], op0=ALU.mult, op1=ALU.add),
                                         reads=[wimb, cQ_b, itmp_b], writes=[ini_im_b])
                                if j >= 4:
                                    tsl = slice((j - 4) * 512, (j - 3) * 512)
                                    o1, o1b = wkj["o1"]; o2, o2b = wkj["o2"]; o3, o3b = wkj["o3"]; o4, o4b = wkj["o4"]
                                    S.op("dve", lambda e, wre=wre: e.tensor_tensor(out=o1[:], in0=wre[:], in1=Cx[:], op=ALU.mult), reads=[wreb, Cx_b], writes=[o1b])
                                    S.op("pool", lambda e, wim=wim: e.tensor_tensor(out=o2[:], in0=wim[:], in1=Sx[:], op=ALU.mult), reads=[wimb, Sx_b], writes=[o2b])
                                    S.op("pool", lambda e, tsl=tsl, pl=pl: e.tensor_tensor(out=xre[:, pl, tsl], in0=o1[:], in1=o2[:], op=ALU.subtract), reads=[o1b, o2b], writes=[xre_b])
                                    S.op("dve", lambda e, wre=wre: e.tensor_tensor(out=o3[:], in0=wre[:], in1=Sx[:], op=ALU.mult), reads=[wreb, Sx_b], writes=[o3b])
                                    S.op("pool", lambda e, wim=wim: e.tensor_tensor(out=o4[:], in0=wim[:], in1=Cx[:], op=ALU.mult), reads=[wimb, Cx_b], writes=[o4b])
                                    S.op("dve", lambda e, tsl=tsl, pl=pl: e.scalar_tensor_tensor(out=nxim[:, pl, tsl], in0=o3[:], scalar=-1.0, in1=o4[:], op0=ALU.mult, op1=ALU.subtract),
                                         reads=[o3b, o4b], writes=[nxim_b])
                    for tt in range(4):
                        tsl = slice(tt * 512, (tt + 1) * 512)
                        pst, psb = kb.psum()
                        for pl in range(4):
                            pr = cc * 4 + pl
                            S.op("pe", lambda e, pl=pl, pr=pr, pst=pst, tsl=tsl: e.matmul(pst[:, :], lhsT=Cre[:, pr * 128:(pr + 1) * 128], rhs=xre[:, pl, tsl],
                                                                                         start=(pl == 0), stop=False), reads=[Cre_b, xre_b], writes=[psb])
                            S.op("pe", lambda e, pl=pl, pr=pr, pst=pst, tsl=tsl: e.matmul(pst[:, :], lhsT=Cim[:, pr * 128:(pr + 1) * 128], rhs=nxim[:, pl, tsl],
                                                                                         start=False, stop=(pl == 3)), reads=[Cim_b, nxim_b], writes=[psb])
                        ysb, ysbb = wk["ysb"]
                        S.op("dve", lambda e, pst=pst, tt=tt, cc=cc, uT=uT: e.scalar_tensor_tensor(
                            out=ysb[:], in0=uT[:, TALL - TOWN + tt * 512:TALL - TOWN + (tt + 1) * 512], scalar=s5d[:, cc:cc + 1], in1=pst[:, :],
                            op0=ALU.mult, op1=ALU.add), reads=[uT_sbb, s5d_b, psb], writes=[ysbb])
                        S.op("act", lambda e, cc=cc, tsl=tsl: e.activation(out=yg[:, cc, tsl], in_=ysb[:], func=AF.Gelu_apprx_tanh),
                             reads=[ysbb], writes=[yg_b])
            S.barrier()
            with contextlib.ExitStack() as pg_:
                gw, gw_b = kb.sb(pg_, "gw", [128, 8, 1024], BF16, dma=True)
                S.dma("pool", gw[:], glu_w_in.rearrange("(c p) o -> p c o", p=128), [], gw_b)
                sg = [kb.sb(pg_, f"sg{i}", [128, 512], BF16) for i in range(2)]
                y2 = [kb.sb(pg_, f"y2_{i}", [128, 512], BF16) for i in range(2)]
                n = 0
                for co in range(8):
                    for tt in range(4):
                        tsl = slice(tt * 512, (tt + 1) * 512)
                        pst, psb = kb.psum()
                        for cc in range(8):
                            S.op("pe", lambda e, cc=cc, co=co, pst=pst, tsl=tsl: e.matmul(pst[:, :], lhsT=gw[:, cc, co * 128:(co + 1) * 128], rhs=yg[:, cc, tsl],
                                                                                         start=(cc == 0), stop=(cc == 7)), reads=[gw_b, yg_b], writes=[psb])
                        sgt, sgb = sg[n % 2]
                        y2t, y2b = y2[n % 2]
                        n += 1
                        S.op("act", lambda e, pst=pst, sgt=sgt, co=co: e.activation(out=sgt[:], in_=pst[:, :], func=AF.Sigmoid, bias=glub[:, co:co + 1], scale=1.0),
                             reads=[psb, glub_b], writes=[sgb])
                        S.op("dve", lambda e, sgt=sgt, y2t=y2t, co=co, tsl=tsl: e.tensor_tensor(out=y2t[:], in0=sgt[:], in1=yg[:, co, tsl], op=ALU.mult),
                             reads=[sgb, yg_b], writes=[y2b])
                        S.dma("sp", y2T_d[co * 128:(co + 1) * 128, tsl], y2t[:], [y2b], y2T_b)
                kb.end(pg_)
            kb.end(ph)


    x1T_d, x1T_b = kb.dscratch("x1T_d", [D, TOWN], F32)
    x2T_d, x2T_b = kb.dscratch("x2T_d", [D, TOWN], F32)
    if stages >= 5 and 5 not in skip:
        wua_in = kb.din("w_up_attn", [1024, D]); wus_in = kb.din("w_up_ssm", [1024, D]); wout_in = kb.din("w_out", [D, D])
        with contextlib.ExitStack() as ph:
            wua, wua_b = kb.sb(ph, "wua", [128, 8, D], BF16, dma=True)
            wus, wus_b = kb.sb(ph, "wus", [128, 8, D], BF16, dma=True)
            for c4 in range(4):
                S.dma("pool", wua[:, :, c4 * 512:(c4 + 1) * 512], wua_in.rearrange("(c p) o -> p c o", p=128)[:, :, c4 * 512:(c4 + 1) * 512], [], wua_b)
                S.dma("pool", wus[:, :, c4 * 512:(c4 + 1) * 512], wus_in.rearrange("(c p) o -> p c o", p=128)[:, :, c4 * 512:(c4 + 1) * 512], [], wus_b)
            wo2 = [kb.sb(ph, f"wo{i}", [128, KC, 128], BF16, dma=True) for i in range(2)]
            gts, gts_b = kb.sb(ph, "gts", [128, 32, 512], BF16, dma=True)
            oTt, oTt_b = kb.sb(ph, "oTt", [128, 8, 512], BF16, dma=True)
            yTt, yTt_b = kb.sb(ph, "yTt", [128, 8, 512], BF16, dma=True)
            mixed, mixed_b = kb.sb(ph, "mixed", [128, KC, 512], BF16)
            xt2 = [kb.sb(ph, f"xres{i}", [128, 512], F32, dma=True) for i in range(2)]
            x1t2 = [kb.sb(ph, f"x1t{i}", [128, 512], F32) for i in range(2)]
            mt1 = [kb.sb(ph, f"mt1_{i}", [128, 512], F32) for i in range(2)]
            mt2 = [kb.sb(ph, f"mt2_{i}", [128, 512], F32) for i in range(2)]
            woutv = wout_in.rearrange("(c p) o -> p c o", p=128)
            xTv_own = xT.rearrange("(k p) t -> p k t", p=128)
            n = 0
            for tt in range(4):
                tsl = slice(tt * 512, (tt + 1) * 512)
                S.dma("sp", gts[:], mgT_d.rearrange("(c p) t -> p c t", p=128)[:, :, tsl], [mgT_b], gts_b)
                S.dma("sp", oTt[:], oT_d.rearrange("(c p) t -> p c t", p=128)[:, :, tsl], [oT_b], oTt_b)
                S.dma("act", yTt[:], y2T_d.rearrange("(c p) t -> p c t", p=128)[:, :, tsl], [y2T_b], yTt_b)
                for dm in range(KC):
                    psA, psA_b = kb.psum()
                    psB, psB_b = kb.psum()
                    for cc in range(8):
                        S.op("pe", lambda e, cc=cc, dm=dm, psA=psA: e.matmul(psA[:, :], lhsT=wua[:, cc, dm * 128:(dm + 1) * 128], rhs=oTt[:, cc, :],
                                                                           start=(cc == 0), stop=(cc == 7)), reads=[wua_b, oTt_b], writes=[psA_b])
                    for cc in range(8):
                        S.op("pe", lambda e, cc=cc, dm=dm, psB=psB: e.matmul(psB[:, :], lhsT=wus[:, cc, dm * 128:(dm + 1) * 128], rhs=yTt[:, cc, :],
                                                                           start=(cc == 0), stop=(cc == 7)), reads=[wus_b, yTt_b], writes=[psB_b])
                    a1, a1b = mt1[dm % 2]
                    a2, a2b = mt2[dm % 2]
                    S.op("dve", lambda e, psA=psA, a1=a1, dm=dm: e.tensor_tensor(out=a1[:], in0=psA[:, :], in1=gts[:, dm, :], op=ALU.mult),
                         reads=[psA_b, gts_b], writes=[a1b])
                    S.op("dve", lambda e, psB=psB, a2=a2, dm=dm: e.tensor_tensor(out=a2[:], in0=psB[:, :], in1=gts[:, 16 + dm, :], op=ALU.mult),
                         reads=[psB_b, gts_b], writes=[a2b])
                    S.op("pool", lambda e, a1=a1, a2=a2, dm=dm: e.tensor_tensor(out=mixed[:, dm, :], in0=a1[:], in1=a2[:], op=ALU.add),
                         reads=[a1b, a2b], writes=[mixed_b])
                for do in range(KC):
                    wo, wo_b = wo2[n % 2]
                    xr, xr_b = xt2[n % 2]
                    x1t, x1t_b = x1t2[n % 2]
                    n += 1
                    S.dma("pool", wo[:], woutv[:, :, do * 128:(do + 1) * 128], [], wo_b)
                    S.dma("act", xr[:], xTv_own[:, do, TALL - TOWN + tt * 512:TALL - TOWN + (tt + 1) * 512], [], xr_b)
                    pst, psb = kb.psum()
                    for dm in range(KC):
                        S.op("pe", lambda e, dm=dm, pst=pst, wo=wo: e.matmul(pst[:, :], lhsT=wo[:, dm, :], rhs=mixed[:, dm, :],
                                                                           start=(dm == 0), stop=(dm == KC - 1)), reads=[wo_b, mixed_b], writes=[psb])
                    S.op("dve", lambda e, pst=pst, do=do, xr=xr, x1t=x1t: e.scalar_tensor_tensor(
                        out=x1t[:], in0=pst[:, :], scalar=mod[:, G1 + do:G1 + do + 1], in1=xr[:], op0=ALU.mult, op1=ALU.add),
                        reads=[psb, mod_b, xr_b], writes=[x1t_b])
                    S.dma("sp", x1T_d[do * 128:(do + 1) * 128, tsl], x1t[:], [x1t_b], x1T_b)
            kb.end(ph)

    x2src_d, x2src_b = (x1T_d, x1T_b)
    if stages >= 6 and 6 not in skip:
        x2src_d, x2src_b = (x2T_d, x2T_b)
        wq_in = kb.din("peer_w_q", [D, D])
        keysT_in = kb.din("keysT", [128, 16 * 128])
        uT_in = kb.din("peer_uT", [D, 16384])
        v_in = kb.din("peer_v", [16384, D])
        h2T_d, h2T_b = kb.dscratch("h2T_d", [D, TOWN], BF16)
        qpT_d, qpT_b = kb.dscratch("qpT_d", [D, TOWN], BF16)
        G_d, G_b = kb.dscratch("G_d", [16, 128, 16384], BF16)
        uTb_d, uTb_b = kb.dscratch("uTb_d", [32, 128, KC * 512], BF16)
        vb_d, vb_b = kb.dscratch("vb_d", [32, 128, 4 * D], BF16)
        with contextlib.ExitStack() as ph:
            TB = 1024
            hT, hT_b = kb.sb(ph, "p_hT", [128, KC, TB], BF16)
            xbufs = [kb.sb(ph, f"p_xb{i}", [128, KC, 256], F32, dma=True) for i in range(2)]
            tmpbufs = [kb.sb(ph, f"p_ntmp{i}", [128, 256], F32) for i in range(2)]
            sqbuf = kb.sb(ph, "p_sqb", [128, KC, 256], BF16)
            rbuf = kb.sb(ph, "p_rstd", [128, 256], F32)
            wbufs = [kb.sb(ph, f"p_wbuf{i}", [128, KC, 512], BF16, dma=True) for i in range(2)]
            obufs = [kb.sb(ph, f"p_obuf{i}", [128, 512], BF16) for i in range(4)]
            x1v = x1T_d.rearrange("(k p) t -> p k t", p=128)
            wqv = wq_in.rearrange("(k p) c -> p k c", p=128)
            n = 0
            for blk in range(TOWN // TB):
                tb0 = blk * TB

                def src_fn(t, nn):
                    return x1v[:, :, t:t + nn]
                norm_block(ph, src_fn, tb0, TB, hT, hT_b, gm2, gm2_b, SH2, xbufs, tmpbufs, sqbuf, rbuf)
                S.dma("act", h2T_d.rearrange("(k p) t -> p k t", p=128)[:, :, tb0:tb0 + TB], hT[:], [hT_b], h2T_b)
                for cg in range(4):
                    wt, wb = wbufs[cg % 2]
                    S.dma("pool", wt[:], wqv[:, :, cg * 512:(cg + 1) * 512], [], wb)
                    for cc in range(4):
                        for tt in range(TB // 512):
                            pst, psb = kb.psum()
                            for k in range(KC):
                                S.op("pe", lambda e, k=k, cc=cc, tt=tt, pst=pst, wt=wt: e.matmul(
                                    pst[:, :], lhsT=wt[:, k, cc * 128:(cc + 1) * 128], rhs=hT[:, k, tt * 512:(tt + 1) * 512],
                                    start=(k == 0), stop=(k == KC - 1)), reads=[wb, hT_b], writes=[psb])
                            ot, ob = obufs[n % 4]
                            n += 1
                            S.op("act", lambda e, pst=pst, ot=ot: e.activation(out=ot[:], in_=pst[:, :], func=AF.Copy), reads=[psb], writes=[ob])
                            r0 = cg * 512 + cc * 128
                            S.dma("sp", qpT_d[r0:r0 + 128, tb0 + tt * 512:tb0 + (tt + 1) * 512], ot[:], [ob], qpT_b)
            kb.end(ph)
        with contextlib.ExitStack() as ph:
            keysT, keysT_b = kb.sb(ph, "keysT_sb", [128, 16, 128], BF16, dma=True)
            S.dma("pool", keysT[:], keysT_in.rearrange("p (a n) -> p a n", n=128), [], keysT_b)
            qts = [kb.sb(ph, f"qts{i}", [128, 16, 128], BF16, dma=True) for i in range(2)]
            s_all, s_all_b = kb.sb(ph, "s_all", [128, 16, 128], F32)
            v16, v16_b = kb.sb(ph, "v16", [128, 16, 16], F32)
            mtmp, mtmp_b = kb.sb(ph, "mtmp", [128, 128], F32)
            cand, cand_b = kb.sb(ph, "cand", [128, 256], F32)
            ctmp, ctmp_b = kb.sb(ph, "ctmp", [128, 256], F32)
            e256, e256_b = kb.sb(ph, "e256", [128, 256], F32)
            c1, c1_b = kb.sb(ph, "c1", [128, 8], F32)
            c2, c2_b = kb.sb(ph, "c2", [128, 8], F32)
            sm, sm_b = kb.sb(ph, "smalls", [128, 8], F32)
            Gs = [kb.sb(ph, f"Gs{i}", [128, 16384], BF16) for i in range(2)]
            Sd2 = [kb.sb(ph, f"Sd{i}", [128, 16, 128], F32) for i in range(2)]
            Ed2 = [kb.sb(ph, f"Ed{i}", [128, 16, 128], BF16) for i in range(2)]
            Md2 = [kb.sb(ph, f"Md{i}", [128, 16, 128], BF16) for i in range(2)]
            thr8, thr8_b = kb.sb(ph, "thr8", [128, 8], F32)
            nb8, nb8_b = kb.sb(ph, "nb8", [128, 8], F32)
            identg, identg_b = kb.sb(ph, "identg", [128, 128], BF16, dma=True)
            ident_src2 = kb.inputs["ident"] if "ident" in kb.inputs else kb.din("ident", [128, 128])
            S.dma("pool", identg[:], ident_src2[:, :], [], identg_b)
            qpv = qpT_d.rearrange("(a p) t -> p a t", p=128)
            nn = 0
            for ts in range(16):
                qt, qtb = qts[ts % 2]
                S.dma("sp", qt[:], qpv[:, :, ts * 128:(ts + 1) * 128], [qpT_b], qtb)
                for b4 in range(4):
                    pst, psb = kb.psum()
                    for a in range(4):
                        hc = b4 * 4 + a
                        S.op("pe", lambda e, hc=hc, a=a, pst=pst, qt=qt: e.matmul(pst[:, a * 128:(a + 1) * 128], lhsT=qt[:, hc, :], rhs=keysT[:, hc, :],
                                                                                 start=True, stop=True), reads=[qtb, keysT_b], writes=[psb])
                    S.op("act", lambda e, b4=b4, pst=pst: e.activation(out=s_all[:, b4 * 4:(b4 + 1) * 4, :].rearrange("p a n -> p (a n)"), in_=pst[:, :], func=AF.Copy),
                         reads=[psb], writes=[s_all_b])
                for hc in range(16):
                    S.op("dve", lambda e, hc=hc: e.max(out=v16[:, hc, 0:8], in_=s_all[:, hc, :]), reads=[s_all_b], writes=[v16_b])
                    S.op("dve", lambda e, hc=hc: e.match_replace(out=mtmp[:], in_to_replace=v16[:, hc, 0:8], in_values=s_all[:, hc, :], imm_value=-1.0e30),
                         reads=[s_all_b, v16_b], writes=[mtmp_b])
                    S.op("dve", lambda e, hc=hc: e.max(out=v16[:, hc, 8:16], in_=mtmp[:]), reads=[mtmp_b], writes=[v16_b])
                G, G_sb = Gs[ts % 2]
                for h in range(8):
                    S.op("dve", lambda e, h=h: e.tensor_tensor(
                        out=cand[:].rearrange("p (a b) -> p a b", a=16),
                        in0=v16[:, 2 * h, :].unsqueeze(2).to_broadcast([128, 16, 16]),
                        in1=v16[:, 2 * h + 1, :].unsqueeze(1).to_broadcast([128, 16, 16]), op=ALU.add),
                        reads=[v16_b], writes=[cand_b])
                    S.op("dve", lambda e: e.max(out=c1[:], in_=cand[:]), reads=[cand_b], writes=[c1_b])
                    S.op("dve", lambda e: e.match_replace(out=ctmp[:], in_to_replace=c1[:], in_values=cand[:], imm_value=-1.0e30),
                         reads=[cand_b, c1_b], writes=[ctmp_b])
                    S.op("dve", lambda e: e.max(out=c2[:], in_=ctmp[:]), reads=[ctmp_b], writes=[c2_b])
                    S.op("dve", lambda e, h=h: e.tensor_copy(out=thr8[:, h:h + 1], in_=c2[:, 7:8]), reads=[c2_b], writes=[thr8_b])
                    S.op("dve", lambda e: e.tensor_scalar(out=sm[:, 0:1], in0=c1[:, 0:1], scalar1=-1.0, scalar2=None, op0=ALU.mult), reads=[c1_b], writes=[sm_b])
                    S.op("act", lambda e: e.activation(out=e256[:], in_=cand[:], func=AF.Exp, bias=sm[:, 0:1], scale=1.0), reads=[cand_b, sm_b], writes=[e256_b])
                    S.op("dve", lambda e: e.scalar_tensor_tensor(out=ctmp[:], in0=cand[:], scalar=c2[:, 7:8], in1=e256[:], op0=ALU.is_ge, op1=ALU.mult),
                         reads=[cand_b, c2_b, e256_b], writes=[ctmp_b])
                    S.op("dve", lambda e: e.tensor_reduce(out=sm[:, 1:2], in_=ctmp[:], axis=AX.X, op=ALU.add), reads=[ctmp_b], writes=[sm_b])
                    S.op("act", lambda e: e.activation(out=sm[:, 2:3], in_=sm[:, 1:2], func=AF.Ln), reads=[sm_b], writes=[sm_b])
                    S.op("dve", lambda e, h=h: e.tensor_tensor(out=nb8[:, h:h + 1], in0=sm[:, 0:1], in1=sm[:, 2:3], op=ALU.subtract), reads=[sm_b], writes=[nb8_b])
                for ib in range(8):
                    for h in range(8):
                        Sd, Sd_b = Sd2[nn % 2]
                        Ed, Ed_b = Ed2[nn % 2]
                        Md, Md_b = Md2[nn % 2]
                        nn += 1
                        S.op("pool", lambda e, h=h, ib=ib, Sd=Sd: e.tensor_tensor(
                            out=Sd[:], in0=s_all[:, 2 * h, ib * 16:(ib + 1) * 16].unsqueeze(2).to_broadcast([128, 16, 128]),
                            in1=s_all[:, 2 * h + 1, :].unsqueeze(1).to_broadcast([128, 16, 128]), op=ALU.add),
                            reads=[s_all_b], writes=[Sd_b])
                        S.op("act", lambda e, Sd=Sd, Ed=Ed, h=h: e.activation(out=Ed[:], in_=Sd[:], func=AF.Exp, bias=nb8[:, h:h + 1], scale=1.0),
                             reads=[Sd_b, nb8_b], writes=[Ed_b])
                        S.op("dve", lambda e, Sd=Sd, Ed=Ed, Md=Md, h=h: e.scalar_tensor_tensor(
                            out=Md[:].rearrange("p a b -> p (a b)"), in0=Sd[:].rearrange("p a b -> p (a b)"), scalar=thr8[:, h:h + 1],
                            in1=Ed[:].rearrange("p a b -> p (a b)"), op0=ALU.is_ge, op1=ALU.mult), reads=[Sd_b, Ed_b, thr8_b], writes=[Md_b])
                        for q4 in range(4):
                            S.op("pe", lambda e, q4=q4, Md=Md, h=h: e.matmul(kb.ps[q4][0][:, :], lhsT=identg[:], rhs=Md[:].rearrange("p a b -> p (a b)")[:, q4 * 512:(q4 + 1) * 512],
                                                                            start=(h == 0), stop=(h == 7)), reads=[identg_b, Md_b], writes=[kb.ps[q4][1]])
                    for q4 in range(4):
                        S.op("act", lambda e, q4=q4, ib=ib, G=G: e.activation(out=G[:, ib * 2048 + q4 * 512:ib * 2048 + (q4 + 1) * 512], in_=kb.ps[q4][0][:, :], func=AF.Copy),
                             reads=[kb.ps[q4][1]], writes=[G_sb])
                S.dma("act", G_d[ts], G[:], [G_sb], G_b)
            kb.end(ph)
        with contextlib.ExitStack() as ph:
            identp, identp_b = kb.sb(ph, "identp", [128, 128], BF16, dma=True)
            ident_src = kb.inputs["ident"] if "ident" in kb.inputs else kb.din("ident", [128, 128])
            S.dma("pool", identp[:], ident_src[:, :], [], identp_b)
            h2t, h2t_b = kb.sb(ph, "h2t", [128, KC, 512], BF16, dma=True)
            uts = [kb.sb(ph, f"uts{i}", [128, KC, 512], BF16, dma=True) for i in range(2)]
            vts = [kb.sb(ph, f"vts{i}", [128, 4, D], BF16, dma=True) for i in range(2)]
            gss = [kb.sb(ph, f"gss{i}", [128, 4, 512], BF16, dma=True) for i in range(2)]
            gas = [kb.sb(ph, f"gas{i}", [128, 512], BF16) for i in range(2)]
            Wts = [kb.sb(ph, f"Wts{i}", [128, 512], BF16) for i in range(4)]
            WT, WT_b = kb.sb(ph, "WT", [128, 4, 512], BF16)
            acc, acc_b = kb.sb(ph, "pacc", [128, KC, 512], F32)
            x1t2 = [kb.sb(ph, f"px1{i}", [128, 512], F32, dma=True) for i in range(2)]
            x2t2 = [kb.sb(ph, f"px2{i}", [128, 512], F32) for i in range(2)]
            pbanks = [(kb.psbf[0][:], kb.psbf[1]), (kb.ps[6][0][:].bitcast(BF16), kb.ps[6][1])]
            uTv = uT_in.rearrange("(k p) e -> p k e", p=128)
            vv = v_in.rearrange("(c p) d -> p c d", p=128)
            h2v = h2T_d.rearrange("(k p) t -> p k t", p=128)
            nW = 0
            nT = 0
            for T in range(4):
                tsl = slice(T * 512, (T + 1) * 512)
                S.dma("sp", h2t[:], h2v[:, :, tsl], [h2T_b], h2t_b)
                for et in range(32):
                    ut, utb = uts[et % 2]
                    vt, vtb = vts[et % 2]
                    gs, gsb = gss[et % 2]
                    if T == 0:
                        S.dma("pool", ut[:], uTv[:, :, et * 512:(et + 1) * 512], [], utb)
                        S.dma("pool", vt[:].rearrange("p c (a b) -> p c a b", b=1024), vv[:, et * 4:(et + 1) * 4, :].rearrange("p c (a b) -> p c a b", b=1024), [], vtb)
                        S.dma("act", uTb_d[et], ut[:].rearrange("p k e -> p (k e)"), [utb], uTb_b)
                        S.dma("act", vb_d[et], vt[:].rearrange("p c d -> p (c d)"), [vtb], vb_b)
                    else:
                        S.dma("sp", ut[:].rearrange("p k e -> p (k e)"), uTb_d[et], [uTb_b], utb)
                        S.dma("act", vt[:].rearrange("p c d -> p (c d)"), vb_d[et], [vb_b], vtb)
                    S.dma("sp", gs[:], G_d[4 * T:4 * T + 4, :, et * 512:(et + 1) * 512].rearrange("a t e -> t a e"), [G_b], gsb)
                    wl = []
                    for a in range(4):
                        pst, psb = kb.psum()
                        for k in range(KC):
                            S.op("pe", lambda e, k=k, a=a, pst=pst, ut=ut: e.matmul(pst[:, :], lhsT=h2t[:, k, a * 128:(a + 1) * 128], rhs=ut[:, k, :],
                                                                                  start=(k == 0), stop=(k == KC - 1)), reads=[h2t_b, utb], writes=[psb])
                        ga, gab = gas[a % 2]
                        S.op("act", lambda e, pst=pst, ga=ga: e.activation(out=ga[:], in_=pst[:, :], func=AF.Gelu_apprx_tanh), reads=[psb], writes=[gab])
                        Wt, Wtb = Wts[nW % 4]
                        nW += 1
                        S.op("dve", lambda e, ga=ga, gs=gs, a=a, Wt=Wt: e.tensor_tensor(out=Wt[:], in0=ga[:], in1=gs[:, a, :], op=ALU.mult),
                             reads=[gab, gsb], writes=[Wtb])
                        wl.append((Wt, Wtb))
                    for c in range(4):
                        pbt, hb = pbanks[nT % 2]
                        nT += 1
                        for a in range(4):
                            S.op("pe", lambda e, a=a, c=c, pbt=pbt, wl=wl: e.transpose(out=pbt[:, a * 128:(a + 1) * 128], in_=wl[a][0][:, c * 128:(c + 1) * 128],
                                                                                      identity=identp[:]), reads=[wl[a][1], identp_b], writes=[hb])
                        S.op("act", lambda e, c=c, pbt=pbt: e.activation(out=WT[:, c, :], in_=pbt[:, 0:512], func=AF.Copy), reads=[hb], writes=[WT_b])
                    for dk in range(KC):
                        pst, psb = kb.psum()
                        for c in range(4):
                            S.op("pe", lambda e, c=c, dk=dk, pst=pst, vt=vt: e.matmul(pst[:, :], lhsT=vt[:, c, dk * 128:(dk + 1) * 128], rhs=WT[:, c, :],
                                                                                    start=(c == 0), stop=(c == 3)), reads=[vtb, WT_b], writes=[psb])
                        if et == 0:
                            S.op("dve", lambda e, dk=dk, pst=pst: e.tensor_copy(out=acc[:, dk, :], in_=pst[:, :]), reads=[psb], writes=[acc_b])
                        else:
                            S.op("dve", lambda e, dk=dk, pst=pst: e.tensor_tensor(out=acc[:, dk, :], in0=pst[:, :], in1=acc[:, dk, :], op=ALU.add),
                                 reads=[psb, acc_b], writes=[acc_b])
                for dk in range(KC):
                    x1t, x1tb = x1t2[dk % 2]
                    x2t, x2tb = x2t2[dk % 2]
                    S.dma("sp", x1t[:], x1T_d[dk * 128:(dk + 1) * 128, tsl], [x1T_b], x1tb)
                    S.op("dve", lambda e, dk=dk, x1t=x1t, x2t=x2t: e.scalar_tensor_tensor(
                        out=x2t[:], in0=acc[:, dk, :], scalar=mod[:, G2 + dk:G2 + dk + 1], in1=x1t[:], op0=ALU.mult, op1=ALU.add),
                        reads=[acc_b, mod_b, x1tb], writes=[x2tb])
                    S.dma("act", x2T_d[dk * 128:(dk + 1) * 128, tsl], x2t[:], [x2tb], x2T_b)
                if T == 0:
                    S.barrier()
                    S.retire([b_ for (_, b_) in uts + vts])
                    for (_, b_) in uts + vts:
                        b_.dma = True
            kb.end(ph)

    if stages >= 7:
        with contextlib.ExitStack() as ph:
            TT = 256
            xb2 = [kb.sb(ph, f"fx{i}", [128, KC, TT], F32, dma=True) for i in range(2)]
            sq, sqb = kb.sb(ph, "fsq", [128, KC, TT], BF16)
            rt_, rb_ = kb.sb(ph, "frstd", [128, TT], F32)
            ob2 = [kb.sb(ph, f"fo{i}", [128, KC, TT], F32) for i in range(2)]
            srcv = x2src_d.rearrange("(k p) t -> p k t", p=128)
            outv = outT.rearrange("(k p) t -> p k t", p=128)
            for ti in range(TOWN // TT):
                xt, xb = xb2[ti % 2]
                ot, ob = ob2[ti % 2]
                S.dma("sp", xt[:], srcv[:, :, ti * TT:(ti + 1) * TT], [x2src_b], xb)
                S.op("act", lambda e, xt=xt: e.activation(out=sq[:], in_=xt[:], func=AF.Square), reads=[xb], writes=[sqb])
                pst, psb = kb.psum()
                for k in range(KC):
                    S.op("pe", lambda e, k=k, pst=pst: e.matmul(pst[:, 0:TT], lhsT=ones_bf[:], rhs=sq[:, k, :], start=(k == 0), stop=(k == KC - 1)),
                         reads=[ones_bf_b, sqb], writes=[psb])
                S.op("act", lambda e, pst=pst: e.activation(out=rt_[:], in_=pst[:, 0:TT], func=AF.Sqrt, bias=eps_t[:, 0:1], scale=1.0 / D),
                     reads=[psb, eps_b], writes=[rb_])
                S.op("dve", lambda e: e.reciprocal(out=rt_[:], in_=rt_[:]), reads=[rb_], writes=[rb_])
                for k in range(KC):
                    S.op("dve", lambda e, k=k, xt=xt, ot=ot: e.scalar_tensor_tensor(out=ot[:, k, :], in0=xt[:, k, :], scalar=gfin[:, k:k + 1], in1=rt_[:],
                                                                                  op0=ALU.mult, op1=ALU.mult), reads=[xb, gfin_b, rb_], writes=[ob])
                S.dma("act", outv[:, :, ti * TT:(ti + 1) * TT], ot[:], [ob], outT_b)
            kb.end(ph)

    for name in debug:
        if name == "mod":
            continue
        src = {"qT_d": (qT_d, qT_b), "vs_d": (vs_d, vs_b), "uT_d": (uT_d, uT_b), "mgT_d": (mgT_d, mgT_b),
               "kcT_d": (kcT_d, kcT_b), "gT_d": (gT_d, gT_b), "oT_d": (oT_d, oT_b), "y2T_d": (y2T_d, y2T_b), "x1T_d": (x1T_d, x1T_b), "x2T_d": (x2T_d, x2T_b)}[name]
        o = nc.dram_tensor("dbg_" + name, list(src[0].shape), src[0].dtype, kind="ExternalOutput").ap()
        ob = S.buf("dbg_" + name, dma=True)
        S.dma("sp", o[:, :], src[0][:, :], [src[1]], ob)
    if "mod" in debug:
        o = nc.dram_tensor("dbg_mod", [128, 96], F32, kind="ExternalOutput").ap()
        ob = S.buf("dbg_mod", dma=True)
        S.dma("sp", o[:, :], mod[:], [mod_b], ob)

    S.barrier()
    es.close()
    return nc, kb


def pcol(v, chunks):
    return np.ascontiguousarray(np.asarray(v, np.float32).reshape(chunks, 128).T)


def t5_bucket_np(dist):
    dist = np.maximum(dist, 0)
    lr = np.log(np.maximum(dist, 1).astype(np.float32) / np.float32(16)) / np.float32(math.log(8.0))
    large = np.minimum(16 + (lr * np.float32(16)).astype(np.int32), 31)
    return np.where(dist < 16, dist, large)


NEG = np.float32(-30000.0)


def attn_tables(half, inp):
    LB, OFFB, LC, OFFC = 4096, 1024, 6400, 2064
    rb = np.asarray(inp["rel_bias"], np.float32)
    m = {}
    d = np.arange(LB) - OFFB
    g = rb[t5_bucket_np(d)].T
    Vb = np.where(d[None, :] >= 0, g, NEG).astype(np.float32)
    Vw = np.where((d[None, :] >= 0) & (d[None, :] < 512), g, NEG).astype(np.float32)
    m["Vb"], m["Vw"] = Vb, Vw
    m["Vwc"] = Vw if half == 1 else np.full_like(Vw, NEG)
    d = np.arange(LC) - OFFC
    g = rb[t5_bucket_np(d)].T
    Vc = np.where(d[None, :] >= 0, g, NEG).astype(np.float32)
    m["Vc"] = Vc
    m["Vc0"] = Vc if half == 1 else np.full_like(Vc, NEG)
    p = np.arange(128)[:, None, None]
    ts = np.arange(16)[None, :, None]
    j = np.arange(64)[None, None, :]
    t_abs = half * TOWN + ts * 128 + p
    cur = t_abs // 64
    ja = j - 32 * (1 - half)
    valid = (ja >= 0) & (ja <= cur)
    forced = (ja == 0) | (ja == cur) | (ja == cur - 1)
    m["selbias"] = np.where(valid, np.float32(1e4) * forced, np.float32(-1e30)).astype(np.float32).reshape(128, 1024)
    m["selinv"] = (~valid).astype(np.float32).reshape(128, 1024)
    pp = np.arange(128)[:, None]
    nn = 128 * np.arange(2)[None, :] + 127 - pp
    cs = nn[:, :, None] * 16
    ss = np.arange(64)[None, None, :] * 64
    ov = np.clip(np.minimum(cs + 32, ss + 64) - np.maximum(cs, ss), 0, None) / 32.0
    ov = np.where(nn[:, :, None] >= 255, 0.0, ov)
    maug = np.concatenate([ov, np.ones((128, 2, 1))], axis=2).astype(np.float32)
    m["maug"] = maug.reshape(128, 130)
    sl = 128 * np.arange(32)[None, :, None] + 127 - np.arange(128)[None, None, :]
    ne = np.where((sl // 64) == np.arange(64)[:, None, None], NEG, np.float32(0)).astype(np.float32).reshape(64, 4096)
    m["negE"] = np.concatenate([ne, ne], axis=0)
    k = np.arange(48)
    sg_ = np.repeat((k[:, None] == k[None, :]).astype(np.float32)[:, :, None], 64, axis=2).reshape(48, 3072)
    m["selg"] = np.concatenate([sg_, np.zeros((16, 3072), np.float32)], axis=0)
    m["ident"] = np.eye(128, dtype=np.float32)
    m["jmat"] = np.eye(128, dtype=np.float32)[::-1].copy()
    m["rel_bias"] = rb
    m["cmp_w1"] = np.ascontiguousarray(inp["cmp_w1"][0])
    m["cmp_posT"] = np.ascontiguousarray(np.transpose(inp["cmp_pos"][0], (0, 2, 1)))
    m["cmp_b1T"] = np.stack([pcol(inp["cmp_b1"][0, jj], 2) for jj in range(2)])
    m["cmp_w2"] = np.ascontiguousarray(inp["cmp_w2"][0])
    m["cmp_b2"] = np.ascontiguousarray(inp["cmp_b2"][0])
    return m


def s5_tables(inp):
    m = {}
    lre = np.asarray(inp["s5_lam_re"][0], np.float32)
    lim = np.asarray(inp["s5_lam_im"][0], np.float32)
    lst = np.asarray(inp["s5_log_step"][0], np.float32)
    def qp(a):
        return a.reshape(32, 2, 64).reshape(32, 128)
    lst2 = np.repeat(lst[:, None], 64, axis=1)
    m["lre_f"] = qp(lre).reshape(1, 4096).copy()
    m["lim_f"] = qp(lim).reshape(1, 4096).copy()
    m["lst_f"] = qp(lst2).reshape(1, 4096).copy()
    m["lreT"] = np.ascontiguousarray(qp(lre).T)
    m["limT"] = np.ascontiguousarray(qp(lim).T)
    m["lstT"] = np.ascontiguousarray(qp(lst2).T)
    bre = np.asarray(inp["s5_b_re"][0], np.float32)
    bim = np.asarray(inp["s5_b_im"][0], np.float32)
    cre = np.asarray(inp["s5_c_re"][0], np.float32)
    cim = np.asarray(inp["s5_c_im"][0], np.float32)
    Bre = np.zeros((128, 32, 128), np.float32); Bim = np.zeros_like(Bre)
    Cre = np.zeros((128, 32, 128), np.float32); Cim = np.zeros_like(Cre)
    for pr in range(32):
        for gg in range(2):
            g = 2 * pr + gg
            lg = g % 8
            Bre[lg * 16:(lg + 1) * 16, pr, gg * 64:(gg + 1) * 64] = bre[g].T
            Bim[lg * 16:(lg + 1) * 16, pr, gg * 64:(gg + 1) * 64] = bim[g].T
            Cre[gg * 64:(gg + 1) * 64, pr, lg * 16:(lg + 1) * 16] = cre[g].T
            Cim[gg * 64:(gg + 1) * 64, pr, lg * 16:(lg + 1) * 16] = cim[g].T
    m["Bpad_re"] = Bre.reshape(128, 4096); m["Bpad_im"] = Bim.reshape(128, 4096)
    m["Cpad_re"] = Cre.reshape(128, 4096); m["Cpad_im"] = Cim.reshape(128, 4096)
    m["tau"] = np.arange(512, dtype=np.float32).reshape(1, 512)
    m["s5dT"] = pcol(inp["s5_d"][0], 8)
    m["glu_w"] = np.ascontiguousarray(inp["glu_w"][0])
    m["glubT"] = pcol(inp["glu_b"][0], 8)
    return m


SHARED = {}


def prep_shared(inp):
    SHARED["peer_uT"] = np.ascontiguousarray(np.asarray(inp["peer_u"][0]).T)
    SHARED["peer_v"] = np.ascontiguousarray(inp["peer_v"][0])


def prep_inputs(core, inp):
    if "peer_uT" not in SHARED:
        prep_shared(inp)
    b, half = core // 2, core % 2
    x = inp["x"]
    own = x[b, half * TOWN:(half + 1) * TOWN]
    ctx = x[b, 0:TOWN] if half == 1 else np.zeros_like(own)
    xT = np.ascontiguousarray(np.concatenate([ctx, own], 0).T)
    m = {
        "xT": xT,
        "xTr": np.ascontiguousarray(xT.reshape(D, TALL // 128, 128)[:, :, ::-1].reshape(D, TALL)),
        "cT": pcol(inp["c"][b], KC),
        "ada_w": np.ascontiguousarray(inp["ada_w"][0]),
        "ada_bT": pcol(inp["ada_b"][0], 96),
        "gmixT": pcol(inp["norm_mix_g"][0], KC),
        "gffnT": pcol(inp["norm_ffn_g"][0], KC),
        "gfinT": pcol(inp["final_g"], KC),
        "w_in": np.ascontiguousarray(inp["w_in"][0]),
        "ctxflag": np.full((128, 1), float(half), np.float32),
    }
    m.update(attn_tables(half, inp))
    m.update(s5_tables(inp))
    m["w_up_attn"] = np.ascontiguousarray(inp["w_up_attn"][0])
    m["w_up_ssm"] = np.ascontiguousarray(inp["w_up_ssm"][0])
    m["w_out"] = np.ascontiguousarray(inp["w_out"][0])
    m["peer_w_q"] = np.ascontiguousarray(inp["peer_w_q"][0])
    sk = np.asarray(inp["peer_sub_keys"][0], np.float32)
    m["keysT"] = np.ascontiguousarray(np.transpose(sk.reshape(16, 128, 128), (2, 0, 1)).reshape(128, 2048))
    m["peer_uT"] = SHARED["peer_uT"]
    m["peer_v"] = SHARED["peer_v"]
    return m


def kernel(**inputs):
    inp = {k: np.asarray(v) for k, v in inputs.items()}
    nc, kb = build()
    in_maps = []
    for core in range(8):
        m = prep_inputs(core, inp)
        in_maps.append({k: m[k] for k in kb.inputs})
    res = run_bass_kernel_spmd(nc, in_maps, core_ids=list(range(8)))
    out = np.zeros((4, 4096, D), np.float32)
    for core in range(8):
        b, half = core // 2, core % 2
        out[b, half * TOWN:(half + 1) * TOWN] = res.results[core]["outT"].T
    return out
```

```python
import contextlib
import math
import os
import numpy as np
import concourse.bass as bass
import concourse.mybir as mybir
from concourse.bass_utils import run_bass_kernel_spmd

F32 = mybir.dt.float32
BF16 = mybir.dt.bfloat16
U32 = mybir.dt.uint32
AF = mybir.ActivationFunctionType
ALU = mybir.AluOpType
AX = mybir.AxisListType

D = 2048
TOWN = 2048
TALL = 4096
KC = 16
INW = 7728
EPS = 1e-6


class _Stop(Exception):
    pass


CUT = int(os.environ.get("KCUT", "0"))


class Buf:
    def __init__(self, name, dsem=None):
        self.name = name
        self.dma = False
        self.kind = None
        self.w = None
        self.r = {}
        self.dsem = dsem
        self.dcount = 0


class Sched:
    LIMIT = 4000

    def __init__(self, nc, es):
        self.nc = nc
        self.es = es
        self.eng = {"pe": nc.tensor, "dve": nc.vector, "act": nc.scalar, "pool": nc.gpsimd, "sp": nc.sync}
        self.sem = {}
        self.cnt = {}
        self.semid = {}
        self.nsem = 0
        self.waited = {e: {} for e in self.eng}
        self.dmabufs = []
        self.freesems = {}
        self.ninst = 0
        for e in self.eng:
            self._newsem(e)

    def _newsem(self, e):
        s = self.es.enter_context(self.nc.semaphore(f"s{self.nsem}_{e}"))
        self.nsem += 1
        self.sem[e] = (s, self.nsem)
        self.cnt[e] = 0

    def buf(self, name, dma=False):
        b = Buf(name)
        b.dma = dma
        b.kind = None
        return b

    def _dsem(self, b, kind):
        if b.dsem is None:
            assert b.dma, b.name
            fl = self.freesems.setdefault(kind, [])
            if fl:
                b.dsem, b.dcount = fl.pop()
            else:
                b.dsem = (self.es.enter_context(self.nc.semaphore(f"d{self.nsem}")), self.nsem + 1)
                self.nsem += 1
                b.dcount = 0
            b.kind = kind
            self.dmabufs.append(b)
        assert b.kind == kind, (b.name, b.kind, kind)

    def retire(self, bufs):
        for b in bufs:
            if b.dsem is not None and b in self.dmabufs:
                self.dmabufs.remove(b)
                self.freesems.setdefault(b.kind, []).append((b.dsem, b.dcount))
                b.dsem = None
                b.dma = False

    def _wait(self, X, tok):
        if tok[0] == "dma":
            b = tok[1]
            if b.dsem is None or b.dcount == 0:
                return
            sem, sid = b.dsem
            val = 16 * b.dcount
            E = None
        else:
            (sem, sid), val, E = tok
        if E == X and X in ("pe", "sp"):
            return
        if self.waited[X].get(sid, 0) >= val:
            return
        self.eng[X].wait_ge(sem, val)
        self.waited[X][sid] = val
        self.ninst += 1

    def _deps(self, X, reads, writes):
        for b in reads:
            if b.w is not None:
                self._wait(X, b.w)
        for b in writes:
            if b.w is not None:
                self._wait(X, b.w)
            for t in b.r.values():
                self._wait(X, t)

    def op(self, X, fn, reads=(), writes=()):
        ex = [b for b in reads if getattr(b, "excl", False) and b not in writes]
        if ex:
            writes = list(writes) + ex
            reads = [b for b in reads if b not in ex]
        self._deps(X, reads, writes)
        inst = fn(self.eng[X])
        if self.cnt[X] >= self.LIMIT:
            self._newsem(X)
        self.cnt[X] += 1
        inst.then_inc(self.sem[X][0], 1)
        tok = (self.sem[X], self.cnt[X], X)
        for b in reads:
            b.r[X] = tok
        for b in writes:
            b.w = tok
            b.r = {}
        self.ninst += 1
        return tok

    def dma(self, X, out, in_, reads, wbuf, **kw):
        self._dsem(wbuf, "sw" if X == "pool" else "hw")
        self._deps(X, reads, [wbuf])
        inst = self.eng[X].dma_start(out=out, in_=in_, **kw)
        inst.then_inc(wbuf.dsem[0], 16)
        wbuf.dcount += 1
        wbuf.w = ("dma", wbuf)
        wbuf.r = {}
        for b in reads:
            b.r[("dma", id(wbuf))] = ("dma", wbuf)
        self.ninst += 1

    def barrier(self):
        for X in self.eng:
            for E in self.eng:
                if E != X and self.cnt[E] > 0:
                    self._wait(X, (self.sem[E], self.cnt[E], E))
            for b in self.dmabufs:
                if b.dsem is not None and b.dcount > 0:
                    self._wait(X, ("dma", b))


class KB:
    def __init__(self, stages=99, debug=()):
        self.stages = stages
        self.debug = debug
        self.nc = bass.Bass("TRN2", target_bir_lowering=False)
        self.es = contextlib.ExitStack()
        self.S = Sched(self.nc, self.es)
        self.inputs = {}
        self.psn = 0
        self.ctxbufs = {}

    def din(self, name, shape, dtype=F32):
        t = self.nc.dram_tensor(name, list(shape), dtype, kind="ExternalInput").ap()
        self.inputs[name] = t
        return t

    def dscratch(self, name, shape, dtype):
        t = self.nc.dram_tensor(name, list(shape), dtype, kind="Internal").ap()
        return t, self.S.buf(name, dma=True)

    def sb(self, ctx, name, shape, dtype, dma=False):
        t = ctx.enter_context(self.nc.sbuf_tensor(name, list(shape), dtype))
        b = self.S.buf(name, dma=dma)
        self.ctxbufs.setdefault(id(ctx), []).append(b)
        return t, b

    def end(self, ctx):
        self.S.barrier()
        self.S.retire(self.ctxbufs.pop(id(ctx), []))

    def psum_init(self):
        self.ps = []
        for i in range(7):
            t = self.es.enter_context(self.nc.psum_tensor(f"ps{i}", [128, 512], F32))
            self.ps.append((t, self.S.buf(f"ps{i}")))
            self.ps[-1][1].excl = True
        t = self.es.enter_context(self.nc.psum_tensor("psbf", [128, 1024], BF16))
        self.psbf = (t, self.S.buf("psbf"))
        self.psbf[1].excl = True

    def psum(self, nrot=4):
        r = self.ps[self.psn % nrot]
        self.psn += 1
        return r


def build(stages=99, debug=(), skip=(), sub=99):
    kb = KB(stages, debug)
    nc, S = kb.nc, kb.S
    kb.psum_init()
    es = kb.es

    xT = kb.din("xT", [D, TALL])
    xTr = kb.din("xTr", [D, TALL])
    cT = kb.din("cT", [128, KC])
    ada_w = kb.din("ada_w", [D, 6 * D])
    ada_bT = kb.din("ada_bT", [128, 96])
    gmixT = kb.din("gmixT", [128, KC])
    gffnT = kb.din("gffnT", [128, KC])
    gfinT = kb.din("gfinT", [128, KC])
    w_in = kb.din("w_in", [D, INW])
    ctxflag = kb.din("ctxflag", [128, 1])

    outT = nc.dram_tensor("outT", [D, TOWN], F32, kind="ExternalOutput").ap()
    outT_b = S.buf("outT", dma=True)

    qT_d, qT_b = kb.dscratch("qT_d", [1024, TOWN], BF16)
    kcT_d, kcT_b = kb.dscratch("kcT_d", [256, TALL], BF16)
    vcT_d, vcT_b = kb.dscratch("vcT_d", [256, TALL], BF16)
    ksT_d, ksT_b = kb.dscratch("ksT_d", [256, TALL], BF16)
    kwT_d, kwT_b = kb.dscratch("kwT_d", [256, TALL], BF16)
    vs_d, vs_b = kb.dscratch("vs_d", [TALL, 256], BF16)
    vw_d, vw_b = kb.dscratch("vw_d", [TALL, 256], BF16)
    gT_d, gT_b = kb.dscratch("gT_d", [48, TOWN], BF16)
    uT_d, uT_b = kb.dscratch("uT_d", [1024, TALL], BF16)
    mgT_d, mgT_b = kb.dscratch("mgT_d", [4096, TOWN], BF16)

    pc = es
    ones_bf, ones_bf_b = kb.sb(pc, "ones_bf", [128, 128], BF16)
    eps_t, eps_b = kb.sb(pc, "eps_t", [128, 1], F32)
    mod, mod_b = kb.sb(pc, "mod", [128, 96], F32)
    gm1, gm1_b = kb.sb(pc, "gm1", [128, KC], F32)
    gm2, gm2_b = kb.sb(pc, "gm2", [128, KC], F32)
    gfin, gfin_b = kb.sb(pc, "gfin", [128, KC], F32, dma=True)
    flag_t, flag_b = kb.sb(pc, "flag_t", [128, 1], F32, dma=True)
    S.op("dve", lambda e: e.memset(ones_bf[:], 1.0), writes=[ones_bf_b])
    S.op("dve", lambda e: e.memset(eps_t[:], EPS), writes=[eps_b])
    S.dma("sp", gfin[:], gfinT[:, :], [], gfin_b)
    S.dma("sp", flag_t[:], ctxflag[:, :], [], flag_b)

    with contextlib.ExitStack() as ph:
        c_sb, c_b = kb.sb(ph, "c_sb", [128, KC], F32, dma=True)
        sc_sb, sc_b = kb.sb(ph, "sc_sb", [128, KC], F32)
        ab_sb, ab_b = kb.sb(ph, "ab_sb", [128, 96], F32, dma=True)
        gx_sb, gx_b = kb.sb(ph, "gx_sb", [128, 2 * KC], F32, dma=True)
        wts = [kb.sb(ph, f"adaw{i}", [128, KC, 512], F32, dma=True) for i in range(2)]
        S.dma("sp", c_sb[:], cT[:, :], [], c_b)
        S.dma("sp", ab_sb[:], ada_bT[:, :], [], ab_b)
        S.dma("sp", gx_sb[:, 0:KC], gmixT[:, :], [], gx_b)
        S.dma("sp", gx_sb[:, KC:2 * KC], gffnT[:, :], [], gx_b)
        S.op("act", lambda e: e.activation(out=sc_sb[:], in_=c_sb[:], func=AF.Silu), reads=[c_b], writes=[sc_b])
        pst, psb = kb.psum()
        awv = ada_w.rearrange("(k p) f -> p k f", p=128)
        for fg in range(24):
            wt, wb = wts[fg % 2]
            S.dma("sp" if fg % 2 == 0 else "act", wt[:], awv[:, :, fg * 512:(fg + 1) * 512], [], wb)
            for fc in range(4):
                col = fg * 4 + fc
                for k in range(KC):
                    S.op("pe", lambda e, k=k, fc=fc, col=col, wt=wt: e.matmul(
                        pst[:, col:col + 1], lhsT=wt[:, k, fc * 128:(fc + 1) * 128], rhs=sc_sb[:, k:k + 1],
                        start=(k == 0), stop=(k == KC - 1)), reads=[wb, sc_b], writes=[psb])
        S.op("dve", lambda e: e.tensor_tensor(out=mod[:], in0=pst[:, 0:96], in1=ab_sb[:], op=ALU.add),
             reads=[psb, ab_b], writes=[mod_b])
        S.op("dve", lambda e: e.scalar_tensor_tensor(out=gm1[:], in0=mod[:, 16:32], scalar=1.0, in1=gx_sb[:, 0:KC],
                                                     op0=ALU.add, op1=ALU.mult), reads=[mod_b, gx_b], writes=[gm1_b])
        S.op("dve", lambda e: e.scalar_tensor_tensor(out=gm2[:], in0=mod[:, 64:80], scalar=1.0, in1=gx_sb[:, KC:2 * KC],
                                                     op0=ALU.add, op1=ALU.mult), reads=[mod_b, gx_b], writes=[gm2_b])
        kb.end(ph)

    SH1, G1, SH2, G2 = 0, 32, 48, 80

    def norm_block(ph, src_ap_fn, t0, nt, hT, hT_b, gm, gm_b, shcol, xbufs, tmpbufs, sqbuf, rbuf):
        TT = 256
        for ti in range(nt // TT):
            xt, xb = xbufs[ti % 2]
            S.dma("sp", xt[:], src_ap_fn(t0 + ti * TT, TT), [], xb)
            sq, sqb = sqbuf
            S.op("act", lambda e, xt=xt: e.activation(out=sq[:], in_=xt[:], func=AF.Square), reads=[xb], writes=[sqb])
            pst, psb = kb.psum()
            for k in range(KC):
                S.op("pe", lambda e, k=k: e.matmul(pst[:, 0:TT], lhsT=ones_bf[:], rhs=sq[:, k, :],
                                                   start=(k == 0), stop=(k == KC - 1)),
                     reads=[ones_bf_b, sqb], writes=[psb])
            rt, rb = rbuf
            S.op("act", lambda e: e.activation(out=rt[:], in_=pst[:, 0:TT], func=AF.Sqrt, bias=eps_t[:, 0:1],
                                               scale=1.0 / D), reads=[psb, eps_b], writes=[rb])
            S.op("dve", lambda e: e.reciprocal(out=rt[:], in_=rt[:]), reads=[rb], writes=[rb])
            for k in range(KC):
                tt, tb = tmpbufs[k % 2]
                S.op("dve", lambda e, k=k, tt=tt, xt=xt: e.scalar_tensor_tensor(
                    out=tt[:], in0=xt[:, k, :], scalar=gm[:, k:k + 1], in1=rt[:], op0=ALU.mult, op1=ALU.mult),
                    reads=[xb, gm_b, rb], writes=[tb])
                S.op("act", lambda e, k=k, tt=tt, ti=ti: e.activation(
                    out=hT[:, k, ti * TT:(ti + 1) * TT], in_=tt[:], func=AF.Identity,
                    bias=mod[:, shcol + k:shcol + k + 1], scale=1.0), reads=[tb, mod_b], writes=[hT_b])

    if stages >= 2:
        with contextlib.ExitStack() as ph:
            TB = 1024
            hT, hT_b = kb.sb(ph, "hT", [128, KC, TB], BF16)
            xbufs = [kb.sb(ph, f"xb{i}", [128, KC, 256], F32, dma=True) for i in range(2)]
            tmpbufs = [kb.sb(ph, f"ntmp{i}", [128, 256], F32) for i in range(2)]
            sqbuf = kb.sb(ph, "sqb", [128, KC, 256], BF16)
            rbuf = kb.sb(ph, "rstd", [128, 256], F32)
            wbufs = [kb.sb(ph, f"wbuf{i}", [128, KC, 512], BF16, dma=True) for i in range(2)]
            obufs = [kb.sb(ph, f"obuf{i}", [128, 512], BF16) for i in range(4)]
            xTv = xT.rearrange("(k p) t -> p k t", p=128)
            winv = w_in.rearrange("(k p) c -> p k c", p=128)
            wcount = [0]
            ocount = [0]

            def load_w(col0, ncols):
                wt, wb = wbufs[wcount[0] % 2]
                wcount[0] += 1
                S.dma("pool", wt[:, :, 0:ncols], winv[:, :, col0:col0 + ncols], [], wb)
                return wt, wb

            def fm_cols(col0, ncols, tb0, epi):
                wt, wb = load_w(col0, ncols)
                for cc in range((ncols + 127) // 128):
                    cw = min(128, ncols - cc * 128)
                    for tt in range(TB // 512):
                        pst, psb = kb.psum()
                        for k in range(KC):
                            S.op("pe", lambda e, k=k, cc=cc, cw=cw, tt=tt, pst=pst, wt=wt: e.matmul(
                                pst[0:cw, :], lhsT=wt[:, k, cc * 128:cc * 128 + cw], rhs=hT[:, k, tt * 512:(tt + 1) * 512],
                                start=(k == 0), stop=(k == KC - 1)), reads=[wb, hT_b], writes=[psb])
                        epi(col0 + cc * 128, cw, tb0 + tt * 512, pst, psb)

            def store_epi(dst_d, dst_b, rowbase, tbase, func=AF.Copy, scale=1.0, flag=False):
                def epi(col, cw, t, pst, psb):
                    ot, ob = obufs[ocount[0] % 4]
                    ocount[0] += 1
                    if flag:
                        S.op("act", lambda e: e.activation(out=ot[0:cw, :], in_=pst[0:cw, :], func=AF.Copy,
                                                           scale=flag_t[0:cw, 0:1]), reads=[psb, flag_b], writes=[ob])
                    else:
                        S.op("act", lambda e: e.activation(out=ot[0:cw, :], in_=pst[0:cw, :], func=func, scale=scale),
                             reads=[psb], writes=[ob])
                    r0 = col - rowbase
                    S.dma("sp", dst_d[r0:r0 + cw, t - tbase:t - tbase + 512], ot[0:cw, :], [ob], dst_b)
                return epi

            def tm_cols(col0, ncols, tb0, dst_d, dst_b):
                wt, wb = load_w(col0, ncols)
                for sc in range(TB // 128):
                    pst, psb = kb.psum()
                    for k in range(KC):
                        S.op("pe", lambda e, k=k, sc=sc, pst=pst, wt=wt: e.matmul(
                            pst[:, 0:ncols], lhsT=hT[:, k, sc * 128:(sc + 1) * 128], rhs=wt[:, k, 0:ncols],
                            start=(k == 0), stop=(k == KC - 1)), reads=[wb, hT_b], writes=[psb])
                    ot, ob = obufs[ocount[0] % 4]
                    ocount[0] += 1
                    S.op("act", lambda e, pst=pst, ot=ot: e.activation(out=ot[:, 0:ncols], in_=pst[:, 0:ncols], func=AF.Copy),
                         reads=[psb], writes=[ob])
                    S.dma("sp", dst_d[tb0 + sc * 128:tb0 + (sc + 1) * 128, :], ot[:, 0:ncols], [ob], dst_b)

            xTrv = xTr.rearrange("(k p) t -> p k t", p=128)
            for rev in (False, True):
                srcv = xTrv if rev else xTv
                for blk in range(TALL // TB):
                    tb0 = blk * TB
                    own = tb0 >= TALL - TOWN
                    norm_block(ph, lambda t, n, srcv=srcv: srcv[:, :, t:t + n], tb0, TB, hT, hT_b, gm1, gm1_b, SH1,
                               xbufs, tmpbufs, sqbuf, rbuf)
                    if rev:
                        fm_cols(1536, 256, tb0, store_epi(ksT_d, ksT_b, 1536, 0))
                        fm_cols(2048, 256, tb0, store_epi(kwT_d, kwT_b, 2048, 0))
                        tm_cols(1792, 256, tb0, vs_d, vs_b)
                        tm_cols(2304, 256, tb0, vw_d, vw_b)
                        continue
                    fm_cols(1024, 256, tb0, store_epi(kcT_d, kcT_b, 1024, 0))
                    fm_cols(1280, 256, tb0, store_epi(vcT_d, vcT_b, 1280, 0))
                    fm_cols(2608, 512, tb0, store_epi(uT_d, uT_b, 2608, 0, flag=not own))
                    fm_cols(3120, 512, tb0, store_epi(uT_d, uT_b, 2608, 0, flag=not own))
                    if own:
                        tq = TALL - TOWN
                        fm_cols(0, 512, tb0, store_epi(qT_d, qT_b, 0, tq, scale=0.125))
                        fm_cols(512, 512, tb0, store_epi(qT_d, qT_b, 0, tq, scale=0.125))
                        fm_cols(2560, 48, tb0, store_epi(gT_d, gT_b, 2560, tq, func=AF.Sigmoid))
                        for g in range(8):
                            fm_cols(3632 + g * 512, 512, tb0, store_epi(mgT_d, mgT_b, 3632, tq, func=AF.Sigmoid))
            kb.end(ph)


    oT_d, oT_b = kb.dscratch("oT_d", [1024, TOWN], BF16)
    LB, OFFB, LC, OFFC = 4096, 1024, 6400, 2064
    if stages >= 3 and 3 not in skip:
        cmp_w1 = kb.din("cmp_w1", [2, 2048, 256])
        cmp_posT = kb.din("cmp_posT", [2, 64, 32])
        cmp_b1T = kb.din("cmp_b1T", [2, 128, 2])
        cmp_w2 = kb.din("cmp_w2", [2, 256, 64])
        cmp_b2 = kb.din("cmp_b2", [2, 64])
        rel_bias = kb.din("rel_bias", [32, 16])
        ident_in = kb.din("ident", [128, 128])
        jmat_in = kb.din("jmat", [128, 128])
        selg_in = kb.din("selg", [64, 48 * 64])
        negE_in = kb.din("negE", [128, 32 * 128])
        maug_in = kb.din("maug", [128, 2 * 65])
        selbias_in = kb.din("selbias", [128, 16 * 64])
        selinv_in = kb.din("selinv", [128, 16 * 64])
        Vb_in = kb.din("Vb", [16, LB])
        Vw_in = kb.din("Vw", [16, LB])
        Vwc_in = kb.din("Vwc", [16, LB])
        Vc_in = kb.din("Vc", [16, LC])
        Vc0_in = kb.din("Vc0", [16, LC])
        with contextlib.ExitStack() as ph:
          try:
              identb, identb_b = kb.sb(ph, "identb", [128, 128], BF16, dma=True)
              Jb, Jb_b = kb.sb(ph, "Jb", [128, 128], BF16, dma=True)
              selg, selg_b = kb.sb(ph, "selg_sb", [64, 48 * 64], BF16, dma=True)
              negE, negE_b = kb.sb(ph, "negE_sb", [128, 32 * 128], BF16, dma=True)
              maug, maug_b = kb.sb(ph, "maug_sb", [128, 2 * 65], BF16, dma=True)
              selbias, selbias_b = kb.sb(ph, "selbias_sb", [128, 16 * 64], F32, dma=True)
              rb31, rb31_b = kb.sb(ph, "rb31", [128, 16], F32, dma=True)
              qT_sb, qT_sbb = kb.sb(ph, "qT_sb", [128, 8, TOWN], BF16, dma=True)
              gT_sb, gT_sbb = kb.sb(ph, "gT_sb", [64, TOWN], BF16, dma=True)
              kccT = [kb.sb(ph, f"kccT{i}", [128, 256], BF16) for i in range(4)]
              vcc = [kb.sb(ph, f"vcc{i}", [128, 2, 64], BF16) for i in range(4)]
              ones64, ones64_b = kb.sb(ph, "ones64", [128, 64], BF16)
              S.op("dve", lambda e: e.memset(ones64[:], 1.0), writes=[ones64_b])
              S.dma("pool", identb[:], ident_in[:, :], [], identb_b)
              S.dma("pool", Jb[:], jmat_in[:, :], [], Jb_b)
              S.dma("pool", selg[:].rearrange("p (a b) -> p a b", b=1024), selg_in.rearrange("p (a b) -> p a b", b=1024), [], selg_b)
              S.dma("pool", negE[:].rearrange("p (a b) -> p a b", b=1024), negE_in.rearrange("p (a b) -> p a b", b=1024), [], negE_b)
              S.dma("pool", maug[:], maug_in[:, :], [], maug_b)
              S.dma("sp", selbias[:], selbias_in[:, :], [], selbias_b)
              selinv, selinv_b = kb.sb(ph, "selinv_sb", [128, 16 * 64], F32, dma=True)
              S.dma("sp", selinv[:], selinv_in[:, :], [], selinv_b)
              S.dma("sp", rb31[:], rel_bias[31:32, :].partition_broadcast(128), [], rb31_b)
              S.dma("sp", qT_sb[:], qT_d.rearrange("(c p) t -> p c t", p=128), [qT_b], qT_sbb)
              S.op("dve", lambda e: e.memset(gT_sb[:], 0.0), writes=[gT_sbb])
              S.dma("sp", gT_sb[0:48, :], gT_d[:, :], [gT_b], gT_sbb)

              with contextlib.ExitStack() as pd:
                  w1r, w1r_b = kb.sb(pd, "w1r", [64, 32, 256], BF16, dma=True)
                  posT, posT_b = kb.sb(pd, "posT", [64, 32], BF16, dma=True)
                  b1T, b1T_b = kb.sb(pd, "b1T", [128, 2], F32, dma=True)
                  w2d, w2d_b = kb.sb(pd, "w2d", [128, 2, 64], BF16, dma=True)
                  b2row, b2row_b = kb.sb(pd, "b2row", [128, 64], F32, dma=True)
                  biasH, biasH_b = kb.sb(pd, "biasH", [128, 2], F32)
                  kch, kch_b = kb.sb(pd, "kch", [64, TALL], BF16, dma=True)
                  hg = [kb.sb(pd, f"hg{i}", [128, 256], BF16) for i in range(2)]
                  kdup, kdup_b = kb.sb(pd, "kdup", [128, 128], BF16)
                  vtm, vtm_b = kb.sb(pd, "vtm", [128, 64], BF16)
                  for i in range(2):
                      S.op("dve", lambda e, i=i: e.memset(hg[i][0][:], 0.0), writes=[hg[i][1]])
                  for j in range(2 if CUT != 1 else 0):
                      S.dma("pool", w1r[:], cmp_w1[j].rearrange("(pos dh) hid -> dh pos hid", dh=64), [], w1r_b)
                      S.dma("pool", posT[:], cmp_posT[j], [], posT_b)
                      S.dma("sp", b1T[:], cmp_b1T[j], [], b1T_b)
                      S.dma("pool", w2d[:], cmp_w2[j].rearrange("(c p) d -> p c d", p=128), [], w2d_b)
                      S.dma("sp", b2row[:], cmp_b2[j:j + 1, :].partition_broadcast(128), [], b2row_b)
                      pst, psb = kb.ps[4]
                      for hc in range(2):
                          for pos in range(32):
                              S.op("pe", lambda e, hc=hc, pos=pos: e.matmul(
                                  pst[:, hc:hc + 1], lhsT=w1r[0:64, pos, hc * 128:(hc + 1) * 128], rhs=posT[0:64, pos:pos + 1],
                                  start=(pos == 0), stop=(pos == 31)), reads=[w1r_b, posT_b], writes=[psb])
                      S.op("dve", lambda e: e.tensor_tensor(out=biasH[:], in0=pst[:, 0:2], in1=b1T[:], op=ALU.add),
                           reads=[psb, b1T_b], writes=[biasH_b])
                      src_d, src_b = (kcT_d, kcT_b) if j == 0 else (vcT_d, vcT_b)
                      for kvh in range(4 if CUT != 2 else 0):
                          S.dma("sp", kch[:], src_d[kvh * 64:(kvh + 1) * 64, :], [src_b], kch_b)
                          for hc in range(2):
                              pst, psb = kb.psum()
                              for pos in range(32):
                                  S.op("pe", lambda e, hc=hc, pos=pos, pst=pst: e.matmul(
                                      pst[:, 0:255], lhsT=w1r[0:64, pos, hc * 128:(hc + 1) * 128],
                                      rhs=kch[0:64, pos:pos + 4065:16], start=(pos == 0), stop=(pos == 31)),
                                      reads=[w1r_b, kch_b], writes=[psb])
                              S.op("act", lambda e, hc=hc, pst=pst: e.activation(
                                  out=hg[hc][0][:, 0:255], in_=pst[:, 0:255], func=AF.Gelu_apprx_tanh,
                                  bias=biasH[:, hc:hc + 1], scale=1.0), reads=[psb, biasH_b], writes=[hg[hc][1]])
                          for nch in range(0 if (CUT == 3 or (CUT == 4 and j == 1) or (CUT == 5 and j == 0)) else 2):
                              pst, psb = kb.psum()
                              for hc in range(2):
                                  S.op("pe", lambda e, hc=hc, nch=nch, pst=pst: e.matmul(
                                      pst[:, 0:64], lhsT=hg[hc][0][:, nch * 128:(nch + 1) * 128], rhs=w2d[:, hc, :],
                                      start=(hc == 0), stop=(hc == 1)), reads=[hg[hc][1], w2d_b], writes=[psb])
                              pst3, psb3 = kb.psum()
                              if j == 0:
                                  for hh in range(2):
                                      S.op("dve", lambda e, hh=hh, pst=pst: e.tensor_tensor(
                                          out=kdup[:, hh * 64:(hh + 1) * 64], in0=pst[:, 0:64], in1=b2row[:], op=ALU.add),
                                          reads=[psb, b2row_b], writes=[kdup_b])
                                  S.op("pe", lambda e, pst3=pst3: e.matmul(pst3[:, 0:128], lhsT=kdup[:], rhs=Jb[:], start=True, stop=True),
                                       reads=[kdup_b, Jb_b], writes=[psb3])
                                  S.op("act", lambda e, kvh=kvh, nch=nch, pst3=pst3: e.activation(
                                      out=kccT[kvh][0][:, nch * 128:(nch + 1) * 128], in_=pst3[:, 0:128], func=AF.Copy),
                                      reads=[psb3], writes=[kccT[kvh][1]])
                              else:
                                  S.op("dve", lambda e, pst=pst: e.tensor_tensor(out=vtm[:], in0=pst[:, 0:64], in1=b2row[:], op=ALU.add),
                                       reads=[psb, b2row_b], writes=[vtm_b])
                                  S.op("pe", lambda e, pst3=pst3: e.matmul(pst3[:, 0:64], lhsT=Jb[:], rhs=vtm[:], start=True, stop=True),
                                       reads=[vtm_b, Jb_b], writes=[psb3])
                                  S.op("act", lambda e, kvh=kvh, nch=nch, pst3=pst3: e.activation(
                                      out=vcc[kvh][0][:, nch, :], in_=pst3[:, 0:64], func=AF.Copy),
                                      reads=[psb3], writes=[vcc[kvh][1]])
                  kb.end(pd)

              ksT2, ksT2_b = kb.sb(ph, "ksT2", [128, TALL], BF16, dma=True)
              kwT2, kwT2_b = kb.sb(ph, "kwT2", [128, TALL], BF16, dma=True)
              vs_all, vs_all_b = kb.sb(ph, "vs_all", [128, 32, 256], BF16, dma=True)
              vw_all, vw_all_b = kb.sb(ph, "vw_all", [128, 32, 256], BF16, dma=True)
              notselT, notselT_b = kb.sb(ph, "notselT", [128, TOWN], BF16)
              oacc, oacc_b = kb.sb(ph, "oacc", [64, 4, TOWN], F32)
              impacc, impacc_b = kb.sb(ph, "impacc", [128, 16, 64], F32)
              btiles = [kb.sb(ph, f"btile{i}", [128, 512], F32, dma=True) for i in range(3)]
              sfp = [kb.sb(ph, f"sfp{i}", [128, 512], F32) for i in range(2)]
              pus = [kb.sb(ph, f"pu{i}", [128, 512], BF16) for i in range(4)]
              rec, rec_b = kb.sb(ph, "rec", [128, 512], F32)
              fac, fac_b = kb.sb(ph, "fac", [128, 512], F32)
              otmp, otmp_b = kb.sb(ph, "otmp", [128, 512], F32)
              rec2, rec2_b = kb.sb(ph, "rec2", [128, 1], F32)
              simp, simp_b = kb.sb(ph, "simp", [128, 64], F32)
              simp2, simp2_b = kb.sb(ph, "simp2", [128, 64], F32)
              m8a, m8a_b = kb.sb(ph, "m8a", [128, 8], F32)
              m8b, m8b_b = kb.sb(ph, "m8b", [128, 8], F32)
              nsel, nsel_b = kb.sb(ph, "nsel", [128, 128], BF16)
              obf, obf_b = kb.sb(ph, "obf", [64, 4, TOWN], BF16)
              cnt = {"bt": 0, "sf": 0, "pu": 0}
              pso, pso_b = kb.ps[6]
              psg, psg_b = kb.ps[5]
              psi, psi_b = kb.ps[4]
              psd, psd_b = kb.ps[3]
              psbf, psbf_b = kb.psbf

              def hankel(src_in, h, c0, pstep):
                  bt, bb = btiles[cnt["bt"] % 3]
                  cnt["bt"] += 1
                  src = bass.AP(tensor=src_in.tensor, offset=h * src_in.shape[1] + c0, ap=[[pstep, 128], [1, 512]])
                  S.dma("sp" if cnt["bt"] % 2 else "act", bt[:], src, [], bb)
                  return bt, bb

              def finish_branch(hq, br, tt, first):
                  g_ = hq % 4
                  tsl = slice(tt * 512, (tt + 1) * 512)
                  S.op("dve", lambda e: e.tensor_scalar(out=rec[0:64, :], in0=psd[0:64, :], scalar1=1e-30,
                                                        scalar2=None, op0=ALU.add), reads=[psd_b], writes=[rec_b])
                  S.op("dve", lambda e: e.reciprocal(out=rec[0:64, :], in_=rec[0:64, :]), reads=[rec_b], writes=[rec_b])
                  gi = hq * 3 + br
                  S.op("pe", lambda e: e.matmul(psg[0:64, :], lhsT=selg[0:64, gi * 64:(gi + 1) * 64], rhs=gT_sb[0:64, tsl],
                                                start=True, stop=True), reads=[selg_b, gT_sbb], writes=[psg_b])
                  S.op("dve", lambda e: e.tensor_tensor(out=fac[0:64, :], in0=psg[0:64, :], in1=rec[0:64, :], op=ALU.mult),
                       reads=[psg_b, rec_b], writes=[fac_b])
                  if first:
                      S.op("dve", lambda e: e.tensor_tensor(out=oacc[:, g_, tsl], in0=pso[0:64, :], in1=fac[0:64, :], op=ALU.mult),
                           reads=[pso_b, fac_b], writes=[oacc_b])
                  else:
                      S.op("dve", lambda e: e.tensor_tensor(out=otmp[0:64, :], in0=pso[0:64, :], in1=fac[0:64, :], op=ALU.mult),
                           reads=[pso_b, fac_b], writes=[otmp_b])
                      S.op("pool", lambda e: e.tensor_tensor(out=oacc[:, g_, tsl], in0=oacc[:, g_, tsl], in1=otmp[0:64, :], op=ALU.add),
                           reads=[otmp_b, oacc_b], writes=[oacc_b])

              for kvh in range(4 if sub >= 2 else 0):
                  for g in range(4):
                      hq = kvh * 4 + g
                      base = 64 * (hq % 2)
                      qc = hq // 2
                      for tt in range(4):
                          tsl = slice(tt * 512, (tt + 1) * 512)
                          pul = []
                          for nch in range(2):
                              c0 = OFFC + (2048 + 512 * tt - 2048 * nch - 2063)
                              bt, bb = hankel(Vc0_in if nch == 0 else Vc_in, hq, c0, 16)
                              pst, psb = kb.psum(3)
                              S.op("pe", lambda e, pst=pst, nch=nch: e.matmul(
                                  pst[:, :], lhsT=kccT[kvh][0][base:base + 64, nch * 128:(nch + 1) * 128],
                                  rhs=qT_sb[base:base + 64, qc, tsl], start=True, stop=True),
                                  reads=[kccT[kvh][1], qT_sbb], writes=[psb])
                              sf, sfb = sfp[cnt["sf"] % 2]
                              cnt["sf"] += 1
                              S.op("dve", lambda e, pst=pst, sf=sf, bt=bt: e.tensor_tensor(out=sf[:], in0=pst[:, :], in1=bt[:], op=ALU.add),
                                   reads=[psb, bb], writes=[sfb])
                              pu, pub = pus[cnt["pu"] % 4]
                              cnt["pu"] += 1
                              S.op("act", lambda e, sf=sf, pu=pu: e.activation(out=pu[:], in_=sf[:], func=AF.Exp),
                                   reads=[sfb], writes=[pub])
                              pul.append((pu, pub))
                          for nch in range(2):
                              S.op("pe", lambda e, nch=nch: e.matmul(pso[0:64, :], lhsT=vcc[kvh][0][:, nch, :], rhs=pul[nch][0][:],
                                                                     start=(nch == 0), stop=(nch == 1)),
                                   reads=[vcc[kvh][1], pul[nch][1]], writes=[pso_b])
                          for nch in range(2):
                              S.op("pe", lambda e, nch=nch: e.matmul(psd[0:64, :], lhsT=ones64[:], rhs=pul[nch][0][:],
                                                                     start=(nch == 0), stop=(nch == 1)),
                                   reads=[ones64_b, pul[nch][1]], writes=[psd_b])
                          finish_branch(hq, 0, tt, True)
                          for tsub in range(4):
                              ts = tt * 4 + tsub
                              for nch in range(2):
                                  S.op("pe", lambda e, nch=nch, tsub=tsub: e.matmul(
                                      psi[:, 0:65], lhsT=pul[nch][0][:, tsub * 128:(tsub + 1) * 128],
                                      rhs=maug[:, nch * 65:(nch + 1) * 65], start=(nch == 0), stop=(nch == 1)),
                                      reads=[pul[nch][1], maug_b], writes=[psi_b])
                              S.op("dve", lambda e: e.tensor_scalar(out=rec2[:], in0=psi[:, 64:65], scalar1=1e-30, scalar2=None,
                                                                    op0=ALU.add), reads=[psi_b], writes=[rec2_b])
                              S.op("dve", lambda e: e.reciprocal(out=rec2[:], in_=rec2[:]), reads=[rec2_b], writes=[rec2_b])
                              if g == 0:
                                  S.op("dve", lambda e, ts=ts: e.tensor_scalar(out=impacc[:, ts, :], in0=psi[:, 0:64], scalar1=rec2[:, 0:1],
                                                                               scalar2=None, op0=ALU.mult),
                                       reads=[psi_b, rec2_b], writes=[impacc_b])
                              else:
                                  S.op("dve", lambda e, ts=ts: e.scalar_tensor_tensor(
                                      out=impacc[:, ts, :], in0=psi[:, 0:64], scalar=rec2[:, 0:1], in1=impacc[:, ts, :],
                                      op0=ALU.mult, op1=ALU.add), reads=[psi_b, rec2_b, impacc_b], writes=[impacc_b])
                  for ts in range(16 if sub >= 3 else 0):
                      S.op("dve", lambda e, ts=ts: e.tensor_tensor(out=simp[:], in0=impacc[:, ts, :], in1=selbias[:, ts * 64:(ts + 1) * 64],
                                                                   op=ALU.add), reads=[impacc_b, selbias_b], writes=[simp_b])
                      S.op("dve", lambda e: e.max(out=m8a[:], in_=simp[:]), reads=[simp_b], writes=[m8a_b])
                      S.op("dve", lambda e: e.match_replace(out=simp2[:], in_to_replace=m8a[:], in_values=simp[:], imm_value=-3.0e38),
                           reads=[simp_b, m8a_b], writes=[simp2_b])
                      S.op("dve", lambda e: e.max(out=m8b[:], in_=simp2[:]), reads=[simp2_b], writes=[m8b_b])
                      for hh in range(2):
                          S.op("dve", lambda e, ts=ts, hh=hh: e.scalar_tensor_tensor(out=nsel[:, hh * 64:(hh + 1) * 64], in0=simp[:], scalar=m8b[:, 7:8],
                                                                                     in1=selinv[:, ts * 64:(ts + 1) * 64], op0=ALU.is_lt, op1=ALU.max),
                               reads=[simp_b, m8b_b, selinv_b], writes=[nsel_b])
                      S.op("pe", lambda e: e.transpose(out=psbf[:, 0:128], in_=nsel[:, :], identity=identb[:]),
                           reads=[nsel_b, identb_b], writes=[psbf_b])
                      S.op("act", lambda e, ts=ts: e.activation(out=notselT[:, ts * 128:(ts + 1) * 128], in_=psbf[:, 0:128], func=AF.Copy),
                           reads=[psbf_b], writes=[notselT_b])
                  for hh in range(2):
                      S.dma("sp", ksT2[hh * 64:(hh + 1) * 64, :], ksT_d[kvh * 64:(kvh + 1) * 64, :], [ksT_b], ksT2_b)
                      S.dma("act", kwT2[hh * 64:(hh + 1) * 64, :], kwT_d[kvh * 64:(kvh + 1) * 64, :], [kwT_b], kwT2_b)
                  if kvh == 0:
                      S.dma("sp", vs_all[:], vs_d.rearrange("(c p) d -> p c d", p=128), [vs_b], vs_all_b)
                      S.dma("act", vw_all[:], vw_d.rearrange("(c p) d -> p c d", p=128), [vw_b], vw_all_b)
                  for g in range(4 if sub >= 4 else 0):
                      hq = kvh * 4 + g
                      base = 64 * (hq % 2)
                      qc = hq // 2
                      for br in (1, 2):
                          kT, kTb = (ksT2, ksT2_b) if br == 1 else (kwT2, kwT2_b)
                          va, vab = (vs_all, vs_all_b) if br == 1 else (vw_all, vw_all_b)
                          for tt in range(4):
                              tsl = slice(tt * 512, (tt + 1) * 512)
                              sc_hi = 16 + 4 * tt + 3
                              sc_lo = 0 if br == 1 else 16 + 4 * tt - 4
                              for sc in range(sc_lo, sc_hi + 1):
                                  r = sc - (16 + 4 * tt)
                                  near = (r >= -1) if br == 1 else True
                                  pst, psb = kb.psum(3)
                                  S.op("pe", lambda e, pst=pst, sc=sc: e.matmul(
                                      pst[:, :], lhsT=kT[base:base + 64, sc * 128:(sc + 1) * 128], rhs=qT_sb[base:base + 64, qc, tsl],
                                      start=True, stop=(br == 2)), reads=[kTb, qT_sbb], writes=[psb])
                                  if br == 1:
                                      S.op("pe", lambda e, pst=pst, sc=sc: e.matmul(
                                          pst[:, :], lhsT=negE[base:base + 64, sc * 128:(sc + 1) * 128], rhs=notselT[base:base + 64, tsl],
                                          start=False, stop=True), reads=[negE_b, notselT_b], writes=[psb])
                                  pu, pub = pus[cnt["pu"] % 4]
                                  cnt["pu"] += 1
                                  if near:
                                      if br == 1:
                                          vin = Vb_in
                                      elif r >= 0:
                                          vin = Vb_in
                                      else:
                                          vin = Vwc_in if (tt == 0) else Vw_in
                                      c0 = OFFB + (2048 + 512 * tt - 128 * sc - 127)
                                      bt, bb = hankel(vin, hq, c0, 1)
                                      sf, sfb = sfp[cnt["sf"] % 2]
                                      cnt["sf"] += 1
                                      S.op("dve", lambda e, pst=pst, sf=sf, bt=bt: e.tensor_tensor(out=sf[:], in0=pst[:, :], in1=bt[:], op=ALU.add),
                                           reads=[psb, bb], writes=[sfb])
                                      S.op("act", lambda e, sf=sf, pu=pu: e.activation(out=pu[:], in_=sf[:], func=AF.Exp),
                                           reads=[sfb], writes=[pub])
                                  else:
                                      S.op("act", lambda e, pst=pst, pu=pu: e.activation(out=pu[:], in_=pst[:, :], func=AF.Exp,
                                                                                         bias=rb31[:, hq:hq + 1], scale=1.0),
                                           reads=[psb, rb31_b], writes=[pub])
                                  S.op("pe", lambda e, sc=sc, pu=pu: e.matmul(pso[0:64, :], lhsT=va[:, sc, kvh * 64:(kvh + 1) * 64], rhs=pu[:],
                                                                              start=(sc == sc_lo), stop=(sc == sc_hi)),
                                       reads=[vab, pub], writes=[pso_b])
                                  S.op("pe", lambda e, sc=sc, pu=pu: e.matmul(psd[0:64, :], lhsT=ones64[:], rhs=pu[:],
                                                                              start=(sc == sc_lo), stop=(sc == sc_hi)),
                                       reads=[ones64_b, pub], writes=[psd_b])
                              finish_branch(hq, br, tt, False)
                  S.op("act", lambda e: e.activation(out=obf[:], in_=oacc[:], func=AF.Copy), reads=[oacc_b], writes=[obf_b])
                  S.dma("sp", oT_d[kvh * 256:(kvh + 1) * 256, :].rearrange("(g p) t -> p g t", p=64), obf[:], [obf_b], oT_b)
              kb.end(ph)
          except _Stop:
              kb.end(ph)


    y2T_d, y2T_b = kb.dscratch("y2T_d", [1024, TOWN], BF16)
    TWO_PI = 2.0 * math.pi
    if stages >= 4 and 4 not in skip:
        lre_f = kb.din("lre_f", [1, 4096]); lim_f = kb.din("lim_f", [1, 4096]); lst_f = kb.din("lst_f", [1, 4096])
        lreT_in = kb.din("lreT", [128, 32]); limT_in = kb.din("limT", [128, 32]); lstT_in = kb.din("lstT", [128, 32])
        Bpre_in = kb.din("Bpad_re", [128, 4096]); Bpim_in = kb.din("Bpad_im", [128, 4096])
        Cpre_in = kb.din("Cpad_re", [128, 4096]); Cpim_in = kb.din("Cpad_im", [128, 4096])
        tau_in = kb.din("tau", [1, 512])
        s5dT_in = kb.din("s5dT", [128, 8])
        glu_w_in = kb.din("glu_w", [1024, 1024]); glubT_in = kb.din("glubT", [128, 8])
        with contextlib.ExitStack() as ph:
            Wre, Wre_b = kb.sb(ph, "Wre", [128, 4096], BF16)
            Wim, Wim_b = kb.sb(ph, "Wim", [128, 4096], BF16)
            Cre, Cre_b = kb.sb(ph, "Cre", [128, 4096], BF16, dma=True)
            Cim, Cim_b = kb.sb(ph, "Cim", [128, 4096], BF16, dma=True)
            S.dma("pool", Cre[:].rearrange("p (a b) -> p a b", b=1024), Cpre_in.rearrange("p (a b) -> p a b", b=1024), [], Cre_b)
            S.dma("pool", Cim[:].rearrange("p (a b) -> p a b", b=1024), Cpim_in.rearrange("p (a b) -> p a b", b=1024), [], Cim_b)
            tau, tau_b = kb.sb(ph, "tau_sb", [128, 512], F32, dma=True)
            S.dma("sp", tau[:], tau_in[0:1, :].partition_broadcast(128), [], tau_b)
            thT, thT_b = kb.sb(ph, "thT", [128, 32], F32)
            rT, rT_b = kb.sb(ph, "rT", [128, 32], F32)
            cQ, cQ_b = kb.sb(ph, "cQ", [128, 32], F32)
            sQ, sQ_b = kb.sb(ph, "sQ", [128, 32], F32)
            nsQ, nsQ_b = kb.sb(ph, "nsQ", [128, 32], F32)
            s5d, s5d_b = kb.sb(ph, "s5d", [128, 8], F32, dma=True)
            glub, glub_b = kb.sb(ph, "glub", [128, 8], F32, dma=True)
            S.dma("sp", s5d[:], s5dT_in[:, :], [], s5d_b)
            S.dma("sp", glub[:], glubT_in[:, :], [], glub_b)

            sc_cache = {}

            def sincos(ctx, A, Ab, N, nm):
                if nm not in sc_cache:
                    sc_cache[nm] = [kb.sb(ctx, nm + "ki", [128, N], mybir.dt.int32), kb.sb(ctx, nm + "kf", [128, N], F32),
                                    kb.sb(ctx, nm + "r", [128, N], F32), kb.sb(ctx, nm + "m", [128, N], F32),
                                    kb.sb(ctx, nm + "S", [128, N], F32), kb.sb(ctx, nm + "C", [128, N], F32)]
                (ki, ki_b), (kf, kf_b), (r, r_b), (mm, mm_b), (Sx, Sx_b), (Cx, Cx_b) = sc_cache[nm]
                S.op("dve", lambda e: e.tensor_scalar(out=ki[:], in0=A, scalar1=1.0 / TWO_PI, scalar2=None, op0=ALU.mult),
                     reads=[Ab], writes=[ki_b])
                S.op("dve", lambda e: e.tensor_copy(out=kf[:], in_=ki[:]), reads=[ki_b], writes=[kf_b])
                S.op("dve", lambda e: e.scalar_tensor_tensor(out=r[:], in0=kf[:], scalar=-TWO_PI, in1=A, op0=ALU.mult, op1=ALU.add),
                     reads=[kf_b, Ab], writes=[r_b])
                S.op("dve", lambda e: e.tensor_scalar(out=mm[:], in0=r[:], scalar1=math.pi, scalar2=-TWO_PI, op0=ALU.is_gt, op1=ALU.mult),
                     reads=[r_b], writes=[mm_b])
                S.op("dve", lambda e: e.tensor_tensor(out=r[:], in0=r[:], in1=mm[:], op=ALU.add), reads=[r_b, mm_b], writes=[r_b])
                S.op("dve", lambda e: e.tensor_scalar(out=mm[:], in0=r[:], scalar1=-math.pi, scalar2=TWO_PI, op0=ALU.is_lt, op1=ALU.mult),
                     reads=[r_b], writes=[mm_b])
                S.op("dve", lambda e: e.tensor_tensor(out=r[:], in0=r[:], in1=mm[:], op=ALU.add), reads=[r_b, mm_b], writes=[r_b])
                S.op("act", lambda e: e.activation(out=Sx[:], in_=r[:], func=AF.Sin), reads=[r_b], writes=[Sx_b])
                S.op("act", lambda e: e.activation(out=mm[:], in_=r[:], func=AF.Abs), reads=[r_b], writes=[mm_b])
                S.op("act", lambda e: e.activation(out=Cx[:], in_=mm[:], func=AF.Sin, scale=-1.0, bias=halfpi[:, 0:1]),
                     reads=[mm_b, halfpi_b], writes=[Cx_b])
                return Sx, Sx_b, Cx, Cx_b

            halfpi, halfpi_b = kb.sb(ph, "halfpi", [128, 1], F32)
            S.op("dve", lambda e: e.memset(halfpi[:], math.pi / 2), writes=[halfpi_b])

            with contextlib.ExitStack() as pp:
                lreT, lreT_b = kb.sb(pp, "lreT_sb", [128, 32], F32, dma=True)
                limT, limT_b = kb.sb(pp, "limT_sb", [128, 32], F32, dma=True)
                lstT, lstT_b = kb.sb(pp, "lstT_sb", [128, 32], F32, dma=True)
                aQ, aQ_b = kb.sb(pp, "aQ", [128, 32], F32)
                S.dma("sp", lreT[:], lreT_in[:, :], [], lreT_b)
                S.dma("sp", limT[:], limT_in[:, :], [], limT_b)
                S.dma("sp", lstT[:], lstT_in[:, :], [], lstT_b)
                S.op("act", lambda e: e.activation(out=lstT[:], in_=lstT[:], func=AF.Exp), reads=[lstT_b], writes=[lstT_b])
                S.op("dve", lambda e: e.tensor_tensor(out=thT[:], in0=limT[:], in1=lstT[:], op=ALU.mult), reads=[limT_b, lstT_b], writes=[thT_b])
                S.op("dve", lambda e: e.tensor_tensor(out=rT[:], in0=lreT[:], in1=lstT[:], op=ALU.mult), reads=[lreT_b, lstT_b], writes=[rT_b])
                S.op("act", lambda e: e.activation(out=rT[:], in_=rT[:], func=AF.Exp), reads=[rT_b], writes=[rT_b])
                S.op("dve", lambda e: e.tensor_scalar(out=aQ[:], in0=thT[:], scalar1=512.0, scalar2=None, op0=ALU.mult), reads=[thT_b], writes=[aQ_b])
                Sx, Sx_b, Cx, Cx_b = sincos(pp, aQ[:], aQ_b, 32, "q_")
                S.op("dve", lambda e: e.tensor_copy(out=sQ[:], in_=Sx[:]), reads=[Sx_b], writes=[sQ_b])
                S.op("dve", lambda e: e.tensor_copy(out=cQ[:], in_=Cx[:]), reads=[Cx_b], writes=[cQ_b])
                S.op("dve", lambda e: e.tensor_scalar(out=nsQ[:], in0=Sx[:], scalar1=-1.0, scalar2=None, op0=ALU.mult), reads=[Sx_b], writes=[nsQ_b])
                kb.end(pp)

            for pg in range(4):
                with contextlib.ExitStack() as pp:
                    NN = 1024
                    csl = slice(pg * NN, (pg + 1) * NN)
                    def T(nm, dma=False):
                        return kb.sb(pp, f"{nm}{pg}", [128, NN], F32, dma=dma)
                    lre, lre_b = T("lre", True); lim, lim_b = T("lim", True); st, st_b = T("st", True)
                    S.dma("sp", lre[:], lre_f[0:1, csl].partition_broadcast(128), [], lre_b)
                    S.dma("sp", lim[:], lim_f[0:1, csl].partition_broadcast(128), [], lim_b)
                    S.dma("sp", st[:], lst_f[0:1, csl].partition_broadcast(128), [], st_b)
                    bre, bre_b = T("bpre", True); bim, bim_b = T("bpim", True)
                    S.dma("act", bre[:], Bpre_in[:, csl], [], bre_b)
                    S.dma("act", bim[:], Bpim_in[:, csl], [], bim_b)
                    ar, ar_b = T("ar"); ai, ai_b = T("ai"); mag, mag_b = T("mag")
                    S.op("act", lambda e: e.activation(out=st[:], in_=st[:], func=AF.Exp), reads=[st_b], writes=[st_b])
                    S.op("dve", lambda e: e.tensor_tensor(out=ar[:], in0=lre[:], in1=st[:], op=ALU.mult), reads=[lre_b, st_b], writes=[ar_b])
                    S.op("dve", lambda e: e.tensor_tensor(out=ai[:], in0=lim[:], in1=st[:], op=ALU.mult), reads=[lim_b, st_b], writes=[ai_b])
                    S.op("act", lambda e: e.activation(out=mag[:], in_=ar[:], func=AF.Exp), reads=[ar_b], writes=[mag_b])
                    Sx, Sx_b, Cx, Cx_b = sincos(pp, ai[:], ai_b, NN, f"p{pg}_")
                    S.op("dve", lambda e: e.tensor_tensor(out=Cx[:], in0=Cx[:], in1=mag[:], op=ALU.mult), reads=[Cx_b, mag_b], writes=[Cx_b])
                    S.op("dve", lambda e: e.tensor_tensor(out=Sx[:], in0=Sx[:], in1=mag[:], op=ALU.mult), reads=[Sx_b, mag_b], writes=[Sx_b])
                    S.op("dve", lambda e: e.tensor_scalar(out=Cx[:], in0=Cx[:], scalar1=-1.0, scalar2=None, op0=ALU.add), reads=[Cx_b], writes=[Cx_b])
                    S.op("dve", lambda e: e.tensor_tensor(out=mag[:], in0=lre[:], in1=lre[:], op=ALU.mult), reads=[lre_b], writes=[mag_b])
                    S.op("dve", lambda e: e.tensor_tensor(out=ar[:], in0=lim[:], in1=lim[:], op=ALU.mult), reads=[lim_b], writes=[ar_b])
                    S.op("dve", lambda e: e.tensor_tensor(out=mag[:], in0=mag[:], in1=ar[:], op=ALU.add), reads=[mag_b, ar_b], writes=[mag_b])
                    S.op("dve", lambda e: e.reciprocal(out=mag[:], in_=mag[:]), reads=[mag_b], writes=[mag_b])
                    S.op("dve", lambda e: e.tensor_tensor(out=ar[:], in0=Cx[:], in1=lre[:], op=ALU.mult), reads=[Cx_b, lre_b], writes=[ar_b])
                    S.op("dve", lambda e: e.tensor_tensor(out=st[:], in0=Sx[:], in1=lim[:], op=ALU.mult), reads=[Sx_b, lim_b], writes=[st_b])
                    S.op("dve", lambda e: e.tensor_tensor(out=ar[:], in0=ar[:], in1=st[:], op=ALU.add), reads=[ar_b, st_b], writes=[ar_b])
                    S.op("dve", lambda e: e.tensor_tensor(out=ar[:], in0=ar[:], in1=mag[:], op=ALU.mult), reads=[ar_b, mag_b], writes=[ar_b])
                    S.op("dve", lambda e: e.tensor_tensor(out=ai[:], in0=Sx[:], in1=lre[:], op=ALU.mult), reads=[Sx_b, lre_b], writes=[ai_b])
                    S.op("dve", lambda e: e.tensor_tensor(out=st[:], in0=Cx[:], in1=lim[:], op=ALU.mult), reads=[Cx_b, lim_b], writes=[st_b])
                    S.op("dve", lambda e: e.tensor_tensor(out=ai[:], in0=ai[:], in1=st[:], op=ALU.subtract), reads=[ai_b, st_b], writes=[ai_b])
                    S.op("dve", lambda e: e.tensor_tensor(out=ai[:], in0=ai[:], in1=mag[:], op=ALU.mult), reads=[ai_b, mag_b], writes=[ai_b])
                    S.op("dve", lambda e: e.tensor_tensor(out=st[:], in0=ar[:], in1=bre[:], op=ALU.mult), reads=[ar_b, bre_b], writes=[st_b])
                    S.op("dve", lambda e: e.tensor_tensor(out=mag[:], in0=ai[:], in1=bim[:], op=ALU.mult), reads=[ai_b, bim_b], writes=[mag_b])
                    S.op("dve", lambda e: e.tensor_tensor(out=Wre[:, csl], in0=st[:], in1=mag[:], op=ALU.subtract), reads=[st_b, mag_b], writes=[Wre_b])
                    S.op("dve", lambda e: e.tensor_tensor(out=st[:], in0=ar[:], in1=bim[:], op=ALU.mult), reads=[ar_b, bim_b], writes=[st_b])
                    S.op("dve", lambda e: e.tensor_tensor(out=mag[:], in0=ai[:], in1=bre[:], op=ALU.mult), reads=[ai_b, bre_b], writes=[mag_b])
                    S.op("dve", lambda e: e.tensor_tensor(out=Wim[:, csl], in0=st[:], in1=mag[:], op=ALU.add), reads=[st_b, mag_b], writes=[Wim_b])
                    kb.end(pp)

            ang, ang_b = kb.sb(ph, "ang", [128, 512], F32)
            rt, rt_b = kb.sb(ph, "rt", [128, 512], F32)
            ones512, ones512_b = kb.sb(ph, "ones512", [128, 512], F32)
            S.op("dve", lambda e: e.memset(ones512[:], 1.0), writes=[ones512_b])
            uTs = [kb.sb(ph, f"uTs{i}", [128, TALL], BF16, dma=True) for i in range(2)]
            xre, xre_b = kb.sb(ph, "xre", [128, 4, TOWN], BF16)
            nxim, nxim_b = kb.sb(ph, "nxim", [128, 4, TOWN], BF16)
            yg, yg_b = kb.sb(ph, "yg", [128, 8, TOWN], BF16)
            wk = {n: kb.sb(ph, "wk_" + n, [128, 512], F32) for n in ("t1", "t2", "t3", "t4", "bre", "bim", "o1", "o2", "o3", "o4", "ysb")}
            wre2 = [kb.sb(ph, f"wre{i}", [128, 512], F32) for i in range(2)]
            wim2 = [kb.sb(ph, f"wim{i}", [128, 512], F32) for i in range(2)]
            ini_re, ini_re_b = kb.sb(ph, "ini_re", [128, 1], F32)
            ini_im, ini_im_b = kb.sb(ph, "ini_im", [128, 1], F32)
            itmp, itmp_b = kb.sb(ph, "itmp", [128, 2], F32)
            with contextlib.ExitStack() as sc_ctx:
                for cc in range(8):
                    uT, uT_sbb = uTs[cc % 2]
                    S.dma("sp", uT[:], uT_d[cc * 128:(cc + 1) * 128, :], [uT_b], uT_sbb)
                    for pl in range(4):
                        pr = cc * 4 + pl
                        if True:
                            pctx = ph
                            S.op("dve", lambda e, pr=pr: e.tensor_scalar(out=ang[:], in0=tau[:], scalar1=thT[:, pr:pr + 1], scalar2=None, op0=ALU.mult),
                                 reads=[tau_b, thT_b], writes=[ang_b])
                            Sx, Sx_b, Cx, Cx_b = sincos(pctx, ang[:], ang_b, 512, "tmain_")
                            S.op("dve", lambda e, pr=pr: e.tensor_scalar(out=rt[:], in0=ones512[:], scalar1=rT[:, pr:pr + 1], scalar2=None, op0=ALU.mult),
                                 reads=[ones512_b, rT_b], writes=[rt_b])
                            for j in range(8):
                                ps_re, ps_re_b = kb.psum()
                                ps_im, ps_im_b = kb.psum()
                                S.op("pe", lambda e, ps_re=ps_re, j=j, pr=pr: e.matmul(
                                    ps_re[:, :], lhsT=Wre[:, pr * 128:(pr + 1) * 128], rhs=uT[:, j * 512:(j + 1) * 512], start=True, stop=True),
                                    reads=[Wre_b, uT_sbb], writes=[ps_re_b])
                                S.op("pe", lambda e, ps_im=ps_im, j=j, pr=pr: e.matmul(
                                    ps_im[:, :], lhsT=Wim[:, pr * 128:(pr + 1) * 128], rhs=uT[:, j * 512:(j + 1) * 512], start=True, stop=True),
                                    reads=[Wim_b, uT_sbb], writes=[ps_im_b])
                                t1, t1b = wk["t1"]; t2, t2b = wk["t2"]; t3, t3b = wk["t3"]; t4, t4b = wk["t4"]
                                bre_, breb = wk["bre"]; bim_, bimb = wk["bim"]
                                S.op("dve", lambda e, ps_re=ps_re: e.tensor_tensor(out=t1[:], in0=ps_re[:, :], in1=Cx[:], op=ALU.mult), reads=[ps_re_b, Cx_b], writes=[t1b])
                                S.op("dve", lambda e, ps_im=ps_im: e.tensor_tensor(out=t2[:], in0=ps_im[:, :], in1=Sx[:], op=ALU.mult), reads=[ps_im_b, Sx_b], writes=[t2b])
                                S.op("pool", lambda e: e.tensor_tensor(out=bre_[:], in0=t1[:], in1=t2[:], op=ALU.add), reads=[t1b, t2b], writes=[breb])
                                S.op("dve", lambda e, ps_im=ps_im: e.tensor_tensor(out=t3[:], in0=ps_im[:, :], in1=Cx[:], op=ALU.mult), reads=[ps_im_b, Cx_b], writes=[t3b])
                                S.op("dve", lambda e, ps_re=ps_re: e.tensor_tensor(out=t4[:], in0=ps_re[:, :], in1=Sx[:], op=ALU.mult), reads=[ps_re_b, Sx_b], writes=[t4b])
                                S.op("pool", lambda e: e.tensor_tensor(out=bim_[:], in0=t3[:], in1=t4[:], op=ALU.subtract), reads=[t3b, t4b], writes=[bimb])
                                wre, wreb = wre2[j % 2]
                                wim, wimb = wim2[j % 2]
                                if j == 0:
                                    S.op("dve", lambda e, wre=wre: e.tensor_tensor_scan(out=wre[:], data0=rt[:], data1=bre_[:], initial=0.0, op0=ALU.mult, op1=ALU.add),
                                         reads=[rt_b, breb], writes=[wreb])
                                    S.op("dve", lambda e, wim=wim: e.tensor_tensor_scan(out=wim[:], data0=rt[:], data1=bim_[:], initial=0.0, op0=ALU.mult, op1=ALU.add),
                                         reads=[rt_b, bimb], writes=[wimb])
                                else:
                                    S.op("dve", lambda e, wre=wre: e.tensor_tensor_scan(out=wre[:], data0=rt[:], data1=bre_[:], initial=ini_re[:, 0:1], op0=ALU.mult, op1=ALU.add),
                                         reads=[rt_b, breb, ini_re_b], writes=[wreb])
                                    S.op("dve", lambda e, wim=wim: e.tensor_tensor_scan(out=wim[:], data0=rt[:], data1=bim_[:], initial=ini_im[:, 0:1], op0=ALU.mult, op1=ALU.add),
                                         reads=[rt_b, bimb, ini_im_b], writes=[wimb])
                                if j < 7:
                                    S.op("dve", lambda e, wre=wre, pr=pr: e.tensor_scalar(out=itmp[:, 0:1], in0=wre[:, 511:512], scalar1=cQ[:, pr:pr + 1], scalar2=None, op0=ALU.mult),
                                         reads=[wreb, cQ_b], writes=[itmp_b])
                                    S.op("dve", lambda e, wre=wre, pr=pr: e.tensor_scalar(out=itmp[:, 1:2], in0=wre[:, 511:512], scalar1=sQ[:, pr:pr + 1], scalar2=None, op0=ALU.mult),
                                         reads=[wreb, sQ_b], writes=[itmp_b])
                                    S.op("dve", lambda e, wim=wim, pr=pr: e.scalar_tensor_tensor(out=ini_re[:], in0=wim[:, 511:512], scalar=nsQ[:, pr:pr + 1], in1=itmp[:, 0:1], op0=ALU.mult, op1=ALU.add),
                                         reads=[wimb, nsQ_b, itmp_b], writes=[ini_re_b])
                                    S.op("dve", lambda e, wim=wim, pr=pr: e.scalar_tensor_tensor(out=ini_im[:], in0=wim[:, 511:512], scalar=cQ[:, pr:pr + 1], in1=itmp[:, 1:2], op0=ALU.mult, op1=ALU.add),
                                         reads=[wimb, cQ_b, itmp_b], writes=[ini_im_b])
                                if j >= 4:
                                    tsl = slice((j - 4) * 512, (j - 3) * 512)
                                    o1, o1b = wk["o1"]; o2, o2b = wk["o2"]; o3, o3b = wk["o3"]; o4, o4b = wk["o4"]
                                    S.op("dve", lambda e, wre=wre: e.tensor_tensor(out=o1[:], in0=wre[:], in1=Cx[:], op=ALU.mult), reads=[wreb, Cx_b], writes=[o1b])
                                    S.op("pool", lambda e, wim=wim: e.tensor_tensor(out=o2[:], in0=wim[:], in1=Sx[:], op=ALU.mult), reads=[wimb, Sx_b], writes=[o2b])
                                    S.op("pool", lambda e, tsl=tsl, pl=pl: e.tensor_tensor(out=xre[:, pl, tsl], in0=o1[:], in1=o2[:], op=ALU.subtract), reads=[o1b, o2b], writes=[xre_b])
                                    S.op("dve", lambda e, wre=wre: e.tensor_tensor(out=o3[:], in0=wre[:], in1=Sx[:], op=ALU.mult), reads=[wreb, Sx_b], writes=[o3b])
                                    S.op("pool", lambda e, wim=wim: e.tensor_tensor(out=o4[:], in0=wim[:], in1=Cx[:], op=ALU.mult), reads=[wimb, Cx_b], writes=[o4b])
                                    S.op("dve", lambda e, tsl=tsl, pl=pl: e.scalar_tensor_tensor(out=nxim[:, pl, tsl], in0=o3[:], scalar=-1.0, in1=o4[:], op0=ALU.mult, op1=ALU.subtract),
                                         reads=[o3b, o4b], writes=[nxim_b])
                    for tt in range(4):
                        tsl = slice(tt * 512, (tt + 1) * 512)
                        pst, psb = kb.psum()
                        for pl in range(4):
                            pr = cc * 4 + pl
                            S.op("pe", lambda e, pl=pl, pr=pr, pst=pst, tsl=tsl: e.matmul(pst[:, :], lhsT=Cre[:, pr * 128:(pr + 1) * 128], rhs=xre[:, pl, tsl],
                                                                                         start=(pl == 0), stop=False), reads=[Cre_b, xre_b], writes=[psb])
                            S.op("pe", lambda e, pl=pl, pr=pr, pst=pst, tsl=tsl: e.matmul(pst[:, :], lhsT=Cim[:, pr * 128:(pr + 1) * 128], rhs=nxim[:, pl, tsl],
                                                                                         start=False, stop=(pl == 3)), reads=[Cim_b, nxim_b], writes=[psb])
                        ysb, ysbb = wk["ysb"]
                        S.op("dve", lambda e, pst=pst, tt=tt, cc=cc, uT=uT: e.scalar_tensor_tensor(
                            out=ysb[:], in0=uT[:, TALL - TOWN + tt * 512:TALL - TOWN + (tt + 1) * 512], scalar=s5d[:, cc:cc + 1], in1=pst[:, :],
                            op0=ALU.mult, op1=ALU.add), reads=[uT_sbb, s5d_b, psb], writes=[ysbb])
                        S.op("act", lambda e, cc=cc, tsl=tsl: e.activation(out=yg[:, cc, tsl], in_=ysb[:], func=AF.Gelu_apprx_tanh),
                             reads=[ysbb], writes=[yg_b])
            S.barrier()
            with contextlib.ExitStack() as pg_:
                gw, gw_b = kb.sb(pg_, "gw", [128, 8, 1024], BF16, dma=True)
                S.dma("pool", gw[:], glu_w_in.rearrange("(c p) o -> p c o", p=128), [], gw_b)
                sg = [kb.sb(pg_, f"sg{i}", [128, 512], BF16) for i in range(2)]
                y2 = [kb.sb(pg_, f"y2_{i}", [128, 512], BF16) for i in range(2)]
                n = 0
                for co in range(8):
                    for tt in range(4):
                        tsl = slice(tt * 512, (tt + 1) * 512)
                        pst, psb = kb.psum()
                        for cc in range(8):
                            S.op("pe", lambda e, cc=cc, co=co, pst=pst, tsl=tsl: e.matmul(pst[:, :], lhsT=gw[:, cc, co * 128:(co + 1) * 128], rhs=yg[:, cc, tsl],
                                                                                         start=(cc == 0), stop=(cc == 7)), reads=[gw_b, yg_b], writes=[psb])
                        sgt, sgb = sg[n % 2]
                        y2t, y2b = y2[n % 2]
                        n += 1
                        S.op("act", lambda e, pst=pst, sgt=sgt, co=co: e.activation(out=sgt[:], in_=pst[:, :], func=AF.Sigmoid, bias=glub[:, co:co + 1], scale=1.0),
                             reads=[psb, glub_b], writes=[sgb])
                        S.op("dve", lambda e, sgt=sgt, y2t=y2t, co=co, tsl=tsl: e.tensor_tensor(out=y2t[:], in0=sgt[:], in1=yg[:, co, tsl], op=ALU.mult),
                             reads=[sgb, yg_b], writes=[y2b])
                        S.dma("sp", y2T_d[co * 128:(co + 1) * 128, tsl], y2t[:], [y2b], y2T_b)
                kb.end(pg_)
            kb.end(ph)


    x1T_d, x1T_b = kb.dscratch("x1T_d", [D, TOWN], F32)
    x2T_d, x2T_b = kb.dscratch("x2T_d", [D, TOWN], F32)
    if stages >= 5 and 5 not in skip:
        wua_in = kb.din("w_up_attn", [1024, D]); wus_in = kb.din("w_up_ssm", [1024, D]); wout_in = kb.din("w_out", [D, D])
        with contextlib.ExitStack() as ph:
            wua, wua_b = kb.sb(ph, "wua", [128, 8, D], BF16, dma=True)
            wus, wus_b = kb.sb(ph, "wus", [128, 8, D], BF16, dma=True)
            for c4 in range(4):
                S.dma("pool", wua[:, :, c4 * 512:(c4 + 1) * 512], wua_in.rearrange("(c p) o -> p c o", p=128)[:, :, c4 * 512:(c4 + 1) * 512], [], wua_b)
                S.dma("pool", wus[:, :, c4 * 512:(c4 + 1) * 512], wus_in.rearrange("(c p) o -> p c o", p=128)[:, :, c4 * 512:(c4 + 1) * 512], [], wus_b)
            wo2 = [kb.sb(ph, f"wo{i}", [128, KC, 128], BF16, dma=True) for i in range(2)]
            gts, gts_b = kb.sb(ph, "gts", [128, 32, 512], BF16, dma=True)
            oTt, oTt_b = kb.sb(ph, "oTt", [128, 8, 512], BF16, dma=True)
            yTt, yTt_b = kb.sb(ph, "yTt", [128, 8, 512], BF16, dma=True)
            mixed, mixed_b = kb.sb(ph, "mixed", [128, KC, 512], BF16)
            xt2 = [kb.sb(ph, f"xres{i}", [128, 512], F32, dma=True) for i in range(2)]
            x1t2 = [kb.sb(ph, f"x1t{i}", [128, 512], F32) for i in range(2)]
            mt1 = [kb.sb(ph, f"mt1_{i}", [128, 512], F32) for i in range(2)]
            mt2 = [kb.sb(ph, f"mt2_{i}", [128, 512], F32) for i in range(2)]
            woutv = wout_in.rearrange("(c p) o -> p c o", p=128)
            xTv_own = xT.rearrange("(k p) t -> p k t", p=128)
            n = 0
            for tt in range(4):
                tsl = slice(tt * 512, (tt + 1) * 512)
                S.dma("sp", gts[:], mgT_d.rearrange("(c p) t -> p c t", p=128)[:, :, tsl], [mgT_b], gts_b)
                S.dma("sp", oTt[:], oT_d.rearrange("(c p) t -> p c t", p=128)[:, :, tsl], [oT_b], oTt_b)
                S.dma("act", yTt[:], y2T_d.rearrange("(c p) t -> p c t", p=128)[:, :, tsl], [y2T_b], yTt_b)
                for dm in range(KC):
                    psA, psA_b = kb.psum()
                    psB, psB_b = kb.psum()
                    for cc in range(8):
                        S.op("pe", lambda e, cc=cc, dm=dm, psA=psA: e.matmul(psA[:, :], lhsT=wua[:, cc, dm * 128:(dm + 1) * 128], rhs=oTt[:, cc, :],
                                                                           start=(cc == 0), stop=(cc == 7)), reads=[wua_b, oTt_b], writes=[psA_b])
                    for cc in range(8):
                        S.op("pe", lambda e, cc=cc, dm=dm, psB=psB: e.matmul(psB[:, :], lhsT=wus[:, cc, dm * 128:(dm + 1) * 128], rhs=yTt[:, cc, :],
                                                                           start=(cc == 0), stop=(cc == 7)), reads=[wus_b, yTt_b], writes=[psB_b])
                    a1, a1b = mt1[dm % 2]
                    a2, a2b = mt2[dm % 2]
                    S.op("dve", lambda e, psA=psA, a1=a1, dm=dm: e.tensor_tensor(out=a1[:], in0=psA[:, :], in1=gts[:, dm, :], op=ALU.mult),
                         reads=[psA_b, gts_b], writes=[a1b])
                    S.op("dve", lambda e, psB=psB, a2=a2, dm=dm: e.tensor_tensor(out=a2[:], in0=psB[:, :], in1=gts[:, 16 + dm, :], op=ALU.mult),
                         reads=[psB_b, gts_b], writes=[a2b])
                    S.op("pool", lambda e, a1=a1, a2=a2, dm=dm: e.tensor_tensor(out=mixed[:, dm, :], in0=a1[:], in1=a2[:], op=ALU.add),
                         reads=[a1b, a2b], writes=[mixed_b])
                for do in range(KC):
                    wo, wo_b = wo2[n % 2]
                    xr, xr_b = xt2[n % 2]
                    x1t, x1t_b = x1t2[n % 2]
                    n += 1
                    S.dma("pool", wo[:], woutv[:, :, do * 128:(do + 1) * 128], [], wo_b)
                    S.dma("act", xr[:], xTv_own[:, do, TALL - TOWN + tt * 512:TALL - TOWN + (tt + 1) * 512], [], xr_b)
                    pst, psb = kb.psum()
                    for dm in range(KC):
                        S.op("pe", lambda e, dm=dm, pst=pst, wo=wo: e.matmul(pst[:, :], lhsT=wo[:, dm, :], rhs=mixed[:, dm, :],
                                                                           start=(dm == 0), stop=(dm == KC - 1)), reads=[wo_b, mixed_b], writes=[psb])
                    S.op("dve", lambda e, pst=pst, do=do, xr=xr, x1t=x1t: e.scalar_tensor_tensor(
                        out=x1t[:], in0=pst[:, :], scalar=mod[:, G1 + do:G1 + do + 1], in1=xr[:], op0=ALU.mult, op1=ALU.add),
                        reads=[psb, mod_b, xr_b], writes=[x1t_b])
                    S.dma("sp", x1T_d[do * 128:(do + 1) * 128, tsl], x1t[:], [x1t_b], x1T_b)
            kb.end(ph)

    x2src_d, x2src_b = (x1T_d, x1T_b)
    if stages >= 6 and 6 not in skip:
        x2src_d, x2src_b = (x2T_d, x2T_b)
        wq_in = kb.din("peer_w_q", [D, D])
        keysT_in = kb.din("keysT", [128, 16 * 128])
        uT_in = kb.din("peer_uT", [D, 16384])
        v_in = kb.din("peer_v", [16384, D])
        h2T_d, h2T_b = kb.dscratch("h2T_d", [D, TOWN], BF16)
        qpT_d, qpT_b = kb.dscratch("qpT_d", [D, TOWN], BF16)
        G_d, G_b = kb.dscratch("G_d", [16, 128, 16384], BF16)
        uTb_d, uTb_b = kb.dscratch("uTb_d", [32, 128, KC * 512], BF16)
        vb_d, vb_b = kb.dscratch("vb_d", [32, 128, 4 * D], BF16)
        with contextlib.ExitStack() as ph:
            TB = 1024
            hT, hT_b = kb.sb(ph, "p_hT", [128, KC, TB], BF16)
            xbufs = [kb.sb(ph, f"p_xb{i}", [128, KC, 256], F32, dma=True) for i in range(2)]
            tmpbufs = [kb.sb(ph, f"p_ntmp{i}", [128, 256], F32) for i in range(2)]
            sqbuf = kb.sb(ph, "p_sqb", [128, KC, 256], BF16)
            rbuf = kb.sb(ph, "p_rstd", [128, 256], F32)
            wbufs = [kb.sb(ph, f"p_wbuf{i}", [128, KC, 512], BF16, dma=True) for i in range(2)]
            obufs = [kb.sb(ph, f"p_obuf{i}", [128, 512], BF16) for i in range(4)]
            x1v = x1T_d.rearrange("(k p) t -> p k t", p=128)
            wqv = wq_in.rearrange("(k p) c -> p k c", p=128)
            n = 0
            for blk in range(TOWN // TB):
                tb0 = blk * TB

                def src_fn(t, nn):
                    return x1v[:, :, t:t + nn]
                norm_block(ph, src_fn, tb0, TB, hT, hT_b, gm2, gm2_b, SH2, xbufs, tmpbufs, sqbuf, rbuf)
                S.dma("act", h2T_d.rearrange("(k p) t -> p k t", p=128)[:, :, tb0:tb0 + TB], hT[:], [hT_b], h2T_b)
                for cg in range(4):
                    wt, wb = wbufs[cg % 2]
                    S.dma("pool", wt[:], wqv[:, :, cg * 512:(cg + 1) * 512], [], wb)
                    for cc in range(4):
                        for tt in range(TB // 512):
                            pst, psb = kb.psum()
                            for k in range(KC):
                                S.op("pe", lambda e, k=k, cc=cc, tt=tt, pst=pst, wt=wt: e.matmul(
                                    pst[:, :], lhsT=wt[:, k, cc * 128:(cc + 1) * 128], rhs=hT[:, k, tt * 512:(tt + 1) * 512],
                                    start=(k == 0), stop=(k == KC - 1)), reads=[wb, hT_b], writes=[psb])
                            ot, ob = obufs[n % 4]
                            n += 1
                            S.op("act", lambda e, pst=pst, ot=ot: e.activation(out=ot[:], in_=pst[:, :], func=AF.Copy), reads=[psb], writes=[ob])
                            r0 = cg * 512 + cc * 128
                            S.dma("sp", qpT_d[r0:r0 + 128, tb0 + tt * 512:tb0 + (tt + 1) * 512], ot[:], [ob], qpT_b)
            kb.end(ph)
        with contextlib.ExitStack() as ph:
            keysT, keysT_b = kb.sb(ph, "keysT_sb", [128, 16, 128], BF16, dma=True)
            S.dma("pool", keysT[:], keysT_in.rearrange("p (a n) -> p a n", n=128), [], keysT_b)
            qts = [kb.sb(ph, f"qts{i}", [128, 16, 128], BF16, dma=True) for i in range(2)]
            s_all, s_all_b = kb.sb(ph, "s_all", [128, 16, 128], F32)
            v16, v16_b = kb.sb(ph, "v16", [128, 16, 16], F32)
            mtmp, mtmp_b = kb.sb(ph, "mtmp", [128, 128], F32)
            cand, cand_b = kb.sb(ph, "cand", [128, 256], F32)
            ctmp, ctmp_b = kb.sb(ph, "ctmp", [128, 256], F32)
            e256, e256_b = kb.sb(ph, "e256", [128, 256], F32)
            c1, c1_b = kb.sb(ph, "c1", [128, 8], F32)
            c2, c2_b = kb.sb(ph, "c2", [128, 8], F32)
            sm, sm_b = kb.sb(ph, "smalls", [128, 8], F32)
            Gs = [kb.sb(ph, f"Gs{i}", [128, 16384], BF16) for i in range(2)]
            Sd2 = [kb.sb(ph, f"Sd{i}", [128, 16, 128], F32) for i in range(2)]
            Ed2 = [kb.sb(ph, f"Ed{i}", [128, 16, 128], BF16) for i in range(2)]
            Md2 = [kb.sb(ph, f"Md{i}", [128, 16, 128], BF16) for i in range(2)]
            thr8, thr8_b = kb.sb(ph, "thr8", [128, 8], F32)
            nb8, nb8_b = kb.sb(ph, "nb8", [128, 8], F32)
            identg, identg_b = kb.sb(ph, "identg", [128, 128], BF16, dma=True)
            ident_src2 = kb.inputs["ident"] if "ident" in kb.inputs else kb.din("ident", [128, 128])
            S.dma("pool", identg[:], ident_src2[:, :], [], identg_b)
            qpv = qpT_d.rearrange("(a p) t -> p a t", p=128)
            nn = 0
            for ts in range(16):
                qt, qtb = qts[ts % 2]
                S.dma("sp", qt[:], qpv[:, :, ts * 128:(ts + 1) * 128], [qpT_b], qtb)
                for b4 in range(4):
                    pst, psb = kb.psum()
                    for a in range(4):
                        hc = b4 * 4 + a
                        S.op("pe", lambda e, hc=hc, a=a, pst=pst, qt=qt: e.matmul(pst[:, a * 128:(a + 1) * 128], lhsT=qt[:, hc, :], rhs=keysT[:, hc, :],
                                                                                 start=True, stop=True), reads=[qtb, keysT_b], writes=[psb])
                    S.op("act", lambda e, b4=b4, pst=pst: e.activation(out=s_all[:, b4 * 4:(b4 + 1) * 4, :].rearrange("p a n -> p (a n)"), in_=pst[:, :], func=AF.Copy),
                         reads=[psb], writes=[s_all_b])
                for hc in range(16):
                    S.op("dve", lambda e, hc=hc: e.max(out=v16[:, hc, 0:8], in_=s_all[:, hc, :]), reads=[s_all_b], writes=[v16_b])
                    S.op("dve", lambda e, hc=hc: e.match_replace(out=mtmp[:], in_to_replace=v16[:, hc, 0:8], in_values=s_all[:, hc, :], imm_value=-1.0e30),
                         reads=[s_all_b, v16_b], writes=[mtmp_b])
                    S.op("dve", lambda e, hc=hc: e.max(out=v16[:, hc, 8:16], in_=mtmp[:]), reads=[mtmp_b], writes=[v16_b])
                G, G_sb = Gs[ts % 2]
                for h in range(8):
                    S.op("dve", lambda e, h=h: e.tensor_tensor(
                        out=cand[:].rearrange("p (a b) -> p a b", a=16),
                        in0=v16[:, 2 * h, :].unsqueeze(2).to_broadcast([128, 16, 16]),
                        in1=v16[:, 2 * h + 1, :].unsqueeze(1).to_broadcast([128, 16, 16]), op=ALU.add),
                        reads=[v16_b], writes=[cand_b])
                    S.op("dve", lambda e: e.max(out=c1[:], in_=cand[:]), reads=[cand_b], writes=[c1_b])
                    S.op("dve", lambda e: e.match_replace(out=ctmp[:], in_to_replace=c1[:], in_values=cand[:], imm_value=-1.0e30),
                         reads=[cand_b, c1_b], writes=[ctmp_b])
                    S.op("dve", lambda e: e.max(out=c2[:], in_=ctmp[:]), reads=[ctmp_b], writes=[c2_b])
                    S.op("dve", lambda e, h=h: e.tensor_copy(out=thr8[:, h:h + 1], in_=c2[:, 7:8]), reads=[c2_b], writes=[thr8_b])
                    S.op("dve", lambda e: e.tensor_scalar(out=sm[:, 0:1], in0=c1[:, 0:1], scalar1=-1.0, scalar2=None, op0=ALU.mult), reads=[c1_b], writes=[sm_b])
                    S.op("act", lambda e: e.activation(out=e256[:], in_=cand[:], func=AF.Exp, bias=sm[:, 0:1], scale=1.0), reads=[cand_b, sm_b], writes=[e256_b])
                    S.op("dve", lambda e: e.scalar_tensor_tensor(out=ctmp[:], in0=cand[:], scalar=c2[:, 7:8], in1=e256[:], op0=ALU.is_ge, op1=ALU.mult),
                         reads=[cand_b, c2_b, e256_b], writes=[ctmp_b])
                    S.op("dve", lambda e: e.tensor_reduce(out=sm[:, 1:2], in_=ctmp[:], axis=AX.X, op=ALU.add), reads=[ctmp_b], writes=[sm_b])
                    S.op("act", lambda e: e.activation(out=sm[:, 2:3], in_=sm[:, 1:2], func=AF.Ln), reads=[sm_b], writes=[sm_b])
                    S.op("dve", lambda e, h=h: e.tensor_tensor(out=nb8[:, h:h + 1], in0=sm[:, 0:1], in1=sm[:, 2:3], op=ALU.subtract), reads=[sm_b], writes=[nb8_b])
                for ib in range(8):
                    for h in range(8):
                        Sd, Sd_b = Sd2[nn % 2]
                        Ed, Ed_b = Ed2[nn % 2]
                        Md, Md_b = Md2[nn % 2]
                        nn += 1
                        S.op("pool", lambda e, h=h, ib=ib, Sd=Sd: e.tensor_tensor(
                            out=Sd[:], in0=s_all[:, 2 * h, ib * 16:(ib + 1) * 16].unsqueeze(2).to_broadcast([128, 16, 128]),
                            in1=s_all[:, 2 * h + 1, :].unsqueeze(1).to_broadcast([128, 16, 128]), op=ALU.add),
                            reads=[s_all_b], writes=[Sd_b])
                        S.op("act", lambda e, Sd=Sd, Ed=Ed, h=h: e.activation(out=Ed[:], in_=Sd[:], func=AF.Exp, bias=nb8[:, h:h + 1], scale=1.0),
                             reads=[Sd_b, nb8_b], writes=[Ed_b])
                        S.op("dve", lambda e, Sd=Sd, Ed=Ed, Md=Md, h=h: e.scalar_tensor_tensor(
                            out=Md[:].rearrange("p a b -> p (a b)"), in0=Sd[:].rearrange("p a b -> p (a b)"), scalar=thr8[:, h:h + 1],
                            in1=Ed[:].rearrange("p a b -> p (a b)"), op0=ALU.is_ge, op1=ALU.mult), reads=[Sd_b, Ed_b, thr8_b], writes=[Md_b])
                        for q4 in range(4):
                            S.op("pe", lambda e, q4=q4, Md=Md, h=h: e.matmul(kb.ps[q4][0][:, :], lhsT=identg[:], rhs=Md[:].rearrange("p a b -> p (a b)")[:, q4 * 512:(q4 + 1) * 512],
                                                                            start=(h == 0), stop=(h == 7)), reads=[identg_b, Md_b], writes=[kb.ps[q4][1]])
                    for q4 in range(4):
                        S.op("act", lambda e, q4=q4, ib=ib, G=G: e.activation(out=G[:, ib * 2048 + q4 * 512:ib * 2048 + (q4 + 1) * 512], in_=kb.ps[q4][0][:, :], func=AF.Copy),
                             reads=[kb.ps[q4][1]], writes=[G_sb])
                S.dma("act", G_d[ts], G[:], [G_sb], G_b)
            kb.end(ph)
        with contextlib.ExitStack() as ph:
            identp, identp_b = kb.sb(ph, "identp", [128, 128], BF16, dma=True)
            ident_src = kb.inputs["ident"] if "ident" in kb.inputs else kb.din("ident", [128, 128])
            S.dma("pool", identp[:], ident_src[:, :], [], identp_b)
            h2t, h2t_b = kb.sb(ph, "h2t", [128, KC, 512], BF16, dma=True)
            uts = [kb.sb(ph, f"uts{i}", [128, KC, 512], BF16, dma=True) for i in range(2)]
            vts = [kb.sb(ph, f"vts{i}", [128, 4, D], BF16, dma=True) for i in range(2)]
            gss = [kb.sb(ph, f"gss{i}", [128, 4, 512], BF16, dma=True) for i in range(2)]
            gas = [kb.sb(ph, f"gas{i}", [128, 512], BF16) for i in range(2)]
            Wts = [kb.sb(ph, f"Wts{i}", [128, 512], BF16) for i in range(4)]
            WT, WT_b = kb.sb(ph, "WT", [128, 4, 512], BF16)
            acc, acc_b = kb.sb(ph, "pacc", [128, KC, 512], F32)
            x1t2 = [kb.sb(ph, f"px1{i}", [128, 512], F32, dma=True) for i in range(2)]
            x2t2 = [kb.sb(ph, f"px2{i}", [128, 512], F32) for i in range(2)]
            pbanks = [(kb.psbf[0][:], kb.psbf[1]), (kb.ps[6][0][:].bitcast(BF16), kb.ps[6][1])]
            uTv = uT_in.rearrange("(k p) e -> p k e", p=128)
            vv = v_in.rearrange("(c p) d -> p c d", p=128)
            h2v = h2T_d.rearrange("(k p) t -> p k t", p=128)
            nW = 0
            nT = 0
            for T in range(4):
                tsl = slice(T * 512, (T + 1) * 512)
                S.dma("sp", h2t[:], h2v[:, :, tsl], [h2T_b], h2t_b)
                for et in range(32):
                    ut, utb = uts[et % 2]
                    vt, vtb = vts[et % 2]
                    gs, gsb = gss[et % 2]
                    if T == 0:
                        S.dma("pool", ut[:], uTv[:, :, et * 512:(et + 1) * 512], [], utb)
                        S.dma("pool", vt[:].rearrange("p c (a b) -> p c a b", b=1024), vv[:, et * 4:(et + 1) * 4, :].rearrange("p c (a b) -> p c a b", b=1024), [], vtb)
                        S.dma("act", uTb_d[et], ut[:].rearrange("p k e -> p (k e)"), [utb], uTb_b)
                        S.dma("act", vb_d[et], vt[:].rearrange("p c d -> p (c d)"), [vtb], vb_b)
                    else:
                        S.dma("sp", ut[:].rearrange("p k e -> p (k e)"), uTb_d[et], [uTb_b], utb)
                        S.dma("act", vt[:].rearrange("p c d -> p (c d)"), vb_d[et], [vb_b], vtb)
                    S.dma("sp", gs[:], G_d[4 * T:4 * T + 4, :, et * 512:(et + 1) * 512].rearrange("a t e -> t a e"), [G_b], gsb)
                    wl = []
                    for a in range(4):
                        pst, psb = kb.psum()
                        for k in range(KC):
                            S.op("pe", lambda e, k=k, a=a, pst=pst, ut=ut: e.matmul(pst[:, :], lhsT=h2t[:, k, a * 128:(a + 1) * 128], rhs=ut[:, k, :],
                                                                                  start=(k == 0), stop=(k == KC - 1)), reads=[h2t_b, utb], writes=[psb])
                        ga, gab = gas[a % 2]
                        S.op("act", lambda e, pst=pst, ga=ga: e.activation(out=ga[:], in_=pst[:, :], func=AF.Gelu_apprx_tanh), reads=[psb], writes=[gab])
                        Wt, Wtb = Wts[nW % 4]
                        nW += 1
                        S.op("dve", lambda e, ga=ga, gs=gs, a=a, Wt=Wt: e.tensor_tensor(out=Wt[:], in0=ga[:], in1=gs[:, a, :], op=ALU.mult),
                             reads=[gab, gsb], writes=[Wtb])
                        wl.append((Wt, Wtb))
                    for c in range(4):
                        pbt, hb = pbanks[nT % 2]
                        nT += 1
                        for a in range(4):
                            S.op("pe", lambda e, a=a, c=c, pbt=pbt, wl=wl: e.transpose(out=pbt[:, a * 128:(a + 1) * 128], in_=wl[a][0][:, c * 128:(c + 1) * 128],
                                                                                      identity=identp[:]), reads=[wl[a][1], identp_b], writes=[hb])
                        S.op("act", lambda e, c=c, pbt=pbt: e.activation(out=WT[:, c, :], in_=pbt[:, 0:512], func=AF.Copy), reads=[hb], writes=[WT_b])
                    for dk in range(KC):
                        pst, psb = kb.psum()
                        for c in range(4):
                            S.op("pe", lambda e, c=c, dk=dk, pst=pst, vt=vt: e.matmul(pst[:, :], lhsT=vt[:, c, dk * 128:(dk + 1) * 128], rhs=WT[:, c, :],
                                                                                    start=(c == 0), stop=(c == 3)), reads=[vtb, WT_b], writes=[psb])
                        if et == 0:
                            S.op("dve", lambda e, dk=dk, pst=pst: e.tensor_copy(out=acc[:, dk, :], in_=pst[:, :]), reads=[psb], writes=[acc_b])
                        else:
                            S.op("dve", lambda e, dk=dk, pst=pst: e.tensor_tensor(out=acc[:, dk, :], in0=pst[:, :], in1=acc[:, dk, :], op=ALU.add),
                                 reads=[psb, acc_b], writes=[acc_b])
                for dk in range(KC):
                    x1t, x1tb = x1t2[dk % 2]
                    x2t, x2tb = x2t2[dk % 2]
                    S.dma("sp", x1t[:], x1T_d[dk * 128:(dk + 1) * 128, tsl], [x1T_b], x1tb)
                    S.op("dve", lambda e, dk=dk, x1t=x1t, x2t=x2t: e.scalar_tensor_tensor(
                        out=x2t[:], in0=acc[:, dk, :], scalar=mod[:, G2 + dk:G2 + dk + 1], in1=x1t[:], op0=ALU.mult, op1=ALU.add),
                        reads=[acc_b, mod_b, x1tb], writes=[x2tb])
                    S.dma("act", x2T_d[dk * 128:(dk + 1) * 128, tsl], x2t[:], [x2tb], x2T_b)
                if T == 0:
                    S.barrier()
                    S.retire([b_ for (_, b_) in uts + vts])
                    for (_, b_) in uts + vts:
                        b_.dma = True
            kb.end(ph)

    if stages >= 7:
        with contextlib.ExitStack() as ph:
            TT = 256
            xb2 = [kb.sb(ph, f"fx{i}", [128, KC, TT], F32, dma=True) for i in range(2)]
            sq, sqb = kb.sb(ph, "fsq", [128, KC, TT], BF16)
            rt_, rb_ = kb.sb(ph, "frstd", [128, TT], F32)
            ob2 = [kb.sb(ph, f"fo{i}", [128, KC, TT], F32) for i in range(2)]
            srcv = x2src_d.rearrange("(k p) t -> p k t", p=128)
            outv = outT.rearrange("(k p) t -> p k t", p=128)
            for ti in range(TOWN // TT):
                xt, xb = xb2[ti % 2]
                ot, ob = ob2[ti % 2]
                S.dma("sp", xt[:], srcv[:, :, ti * TT:(ti + 1) * TT], [x2src_b], xb)
                S.op("act", lambda e, xt=xt: e.activation(out=sq[:], in_=xt[:], func=AF.Square), reads=[xb], writes=[sqb])
                pst, psb = kb.psum()
                for k in range(KC):
                    S.op("pe", lambda e, k=k, pst=pst: e.matmul(pst[:, 0:TT], lhsT=ones_bf[:], rhs=sq[:, k, :], start=(k == 0), stop=(k == KC - 1)),
                         reads=[ones_bf_b, sqb], writes=[psb])
                S.op("act", lambda e, pst=pst: e.activation(out=rt_[:], in_=pst[:, 0:TT], func=AF.Sqrt, bias=eps_t[:, 0:1], scale=1.0 / D),
                     reads=[psb, eps_b], writes=[rb_])
                S.op("dve", lambda e: e.reciprocal(out=rt_[:], in_=rt_[:]), reads=[rb_], writes=[rb_])
                for k in range(KC):
                    S.op("dve", lambda e, k=k, xt=xt, ot=ot: e.scalar_tensor_tensor(out=ot[:, k, :], in0=xt[:, k, :], scalar=gfin[:, k:k + 1], in1=rt_[:],
                                                                                  op0=ALU.mult, op1=ALU.mult), reads=[xb, gfin_b, rb_], writes=[ob])
                S.dma("act", outv[:, :, ti * TT:(ti + 1) * TT], ot[:], [ob], outT_b)
            kb.end(ph)

    for name in debug:
        if name == "mod":
            continue
        src = {"qT_d": (qT_d, qT_b), "vs_d": (vs_d, vs_b), "uT_d": (uT_d, uT_b), "mgT_d": (mgT_d, mgT_b),
               "kcT_d": (kcT_d, kcT_b), "gT_d": (gT_d, gT_b), "oT_d": (oT_d, oT_b), "y2T_d": (y2T_d, y2T_b), "x1T_d": (x1T_d, x1T_b), "x2T_d": (x2T_d, x2T_b)}[name]
        o = nc.dram_tensor("dbg_" + name, list(src[0].shape), src[0].dtype, kind="ExternalOutput").ap()
        ob = S.buf("dbg_" + name, dma=True)
        S.dma("sp", o[:, :], src[0][:, :], [src[1]], ob)
    if "mod" in debug:
        o = nc.dram_tensor("dbg_mod", [128, 96], F32, kind="ExternalOutput").ap()
        ob = S.buf("dbg_mod", dma=True)
        S.dma("sp", o[:, :], mod[:], [mod_b], ob)

    S.barrier()
    es.close()
    return nc, kb


def pcol(v, chunks):
    return np.ascontiguousarray(np.asarray(v, np.float32).reshape(chunks, 128).T)


def t5_bucket_np(dist):
    dist = np.maximum(dist, 0)
    lr = np.log(np.maximum(dist, 1).astype(np.float32) / np.float32(16)) / np.float32(math.log(8.0))
    large = np.minimum(16 + (lr * np.float32(16)).astype(np.int32), 31)
    return np.where(dist < 16, dist, large)


NEG = np.float32(-30000.0)


def attn_tables(half, inp):
    LB, OFFB, LC, OFFC = 4096, 1024, 6400, 2064
    rb = np.asarray(inp["rel_bias"], np.float32)
    m = {}
    d = np.arange(LB) - OFFB
    g = rb[t5_bucket_np(d)].T
    Vb = np.where(d[None, :] >= 0, g, NEG).astype(np.float32)
    Vw = np.where((d[None, :] >= 0) & (d[None, :] < 512), g, NEG).astype(np.float32)
    m["Vb"], m["Vw"] = Vb, Vw
    m["Vwc"] = Vw if half == 1 else np.full_like(Vw, NEG)
    d = np.arange(LC) - OFFC
    g = rb[t5_bucket_np(d)].T
    Vc = np.where(d[None, :] >= 0, g, NEG).astype(np.float32)
    m["Vc"] = Vc
    m["Vc0"] = Vc if half == 1 else np.full_like(Vc, NEG)
    p = np.arange(128)[:, None, None]
    ts = np.arange(16)[None, :, None]
    j = np.arange(64)[None, None, :]
    t_abs = half * TOWN + ts * 128 + p
    cur = t_abs // 64
    ja = j - 32 * (1 - half)
    valid = (ja >= 0) & (ja <= cur)
    forced = (ja == 0) | (ja == cur) | (ja == cur - 1)
    m["selbias"] = np.where(valid, np.float32(1e4) * forced, np.float32(-1e30)).astype(np.float32).reshape(128, 1024)
    m["selinv"] = (~valid).astype(np.float32).reshape(128, 1024)
    pp = np.arange(128)[:, None]
    nn = 128 * np.arange(2)[None, :] + 127 - pp
    cs = nn[:, :, None] * 16
    ss = np.arange(64)[None, None, :] * 64
    ov = np.clip(np.minimum(cs + 32, ss + 64) - np.maximum(cs, ss), 0, None) / 32.0
    ov = np.where(nn[:, :, None] >= 255, 0.0, ov)
    maug = np.concatenate([ov, np.ones((128, 2, 1))], axis=2).astype(np.float32)
    m["maug"] = maug.reshape(128, 130)
    sl = 128 * np.arange(32)[None, :, None] + 127 - np.arange(128)[None, None, :]
    ne = np.where((sl // 64) == np.arange(64)[:, None, None], NEG, np.float32(0)).astype(np.float32).reshape(64, 4096)
    m["negE"] = np.concatenate([ne, ne], axis=0)
    k = np.arange(48)
    sg_ = np.repeat((k[:, None] == k[None, :]).astype(np.float32)[:, :, None], 64, axis=2).reshape(48, 3072)
    m["selg"] = np.concatenate([sg_, np.zeros((16, 3072), np.float32)], axis=0)
    m["ident"] = np.eye(128, dtype=np.float32)
    m["jmat"] = np.eye(128, dtype=np.float32)[::-1].copy()
    m["rel_bias"] = rb
    m["cmp_w1"] = np.ascontiguousarray(inp["cmp_w1"][0])
    m["cmp_posT"] = np.ascontiguousarray(np.transpose(inp["cmp_pos"][0], (0, 2, 1)))
    m["cmp_b1T"] = np.stack([pcol(inp["cmp_b1"][0, jj], 2) for jj in range(2)])
    m["cmp_w2"] = np.ascontiguousarray(inp["cmp_w2"][0])
    m["cmp_b2"] = np.ascontiguousarray(inp["cmp_b2"][0])
    return m


def s5_tables(inp):
    m = {}
    lre = np.asarray(inp["s5_lam_re"][0], np.float32)
    lim = np.asarray(inp["s5_lam_im"][0], np.float32)
    lst = np.asarray(inp["s5_log_step"][0], np.float32)
    def qp(a):
        return a.reshape(32, 2, 64).reshape(32, 128)
    lst2 = np.repeat(lst[:, None], 64, axis=1)
    m["lre_f"] = qp(lre).reshape(1, 4096).copy()
    m["lim_f"] = qp(lim).reshape(1, 4096).copy()
    m["lst_f"] = qp(lst2).reshape(1, 4096).copy()
    m["lreT"] = np.ascontiguousarray(qp(lre).T)
    m["limT"] = np.ascontiguousarray(qp(lim).T)
    m["lstT"] = np.ascontiguousarray(qp(lst2).T)
    bre = np.asarray(inp["s5_b_re"][0], np.float32)
    bim = np.asarray(inp["s5_b_im"][0], np.float32)
    cre = np.asarray(inp["s5_c_re"][0], np.float32)
    cim = np.asarray(inp["s5_c_im"][0], np.float32)
    Bre = np.zeros((128, 32, 128), np.float32); Bim = np.zeros_like(Bre)
    Cre = np.zeros((128, 32, 128), np.float32); Cim = np.zeros_like(Cre)
    for pr in range(32):
        for gg in range(2):
            g = 2 * pr + gg
            lg = g % 8
            Bre[lg * 16:(lg + 1) * 16, pr, gg * 64:(gg + 1) * 64] = bre[g].T
            Bim[lg * 16:(lg + 1) * 16, pr, gg * 64:(gg + 1) * 64] = bim[g].T
            Cre[gg * 64:(gg + 1) * 64, pr, lg * 16:(lg + 1) * 16] = cre[g].T
            Cim[gg * 64:(gg + 1) * 64, pr, lg * 16:(lg + 1) * 16] = cim[g].T
    m["Bpad_re"] = Bre.reshape(128, 4096); m["Bpad_im"] = Bim.reshape(128, 4096)
    m["Cpad_re"] = Cre.reshape(128, 4096); m["Cpad_im"] = Cim.reshape(128, 4096)
    m["tau"] = np.arange(512, dtype=np.float32).reshape(1, 512)
    m["s5dT"] = pcol(inp["s5_d"][0], 8)
    m["glu_w"] = np.ascontiguousarray(inp["glu_w"][0])
    m["glubT"] = pcol(inp["glu_b"][0], 8)
    return m


SHARED = {}


def prep_shared(inp):
    SHARED["peer_uT"] = np.ascontiguousarray(np.asarray(inp["peer_u"][0]).T)
    SHARED["peer_v"] = np.ascontiguousarray(inp["peer_v"][0])


def prep_inputs(core, inp):
    if "peer_uT" not in SHARED:
        prep_shared(inp)
    b, half = core // 2, core % 2
    x = inp["x"]
    own = x[b, half * TOWN:(half + 1) * TOWN]
    ctx = x[b, 0:TOWN] if half == 1 else np.zeros_like(own)
    xT = np.ascontiguousarray(np.concatenate([ctx, own], 0).T)
    m = {
        "xT": xT,
        "xTr": np.ascontiguousarray(xT.reshape(D, TALL // 128, 128)[:, :, ::-1].reshape(D, TALL)),
        "cT": pcol(inp["c"][b], KC),
        "ada_w": np.ascontiguousarray(inp["ada_w"][0]),
        "ada_bT": pcol(inp["ada_b"][0], 96),
        "gmixT": pcol(inp["norm_mix_g"][0], KC),
        "gffnT": pcol(inp["norm_ffn_g"][0], KC),
        "gfinT": pcol(inp["final_g"], KC),
        "w_in": np.ascontiguousarray(inp["w_in"][0]),
        "ctxflag": np.full((128, 1), float(half), np.float32),
    }
    m.update(attn_tables(half, inp))
    m.update(s5_tables(inp))
    m["w_up_attn"] = np.ascontiguousarray(inp["w_up_attn"][0])
    m["w_up_ssm"] = np.ascontiguousarray(inp["w_up_ssm"][0])
    m["w_out"] = np.ascontiguousarray(inp["w_out"][0])
    m["peer_w_q"] = np.ascontiguousarray(inp["peer_w_q"][0])
    sk = np.asarray(inp["peer_sub_keys"][0], np.float32)
    m["keysT"] = np.ascontiguousarray(np.transpose(sk.reshape(16, 128, 128), (2, 0, 1)).reshape(128, 2048))
    m["peer_uT"] = SHARED["peer_uT"]
    m["peer_v"] = SHARED["peer_v"]
    return m


def kernel(**inputs):
    inp = {k: np.asarray(v) for k, v in inputs.items()}
    SHARED.clear()
    nc, kb = build()
    in_maps = []
    for core in range(8):
        m = prep_inputs(core, inp)
        in_maps.append({k: m[k] for k in kb.inputs})
    res = run_bass_kernel_spmd(nc, in_maps, core_ids=list(range(8)))
    out = np.zeros((4, 4096, D), np.float32)
    for core in range(8):
        b, half = core // 2, core % 2
        out[b, half * TOWN:(half + 1) * TOWN] = res.results[core]["outT"].T
    return out
```

```python
import contextlib
import math
import os
import numpy as np
import concourse.bass as bass
import concourse.mybir as mybir
from concourse.bass_utils import run_bass_kernel_spmd

F32 = mybir.dt.float32
BF16 = mybir.dt.bfloat16
U32 = mybir.dt.uint32
AF = mybir.ActivationFunctionType
ALU = mybir.AluOpType
AX = mybir.AxisListType

D = 2048
TOWN = 2048
TALL = 4096
KC = 16
INW = 7728
EPS = 1e-6


class _Stop(Exception):
    pass


CUT = int(os.environ.get("KCUT", "0"))


class Buf:
    def __init__(self, name, dsem=None):
        self.name = name
        self.dma = False
        self.kind = None
        self.w = None
        self.r = {}
        self.dsem = dsem
        self.dcount = 0


class Sched:
    LIMIT = 4000

    def __init__(self, nc, es):
        self.nc = nc
        self.es = es
        self.eng = {"pe": nc.tensor, "dve": nc.vector, "act": nc.scalar, "pool": nc.gpsimd, "sp": nc.sync}
        self.sem = {}
        self.cnt = {}
        self.semid = {}
        self.nsem = 0
        self.waited = {e: {} for e in self.eng}
        self.dmabufs = []
        self.freesems = {}
        self.ninst = 0
        for e in self.eng:
            self._newsem(e)

    def _newsem(self, e):
        s = self.es.enter_context(self.nc.semaphore(f"s{self.nsem}_{e}"))
        self.nsem += 1
        self.sem[e] = (s, self.nsem)
        self.cnt[e] = 0

    def buf(self, name, dma=False):
        b = Buf(name)
        b.dma = dma
        b.kind = None
        return b

    def _dsem(self, b, kind):
        if b.dsem is None:
            assert b.dma, b.name
            fl = self.freesems.setdefault(kind, [])
            if fl:
                b.dsem, b.dcount = fl.pop()
            else:
                b.dsem = (self.es.enter_context(self.nc.semaphore(f"d{self.nsem}")), self.nsem + 1)
                self.nsem += 1
                b.dcount = 0
            b.kind = kind
            self.dmabufs.append(b)
        assert b.kind == kind, (b.name, b.kind, kind)

    def retire(self, bufs):
        for b in bufs:
            if b.dsem is not None and b in self.dmabufs:
                self.dmabufs.remove(b)
                self.freesems.setdefault(b.kind, []).append((b.dsem, b.dcount))
                b.dsem = None
                b.dma = False

    def _wait(self, X, tok):
        if tok[0] == "dma":
            b = tok[1]
            if b.dsem is None or b.dcount == 0:
                return
            sem, sid = b.dsem
            val = 16 * b.dcount
            E = None
        else:
            (sem, sid), val, E = tok
        if E == X and X in ("pe", "sp"):
            return
        if self.waited[X].get(sid, 0) >= val:
            return
        self.eng[X].wait_ge(sem, val)
        self.waited[X][sid] = val
        self.ninst += 1

    def _deps(self, X, reads, writes):
        for b in reads:
            if b.w is not None:
                self._wait(X, b.w)
        for b in writes:
            if b.w is not None:
                self._wait(X, b.w)
            for t in b.r.values():
                self._wait(X, t)

    def op(self, X, fn, reads=(), writes=()):
        ex = [b for b in reads if getattr(b, "excl", False) and b not in writes]
        if ex:
            writes = list(writes) + ex
            reads = [b for b in reads if b not in ex]
        self._deps(X, reads, writes)
        inst = fn(self.eng[X])
        if self.cnt[X] >= self.LIMIT:
            self._newsem(X)
        self.cnt[X] += 1
        inst.then_inc(self.sem[X][0], 1)
        tok = (self.sem[X], self.cnt[X], X)
        for b in reads:
            b.r[X] = tok
        for b in writes:
            b.w = tok
            b.r = {}
        self.ninst += 1
        return tok

    def dma(self, X, out, in_, reads, wbuf, **kw):
        self._dsem(wbuf, "sw" if X == "pool" else "hw")
        self._deps(X, reads, [wbuf])
        inst = self.eng[X].dma_start(out=out, in_=in_, **kw)
        inst.then_inc(wbuf.dsem[0], 16)
        wbuf.dcount += 1
        wbuf.w = ("dma", wbuf)
        wbuf.r = {}
        for b in reads:
            b.r[("dma", id(wbuf))] = ("dma", wbuf)
        self.ninst += 1

    def barrier(self):
        for X in self.eng:
            for E in self.eng:
                if E != X and self.cnt[E] > 0:
                    self._wait(X, (self.sem[E], self.cnt[E], E))
            for b in self.dmabufs:
                if b.dsem is not None and b.dcount > 0:
                    self._wait(X, ("dma", b))


class KB:
    def __init__(self, stages=99, debug=()):
        self.stages = stages
        self.debug = debug
        self.nc = bass.Bass("TRN2", target_bir_lowering=False)
        self.es = contextlib.ExitStack()
        self.S = Sched(self.nc, self.es)
        self.inputs = {}
        self.psn = 0
        self.ctxbufs = {}

    def din(self, name, shape, dtype=F32):
        t = self.nc.dram_tensor(name, list(shape), dtype, kind="ExternalInput").ap()
        self.inputs[name] = t
        return t

    def dscratch(self, name, shape, dtype):
        t = self.nc.dram_tensor(name, list(shape), dtype, kind="Internal").ap()
        return t, self.S.buf(name, dma=True)

    def sb(self, ctx, name, shape, dtype, dma=False):
        t = ctx.enter_context(self.nc.sbuf_tensor(name, list(shape), dtype))
        b = self.S.buf(name, dma=dma)
        self.ctxbufs.setdefault(id(ctx), []).append(b)
        return t, b

    def end(self, ctx):
        self.S.barrier()
        self.S.retire(self.ctxbufs.pop(id(ctx), []))

    def psum_init(self):
        self.ps = []
        for i in range(7):
            t = self.es.enter_context(self.nc.psum_tensor(f"ps{i}", [128, 512], F32))
            self.ps.append((t, self.S.buf(f"ps{i}")))
            self.ps[-1][1].excl = True
        t = self.es.enter_context(self.nc.psum_tensor("psbf", [128, 1024], BF16))
        self.psbf = (t, self.S.buf("psbf"))
        self.psbf[1].excl = True

    def psum(self, nrot=4):
        r = self.ps[self.psn % nrot]
        self.psn += 1
        return r


def build(stages=99, debug=(), skip=(), sub=99):
    kb = KB(stages, debug)
    nc, S = kb.nc, kb.S
    kb.psum_init()
    es = kb.es

    xT = kb.din("xT", [D, TALL])
    xTr = kb.din("xTr", [D, TALL])
    cT = kb.din("cT", [128, KC])
    ada_w = kb.din("ada_w", [D, 6 * D])
    ada_bT = kb.din("ada_bT", [128, 96])
    gmixT = kb.din("gmixT", [128, KC])
    gffnT = kb.din("gffnT", [128, KC])
    gfinT = kb.din("gfinT", [128, KC])
    w_in = kb.din("w_in", [D, INW])
    ctxflag = kb.din("ctxflag", [128, 1])

    outT = nc.dram_tensor("outT", [D, TOWN], F32, kind="ExternalOutput").ap()
    outT_b = S.buf("outT", dma=True)

    qT_d, qT_b = kb.dscratch("qT_d", [1024, TOWN], BF16)
    kcT_d, kcT_b = kb.dscratch("kcT_d", [256, TALL], BF16)
    vcT_d, vcT_b = kb.dscratch("vcT_d", [256, TALL], BF16)
    ksT_d, ksT_b = kb.dscratch("ksT_d", [256, TALL], BF16)
    kwT_d, kwT_b = kb.dscratch("kwT_d", [256, TALL], BF16)
    vs_d, vs_b = kb.dscratch("vs_d", [TALL, 256], BF16)
    vw_d, vw_b = kb.dscratch("vw_d", [TALL, 256], BF16)
    gT_d, gT_b = kb.dscratch("gT_d", [48, TOWN], BF16)
    uT_d, uT_b = kb.dscratch("uT_d", [1024, TALL], BF16)
    mgT_d, mgT_b = kb.dscratch("mgT_d", [4096, TOWN], BF16)

    pc = es
    ones_bf, ones_bf_b = kb.sb(pc, "ones_bf", [128, 128], BF16)
    eps_t, eps_b = kb.sb(pc, "eps_t", [128, 1], F32)
    mod, mod_b = kb.sb(pc, "mod", [128, 96], F32)
    gm1, gm1_b = kb.sb(pc, "gm1", [128, KC], F32)
    gm2, gm2_b = kb.sb(pc, "gm2", [128, KC], F32)
    gfin, gfin_b = kb.sb(pc, "gfin", [128, KC], F32, dma=True)
    flag_t, flag_b = kb.sb(pc, "flag_t", [128, 1], F32, dma=True)
    S.op("dve", lambda e: e.memset(ones_bf[:], 1.0), writes=[ones_bf_b])
    S.op("dve", lambda e: e.memset(eps_t[:], EPS), writes=[eps_b])
    S.dma("sp", gfin[:], gfinT[:, :], [], gfin_b)
    S.dma("sp", flag_t[:], ctxflag[:, :], [], flag_b)

    with contextlib.ExitStack() as ph:
        c_sb, c_b = kb.sb(ph, "c_sb", [128, KC], F32, dma=True)
        sc_sb, sc_b = kb.sb(ph, "sc_sb", [128, KC], F32)
        ab_sb, ab_b = kb.sb(ph, "ab_sb", [128, 96], F32, dma=True)
        gx_sb, gx_b = kb.sb(ph, "gx_sb", [128, 2 * KC], F32, dma=True)
        wts = [kb.sb(ph, f"adaw{i}", [128, KC, 512], F32, dma=True) for i in range(2)]
        S.dma("sp", c_sb[:], cT[:, :], [], c_b)
        S.dma("sp", ab_sb[:], ada_bT[:, :], [], ab_b)
        S.dma("sp", gx_sb[:, 0:KC], gmixT[:, :], [], gx_b)
        S.dma("sp", gx_sb[:, KC:2 * KC], gffnT[:, :], [], gx_b)
        S.op("act", lambda e: e.activation(out=sc_sb[:], in_=c_sb[:], func=AF.Silu), reads=[c_b], writes=[sc_b])
        pst, psb = kb.psum()
        awv = ada_w.rearrange("(k p) f -> p k f", p=128)
        for fg in range(24):
            wt, wb = wts[fg % 2]
            S.dma("sp" if fg % 2 == 0 else "act", wt[:], awv[:, :, fg * 512:(fg + 1) * 512], [], wb)
            for fc in range(4):
                col = fg * 4 + fc
                for k in range(KC):
                    S.op("pe", lambda e, k=k, fc=fc, col=col, wt=wt: e.matmul(
                        pst[:, col:col + 1], lhsT=wt[:, k, fc * 128:(fc + 1) * 128], rhs=sc_sb[:, k:k + 1],
                        start=(k == 0), stop=(k == KC - 1)), reads=[wb, sc_b], writes=[psb])
        S.op("dve", lambda e: e.tensor_tensor(out=mod[:], in0=pst[:, 0:96], in1=ab_sb[:], op=ALU.add),
             reads=[psb, ab_b], writes=[mod_b])
        S.op("dve", lambda e: e.scalar_tensor_tensor(out=gm1[:], in0=mod[:, 16:32], scalar=1.0, in1=gx_sb[:, 0:KC],
                                                     op0=ALU.add, op1=ALU.mult), reads=[mod_b, gx_b], writes=[gm1_b])
        S.op("dve", lambda e: e.scalar_tensor_tensor(out=gm2[:], in0=mod[:, 64:80], scalar=1.0, in1=gx_sb[:, KC:2 * KC],
                                                     op0=ALU.add, op1=ALU.mult), reads=[mod_b, gx_b], writes=[gm2_b])
        kb.end(ph)

    SH1, G1, SH2, G2 = 0, 32, 48, 80

    def norm_block(ph, src_ap_fn, t0, nt, hT, hT_b, gm, gm_b, shcol, xbufs, tmpbufs, sqbuf, rbuf):
        TT = 256
        for ti in range(nt // TT):
            xt, xb = xbufs[ti % 2]
            S.dma("sp", xt[:], src_ap_fn(t0 + ti * TT, TT), [], xb)
            sq, sqb = sqbuf
            S.op("act", lambda e, xt=xt: e.activation(out=sq[:], in_=xt[:], func=AF.Square), reads=[xb], writes=[sqb])
            pst, psb = kb.psum()
            for k in range(KC):
                S.op("pe", lambda e, k=k: e.matmul(pst[:, 0:TT], lhsT=ones_bf[:], rhs=sq[:, k, :],
                                                   start=(k == 0), stop=(k == KC - 1)),
                     reads=[ones_bf_b, sqb], writes=[psb])
            rt, rb = rbuf
            S.op("act", lambda e: e.activation(out=rt[:], in_=pst[:, 0:TT], func=AF.Sqrt, bias=eps_t[:, 0:1],
                                               scale=1.0 / D), reads=[psb, eps_b], writes=[rb])
            S.op("dve", lambda e: e.reciprocal(out=rt[:], in_=rt[:]), reads=[rb], writes=[rb])
            for k in range(KC):
                tt, tb = tmpbufs[k % 2]
                S.op("dve", lambda e, k=k, tt=tt, xt=xt: e.scalar_tensor_tensor(
                    out=tt[:], in0=xt[:, k, :], scalar=gm[:, k:k + 1], in1=rt[:], op0=ALU.mult, op1=ALU.mult),
                    reads=[xb, gm_b, rb], writes=[tb])
                S.op("act", lambda e, k=k, tt=tt, ti=ti: e.activation(
                    out=hT[:, k, ti * TT:(ti + 1) * TT], in_=tt[:], func=AF.Identity,
                    bias=mod[:, shcol + k:shcol + k + 1], scale=1.0), reads=[tb, mod_b], writes=[hT_b])

    if stages >= 2:
        with contextlib.ExitStack() as ph:
            TB = 1024
            hT, hT_b = kb.sb(ph, "hT", [128, KC, TB], BF16)
            xbufs = [kb.sb(ph, f"xb{i}", [128, KC, 256], F32, dma=True) for i in range(2)]
            tmpbufs = [kb.sb(ph, f"ntmp{i}", [128, 256], F32) for i in range(2)]
            sqbuf = kb.sb(ph, "sqb", [128, KC, 256], BF16)
            rbuf = kb.sb(ph, "rstd", [128, 256], F32)
            wbufs = [kb.sb(ph, f"wbuf{i}", [128, KC, 512], BF16, dma=True) for i in range(2)]
            obufs = [kb.sb(ph, f"obuf{i}", [128, 512], BF16) for i in range(4)]
            xTv = xT.rearrange("(k p) t -> p k t", p=128)
            winv = w_in.rearrange("(k p) c -> p k c", p=128)
            wcount = [0]
            ocount = [0]

            def load_w(col0, ncols):
                wt, wb = wbufs[wcount[0] % 2]
                wcount[0] += 1
                S.dma("pool", wt[:, :, 0:ncols], winv[:, :, col0:col0 + ncols], [], wb)
                return wt, wb

            def fm_cols(col0, ncols, tb0, epi):
                wt, wb = load_w(col0, ncols)
                for cc in range((ncols + 127) // 128):
                    cw = min(128, ncols - cc * 128)
                    for tt in range(TB // 512):
                        pst, psb = kb.psum()
                        for k in range(KC):
                            S.op("pe", lambda e, k=k, cc=cc, cw=cw, tt=tt, pst=pst, wt=wt: e.matmul(
                                pst[0:cw, :], lhsT=wt[:, k, cc * 128:cc * 128 + cw], rhs=hT[:, k, tt * 512:(tt + 1) * 512],
                                start=(k == 0), stop=(k == KC - 1)), reads=[wb, hT_b], writes=[psb])
                        epi(col0 + cc * 128, cw, tb0 + tt * 512, pst, psb)

            def store_epi(dst_d, dst_b, rowbase, tbase, func=AF.Copy, scale=1.0, flag=False):
                def epi(col, cw, t, pst, psb):
                    ot, ob = obufs[ocount[0] % 4]
                    ocount[0] += 1
                    if flag:
                        S.op("act", lambda e: e.activation(out=ot[0:cw, :], in_=pst[0:cw, :], func=AF.Copy,
                                                           scale=flag_t[0:cw, 0:1]), reads=[psb, flag_b], writes=[ob])
                    else:
                        S.op("act", lambda e: e.activation(out=ot[0:cw, :], in_=pst[0:cw, :], func=func, scale=scale),
                             reads=[psb], writes=[ob])
                    r0 = col - rowbase
                    S.dma("sp", dst_d[r0:r0 + cw, t - tbase:t - tbase + 512], ot[0:cw, :], [ob], dst_b)
                return epi

            def tm_cols(col0, ncols, tb0, dst_d, dst_b):
                wt, wb = load_w(col0, ncols)
                for sc in range(TB // 128):
                    pst, psb = kb.psum()
                    for k in range(KC):
                        S.op("pe", lambda e, k=k, sc=sc, pst=pst, wt=wt: e.matmul(
                            pst[:, 0:ncols], lhsT=hT[:, k, sc * 128:(sc + 1) * 128], rhs=wt[:, k, 0:ncols],
                            start=(k == 0), stop=(k == KC - 1)), reads=[wb, hT_b], writes=[psb])
                    ot, ob = obufs[ocount[0] % 4]
                    ocount[0] += 1
                    S.op("act", lambda e, pst=pst, ot=ot: e.activation(out=ot[:, 0:ncols], in_=pst[:, 0:ncols], func=AF.Copy),
                         reads=[psb], writes=[ob])
                    S.dma("sp", dst_d[tb0 + sc * 128:tb0 + (sc + 1) * 128, :], ot[:, 0:ncols], [ob], dst_b)

            xTrv = xTr.rearrange("(k p) t -> p k t", p=128)
            for rev in (False, True):
                srcv = xTrv if rev else xTv
                for blk in range(TALL // TB):
                    tb0 = blk * TB
                    own = tb0 >= TALL - TOWN
                    norm_block(ph, lambda t, n, srcv=srcv: srcv[:, :, t:t + n], tb0, TB, hT, hT_b, gm1, gm1_b, SH1,
                               xbufs, tmpbufs, sqbuf, rbuf)
                    if rev:
                        fm_cols(1536, 256, tb0, store_epi(ksT_d, ksT_b, 1536, 0))
                        fm_cols(2048, 256, tb0, store_epi(kwT_d, kwT_b, 2048, 0))
                        tm_cols(1792, 256, tb0, vs_d, vs_b)
                        tm_cols(2304, 256, tb0, vw_d, vw_b)
                        continue
                    fm_cols(1024, 256, tb0, store_epi(kcT_d, kcT_b, 1024, 0))
                    fm_cols(1280, 256, tb0, store_epi(vcT_d, vcT_b, 1280, 0))
                    fm_cols(2608, 512, tb0, store_epi(uT_d, uT_b, 2608, 0, flag=not own))
                    fm_cols(3120, 512, tb0, store_epi(uT_d, uT_b, 2608, 0, flag=not own))
                    if own:
                        tq = TALL - TOWN
                        fm_cols(0, 512, tb0, store_epi(qT_d, qT_b, 0, tq, scale=0.125))
                        fm_cols(512, 512, tb0, store_epi(qT_d, qT_b, 0, tq, scale=0.125))
                        fm_cols(2560, 48, tb0, store_epi(gT_d, gT_b, 2560, tq, func=AF.Sigmoid))
                        for g in range(8):
                            fm_cols(3632 + g * 512, 512, tb0, store_epi(mgT_d, mgT_b, 3632, tq, func=AF.Sigmoid))
            kb.end(ph)


    oT_d, oT_b = kb.dscratch("oT_d", [1024, TOWN], BF16)
    LB, OFFB, LC, OFFC = 4096, 1024, 6400, 2064
    if stages >= 3 and 3 not in skip:
        cmp_w1 = kb.din("cmp_w1", [2, 2048, 256])
        cmp_posT = kb.din("cmp_posT", [2, 64, 32])
        cmp_b1T = kb.din("cmp_b1T", [2, 128, 2])
        cmp_w2 = kb.din("cmp_w2", [2, 256, 64])
        cmp_b2 = kb.din("cmp_b2", [2, 64])
        rel_bias = kb.din("rel_bias", [32, 16])
        ident_in = kb.din("ident", [128, 128])
        jmat_in = kb.din("jmat", [128, 128])
        selg_in = kb.din("selg", [64, 48 * 64])
        negE_in = kb.din("negE", [128, 32 * 128])
        maug_in = kb.din("maug", [128, 2 * 65])
        selbias_in = kb.din("selbias", [128, 16 * 64])
        selinv_in = kb.din("selinv", [128, 16 * 64])
        Vb_in = kb.din("Vb", [16, LB])
        Vw_in = kb.din("Vw", [16, LB])
        Vwc_in = kb.din("Vwc", [16, LB])
        Vc_in = kb.din("Vc", [16, LC])
        Vc0_in = kb.din("Vc0", [16, LC])
        with contextlib.ExitStack() as ph:
          try:
              identb, identb_b = kb.sb(ph, "identb", [128, 128], BF16, dma=True)
              Jb, Jb_b = kb.sb(ph, "Jb", [128, 128], BF16, dma=True)
              selg, selg_b = kb.sb(ph, "selg_sb", [64, 48 * 64], BF16, dma=True)
              negE, negE_b = kb.sb(ph, "negE_sb", [128, 32 * 128], BF16, dma=True)
              maug, maug_b = kb.sb(ph, "maug_sb", [128, 2 * 65], BF16, dma=True)
              selbias, selbias_b = kb.sb(ph, "selbias_sb", [128, 16 * 64], F32, dma=True)
              rb31, rb31_b = kb.sb(ph, "rb31", [128, 16], F32, dma=True)
              qT_sb, qT_sbb = kb.sb(ph, "qT_sb", [128, 8, TOWN], BF16, dma=True)
              gT_sb, gT_sbb = kb.sb(ph, "gT_sb", [64, TOWN], BF16, dma=True)
              kccT = [kb.sb(ph, f"kccT{i}", [128, 256], BF16) for i in range(4)]
              vcc = [kb.sb(ph, f"vcc{i}", [128, 2, 64], BF16) for i in range(4)]
              ones64, ones64_b = kb.sb(ph, "ones64", [128, 64], BF16)
              S.op("dve", lambda e: e.memset(ones64[:], 1.0), writes=[ones64_b])
              S.dma("pool", identb[:], ident_in[:, :], [], identb_b)
              S.dma("pool", Jb[:], jmat_in[:, :], [], Jb_b)
              S.dma("pool", selg[:].rearrange("p (a b) -> p a b", b=1024), selg_in.rearrange("p (a b) -> p a b", b=1024), [], selg_b)
              S.dma("pool", negE[:].rearrange("p (a b) -> p a b", b=1024), negE_in.rearrange("p (a b) -> p a b", b=1024), [], negE_b)
              S.dma("pool", maug[:], maug_in[:, :], [], maug_b)
              S.dma("sp", selbias[:], selbias_in[:, :], [], selbias_b)
              selinv, selinv_b = kb.sb(ph, "selinv_sb", [128, 16 * 64], F32, dma=True)
              S.dma("sp", selinv[:], selinv_in[:, :], [], selinv_b)
              S.dma("sp", rb31[:], rel_bias[31:32, :].partition_broadcast(128), [], rb31_b)
              S.dma("sp", qT_sb[:], qT_d.rearrange("(c p) t -> p c t", p=128), [qT_b], qT_sbb)
              S.op("dve", lambda e: e.memset(gT_sb[:], 0.0), writes=[gT_sbb])
              S.dma("sp", gT_sb[0:48, :], gT_d[:, :], [gT_b], gT_sbb)

              with contextlib.ExitStack() as pd:
                  w1r, w1r_b = kb.sb(pd, "w1r", [64, 32, 256], BF16, dma=True)
                  posT, posT_b = kb.sb(pd, "posT", [64, 32], BF16, dma=True)
                  b1T, b1T_b = kb.sb(pd, "b1T", [128, 2], F32, dma=True)
                  w2d, w2d_b = kb.sb(pd, "w2d", [128, 2, 64], BF16, dma=True)
                  b2row, b2row_b = kb.sb(pd, "b2row", [128, 64], F32, dma=True)
                  biasH, biasH_b = kb.sb(pd, "biasH", [128, 2], F32)
                  kch, kch_b = kb.sb(pd, "kch", [64, TALL], BF16, dma=True)
                  hg = [kb.sb(pd, f"hg{i}", [128, 256], BF16) for i in range(2)]
                  kdup, kdup_b = kb.sb(pd, "kdup", [128, 128], BF16)
                  vtm, vtm_b = kb.sb(pd, "vtm", [128, 64], BF16)
                  for i in range(2):
                      S.op("dve", lambda e, i=i: e.memset(hg[i][0][:], 0.0), writes=[hg[i][1]])
                  for j in range(2 if CUT != 1 else 0):
                      S.dma("pool", w1r[:], cmp_w1[j].rearrange("(pos dh) hid -> dh pos hid", dh=64), [], w1r_b)
                      S.dma("pool", posT[:], cmp_posT[j], [], posT_b)
                      S.dma("sp", b1T[:], cmp_b1T[j], [], b1T_b)
                      S.dma("pool", w2d[:], cmp_w2[j].rearrange("(c p) d -> p c d", p=128), [], w2d_b)
                      S.dma("sp", b2row[:], cmp_b2[j:j + 1, :].partition_broadcast(128), [], b2row_b)
                      pst, psb = kb.ps[4]
                      for hc in range(2):
                          for pos in range(32):
                              S.op("pe", lambda e, hc=hc, pos=pos: e.matmul(
                                  pst[:, hc:hc + 1], lhsT=w1r[0:64, pos, hc * 128:(hc + 1) * 128], rhs=posT[0:64, pos:pos + 1],
                                  start=(pos == 0), stop=(pos == 31)), reads=[w1r_b, posT_b], writes=[psb])
                      S.op("dve", lambda e: e.tensor_tensor(out=biasH[:], in0=pst[:, 0:2], in1=b1T[:], op=ALU.add),
                           reads=[psb, b1T_b], writes=[biasH_b])
                      src_d, src_b = (kcT_d, kcT_b) if j == 0 else (vcT_d, vcT_b)
                      for kvh in range(4 if CUT != 2 else 0):
                          S.dma("sp", kch[:], src_d[kvh * 64:(kvh + 1) * 64, :], [src_b], kch_b)
                          for hc in range(2):
                              pst, psb = kb.psum()
                              for pos in range(32):
                                  S.op("pe", lambda e, hc=hc, pos=pos, pst=pst: e.matmul(
                                      pst[:, 0:255], lhsT=w1r[0:64, pos, hc * 128:(hc + 1) * 128],
                                      rhs=kch[0:64, pos:pos + 4065:16], start=(pos == 0), stop=(pos == 31)),
                                      reads=[w1r_b, kch_b], writes=[psb])
                              S.op("act", lambda e, hc=hc, pst=pst: e.activation(
                                  out=hg[hc][0][:, 0:255], in_=pst[:, 0:255], func=AF.Gelu_apprx_tanh,
                                  bias=biasH[:, hc:hc + 1], scale=1.0), reads=[psb, biasH_b], writes=[hg[hc][1]])
                          for nch in range(0 if (CUT == 3 or (CUT == 4 and j == 1) or (CUT == 5 and j == 0)) else 2):
                              pst, psb = kb.psum()
                              for hc in range(2):
                                  S.op("pe", lambda e, hc=hc, nch=nch, pst=pst: e.matmul(
                                      pst[:, 0:64], lhsT=hg[hc][0][:, nch * 128:(nch + 1) * 128], rhs=w2d[:, hc, :],
                                      start=(hc == 0), stop=(hc == 1)), reads=[hg[hc][1], w2d_b], writes=[psb])
                              pst3, psb3 = kb.psum()
                              if j == 0:
                                  for hh in range(2):
                                      S.op("dve", lambda e, hh=hh, pst=pst: e.tensor_tensor(
                                          out=kdup[:, hh * 64:(hh + 1) * 64], in0=pst[:, 0:64], in1=b2row[:], op=ALU.add),
                                          reads=[psb, b2row_b], writes=[kdup_b])
                                  S.op("pe", lambda e, pst3=pst3: e.matmul(pst3[:, 0:128], lhsT=kdup[:], rhs=Jb[:], start=True, stop=True),
                                       reads=[kdup_b, Jb_b], writes=[psb3])
                                  S.op("act", lambda e, kvh=kvh, nch=nch, pst3=pst3: e.activation(
                                      out=kccT[kvh][0][:, nch * 128:(nch + 1) * 128], in_=pst3[:, 0:128], func=AF.Copy),
                                      reads=[psb3], writes=[kccT[kvh][1]])
                              else:
                                  S.op("dve", lambda e, pst=pst: e.tensor_tensor(out=vtm[:], in0=pst[:, 0:64], in1=b2row[:], op=ALU.add),
                                       reads=[psb, b2row_b], writes=[vtm_b])
                                  S.op("pe", lambda e, pst3=pst3: e.matmul(pst3[:, 0:64], lhsT=Jb[:], rhs=vtm[:], start=True, stop=True),
                                       reads=[vtm_b, Jb_b], writes=[psb3])
                                  S.op("act", lambda e, kvh=kvh, nch=nch, pst3=pst3: e.activation(
                                      out=vcc[kvh][0][:, nch, :], in_=pst3[:, 0:64], func=AF.Copy),
                                      reads=[psb3], writes=[vcc[kvh][1]])
                  kb.end(pd)

              ksT2, ksT2_b = kb.sb(ph, "ksT2", [128, TALL], BF16, dma=True)
              kwT2, kwT2_b = kb.sb(ph, "kwT2", [128, TALL], BF16, dma=True)
              vs_all, vs_all_b = kb.sb(ph, "vs_all", [128, 32, 256], BF16, dma=True)
              vw_all, vw_all_b = kb.sb(ph, "vw_all", [128, 32, 256], BF16, dma=True)
              notselT, notselT_b = kb.sb(ph, "notselT", [128, TOWN], BF16)
              oacc, oacc_b = kb.sb(ph, "oacc", [64, 4, TOWN], F32)
              impacc, impacc_b = kb.sb(ph, "impacc", [128, 16, 64], F32)
              btiles = [kb.sb(ph, f"btile{i}", [128, 512], F32, dma=True) for i in range(3)]
              sfp = [kb.sb(ph, f"sfp{i}", [128, 512], F32) for i in range(2)]
              pus = [kb.sb(ph, f"pu{i}", [128, 512], BF16) for i in range(4)]
              rec, rec_b = kb.sb(ph, "rec", [128, 512], F32)
              fac, fac_b = kb.sb(ph, "fac", [128, 512], F32)
              otmp, otmp_b = kb.sb(ph, "otmp", [128, 512], F32)
              rec2, rec2_b = kb.sb(ph, "rec2", [128, 1], F32)
              simp, simp_b = kb.sb(ph, "simp", [128, 64], F32)
              simp2, simp2_b = kb.sb(ph, "simp2", [128, 64], F32)
              m8a, m8a_b = kb.sb(ph, "m8a", [128, 8], F32)
              m8b, m8b_b = kb.sb(ph, "m8b", [128, 8], F32)
              nsel, nsel_b = kb.sb(ph, "nsel", [128, 128], BF16)
              obf, obf_b = kb.sb(ph, "obf", [64, 4, TOWN], BF16)
              cnt = {"bt": 0, "sf": 0, "pu": 0}
              pso, pso_b = kb.ps[6]
              psg, psg_b = kb.ps[5]
              psi, psi_b = kb.ps[4]
              psd, psd_b = kb.ps[3]
              psbf, psbf_b = kb.psbf

              def hankel(src_in, h, c0, pstep):
                  bt, bb = btiles[cnt["bt"] % 3]
                  cnt["bt"] += 1
                  src = bass.AP(tensor=src_in.tensor, offset=h * src_in.shape[1] + c0, ap=[[pstep, 128], [1, 512]])
                  S.dma("sp" if cnt["bt"] % 2 else "act", bt[:], src, [], bb)
                  return bt, bb

              def finish_branch(hq, br, tt, first):
                  g_ = hq % 4
                  tsl = slice(tt * 512, (tt + 1) * 512)
                  S.op("dve", lambda e: e.tensor_scalar(out=rec[0:64, :], in0=psd[0:64, :], scalar1=1e-30,
                                                        scalar2=None, op0=ALU.add), reads=[psd_b], writes=[rec_b])
                  S.op("dve", lambda e: e.reciprocal(out=rec[0:64, :], in_=rec[0:64, :]), reads=[rec_b], writes=[rec_b])
                  gi = hq * 3 + br
                  S.op("pe", lambda e: e.matmul(psg[0:64, :], lhsT=selg[0:64, gi * 64:(gi + 1) * 64], rhs=gT_sb[0:64, tsl],
                                                start=True, stop=True), reads=[selg_b, gT_sbb], writes=[psg_b])
                  S.op("dve", lambda e: e.tensor_tensor(out=fac[0:64, :], in0=psg[0:64, :], in1=rec[0:64, :], op=ALU.mult),
                       reads=[psg_b, rec_b], writes=[fac_b])
                  if first:
                      S.op("dve", lambda e: e.tensor_tensor(out=oacc[:, g_, tsl], in0=pso[0:64, :], in1=fac[0:64, :], op=ALU.mult),
                           reads=[pso_b, fac_b], writes=[oacc_b])
                  else:
                      S.op("dve", lambda e: e.tensor_tensor(out=otmp[0:64, :], in0=pso[0:64, :], in1=fac[0:64, :], op=ALU.mult),
                           reads=[pso_b, fac_b], writes=[otmp_b])
                      S.op("pool", lambda e: e.tensor_tensor(out=oacc[:, g_, tsl], in0=oacc[:, g_, tsl], in1=otmp[0:64, :], op=ALU.add),
                           reads=[otmp_b, oacc_b], writes=[oacc_b])

              for kvh in range(4 if sub >= 2 else 0):
                  for g in range(4):
                      hq = kvh * 4 + g
                      base = 64 * (hq % 2)
                      qc = hq // 2
                      for tt in range(4):
                          tsl = slice(tt * 512, (tt + 1) * 512)
                          pul = []
                          for nch in range(2):
                              c0 = OFFC + (2048 + 512 * tt - 2048 * nch - 2063)
                              bt, bb = hankel(Vc0_in if nch == 0 else Vc_in, hq, c0, 16)
                              pst, psb = kb.psum(3)
                              S.op("pe", lambda e, pst=pst, nch=nch: e.matmul(
                                  pst[:, :], lhsT=kccT[kvh][0][base:base + 64, nch * 128:(nch + 1) * 128],
                                  rhs=qT_sb[base:base + 64, qc, tsl], start=True, stop=True),
                                  reads=[kccT[kvh][1], qT_sbb], writes=[psb])
                              sf, sfb = sfp[cnt["sf"] % 2]
                              cnt["sf"] += 1
                              S.op("dve", lambda e, pst=pst, sf=sf, bt=bt: e.tensor_tensor(out=sf[:], in0=pst[:, :], in1=bt[:], op=ALU.add),
                                   reads=[psb, bb], writes=[sfb])
                              pu, pub = pus[cnt["pu"] % 4]
                              cnt["pu"] += 1
                              S.op("act", lambda e, sf=sf, pu=pu: e.activation(out=pu[:], in_=sf[:], func=AF.Exp),
                                   reads=[sfb], writes=[pub])
                              pul.append((pu, pub))
                          for nch in range(2):
                              S.op("pe", lambda e, nch=nch: e.matmul(pso[0:64, :], lhsT=vcc[kvh][0][:, nch, :], rhs=pul[nch][0][:],
                                                                     start=(nch == 0), stop=(nch == 1)),
                                   reads=[vcc[kvh][1], pul[nch][1]], writes=[pso_b])
                          for nch in range(2):
                              S.op("pe", lambda e, nch=nch: e.matmul(psd[0:64, :], lhsT=ones64[:], rhs=pul[nch][0][:],
                                                                     start=(nch == 0), stop=(nch == 1)),
                                   reads=[ones64_b, pul[nch][1]], writes=[psd_b])
                          finish_branch(hq, 0, tt, True)
                          for tsub in range(4):
                              ts = tt * 4 + tsub
                              for nch in range(2):
                                  S.op("pe", lambda e, nch=nch, tsub=tsub: e.matmul(
                                      psi[:, 0:65], lhsT=pul[nch][0][:, tsub * 128:(tsub + 1) * 128],
                                      rhs=maug[:, nch * 65:(nch + 1) * 65], start=(nch == 0), stop=(nch == 1)),
                                      reads=[pul[nch][1], maug_b], writes=[psi_b])
                              S.op("dve", lambda e: e.tensor_scalar(out=rec2[:], in0=psi[:, 64:65], scalar1=1e-30, scalar2=None,
                                                                    op0=ALU.add), reads=[psi_b], writes=[rec2_b])
                              S.op("dve", lambda e: e.reciprocal(out=rec2[:], in_=rec2[:]), reads=[rec2_b], writes=[rec2_b])
                              if g == 0:
                                  S.op("dve", lambda e, ts=ts: e.tensor_scalar(out=impacc[:, ts, :], in0=psi[:, 0:64], scalar1=rec2[:, 0:1],
                                                                               scalar2=None, op0=ALU.mult),
                                       reads=[psi_b, rec2_b], writes=[impacc_b])
                              else:
                                  S.op("dve", lambda e, ts=ts: e.scalar_tensor_tensor(
                                      out=impacc[:, ts, :], in0=psi[:, 0:64], scalar=rec2[:, 0:1], in1=impacc[:, ts, :],
                                      op0=ALU.mult, op1=ALU.add), reads=[psi_b, rec2_b, impacc_b], writes=[impacc_b])
                  for ts in range(16 if sub >= 3 else 0):
                      S.op("dve", lambda e, ts=ts: e.tensor_tensor(out=simp[:], in0=impacc[:, ts, :], in1=selbias[:, ts * 64:(ts + 1) * 64],
                                                                   op=ALU.add), reads=[impacc_b, selbias_b], writes=[simp_b])
                      S.op("dve", lambda e: e.max(out=m8a[:], in_=simp[:]), reads=[simp_b], writes=[m8a_b])
                      S.op("dve", lambda e: e.match_replace(out=simp2[:], in_to_replace=m8a[:], in_values=simp[:], imm_value=-3.0e38),
                           reads=[simp_b, m8a_b], writes=[simp2_b])
                      S.op("dve", lambda e: e.max(out=m8b[:], in_=simp2[:]), reads=[simp2_b], writes=[m8b_b])
                      for hh in range(2):
                          S.op("dve", lambda e, ts=ts, hh=hh: e.scalar_tensor_tensor(out=nsel[:, hh * 64:(hh + 1) * 64], in0=simp[:], scalar=m8b[:, 7:8],
                                                                                     in1=selinv[:, ts * 64:(ts + 1) * 64], op0=ALU.is_lt, op1=ALU.max),
                               reads=[simp_b, m8b_b, selinv_b], writes=[nsel_b])
                      S.op("pe", lambda e: e.transpose(out=psbf[:, 0:128], in_=nsel[:, :], identity=identb[:]),
                           reads=[nsel_b, identb_b], writes=[psbf_b])
                      S.op("act", lambda e, ts=ts: e.activation(out=notselT[:, ts * 128:(ts + 1) * 128], in_=psbf[:, 0:128], func=AF.Copy),
                           reads=[psbf_b], writes=[notselT_b])
                  for hh in range(2):
                      S.dma("sp", ksT2[hh * 64:(hh + 1) * 64, :], ksT_d[kvh * 64:(kvh + 1) * 64, :], [ksT_b], ksT2_b)
                      S.dma("act", kwT2[hh * 64:(hh + 1) * 64, :], kwT_d[kvh * 64:(kvh + 1) * 64, :], [kwT_b], kwT2_b)
                  if kvh == 0:
                      S.dma("sp", vs_all[:], vs_d.rearrange("(c p) d -> p c d", p=128), [vs_b], vs_all_b)
                      S.dma("act", vw_all[:], vw_d.rearrange("(c p) d -> p c d", p=128), [vw_b], vw_all_b)
                  for g in range(4 if sub >= 4 else 0):
                      hq = kvh * 4 + g
                      base = 64 * (hq % 2)
                      qc = hq // 2
                      for br in (1, 2):
                          kT, kTb = (ksT2, ksT2_b) if br == 1 else (kwT2, kwT2_b)
                          va, vab = (vs_all, vs_all_b) if br == 1 else (vw_all, vw_all_b)
                          for tt in range(4):
                              tsl = slice(tt * 512, (tt + 1) * 512)
                              sc_hi = 16 + 4 * tt + 3
                              sc_lo = 0 if br == 1 else 16 + 4 * tt - 4
                              for sc in range(sc_lo, sc_hi + 1):
                                  r = sc - (16 + 4 * tt)
                                  near = (r >= -1) if br == 1 else True
                                  pst, psb = kb.psum(3)
                                  S.op("pe", lambda e, pst=pst, sc=sc: e.matmul(
                                      pst[:, :], lhsT=kT[base:base + 64, sc * 128:(sc + 1) * 128], rhs=qT_sb[base:base + 64, qc, tsl],
                                      start=True, stop=(br == 2)), reads=[kTb, qT_sbb], writes=[psb])
                                  if br == 1:
                                      S.op("pe", lambda e, pst=pst, sc=sc: e.matmul(
                                          pst[:, :], lhsT=negE[base:base + 64, sc * 128:(sc + 1) * 128], rhs=notselT[base:base + 64, tsl],
                                          start=False, stop=True), reads=[negE_b, notselT_b], writes=[psb])
                                  pu, pub = pus[cnt["pu"] % 4]
                                  cnt["pu"] += 1
                                  if near:
                                      if br == 1:
                                          vin = Vb_in
                                      elif r >= 0:
                                          vin = Vb_in
                                      else:
                                          vin = Vwc_in if (tt == 0) else Vw_in
                                      c0 = OFFB + (2048 + 512 * tt - 128 * sc - 127)
                                      bt, bb = hankel(vin, hq, c0, 1)
                                      sf, sfb = sfp[cnt["sf"] % 2]
                                      cnt["sf"] += 1
                                      S.op("dve", lambda e, pst=pst, sf=sf, bt=bt: e.tensor_tensor(out=sf[:], in0=pst[:, :], in1=bt[:], op=ALU.add),
                                           reads=[psb, bb], writes=[sfb])
                                      S.op("act", lambda e, sf=sf, pu=pu: e.activation(out=pu[:], in_=sf[:], func=AF.Exp),
                                           reads=[sfb], writes=[pub])
                                  else:
                                      S.op("act", lambda e, pst=pst, pu=pu: e.activation(out=pu[:], in_=pst[:, :], func=AF.Exp,
                                                                                         bias=rb31[:, hq:hq + 1], scale=1.0),
                                           reads=[psb, rb31_b], writes=[pub])
                                  S.op("pe", lambda e, sc=sc, pu=pu: e.matmul(pso[0:64, :], lhsT=va[:, sc, kvh * 64:(kvh + 1) * 64], rhs=pu[:],
                                                                              start=(sc == sc_lo), stop=(sc == sc_hi)),
                                       reads=[vab, pub], writes=[pso_b])
                                  S.op("pe", lambda e, sc=sc, pu=pu: e.matmul(psd[0:64, :], lhsT=ones64[:], rhs=pu[:],
                                                                              start=(sc == sc_lo), stop=(sc == sc_hi)),
                                       reads=[ones64_b, pub], writes=[psd_b])
                              finish_branch(hq, br, tt, False)
                  S.op("act", lambda e: e.activation(out=obf[:], in_=oacc[:], func=AF.Copy), reads=[oacc_b], writes=[obf_b])
                  S.dma("sp", oT_d[kvh * 256:(kvh + 1) * 256, :].rearrange("(g p) t -> p g t", p=64), obf[:], [obf_b], oT_b)
              kb.end(ph)
          except _Stop:
              kb.end(ph)


    y2T_d, y2T_b = kb.dscratch("y2T_d", [1024, TOWN], BF16)
    TWO_PI = 2.0 * math.pi
    if stages >= 4 and 4 not in skip:
        lre_f = kb.din("lre_f", [1, 4096]); lim_f = kb.din("lim_f", [1, 4096]); lst_f = kb.din("lst_f", [1, 4096])
        lreT_in = kb.din("lreT", [128, 32]); limT_in = kb.din("limT", [128, 32]); lstT_in = kb.din("lstT", [128, 32])
        Bpre_in = kb.din("Bpad_re", [128, 4096]); Bpim_in = kb.din("Bpad_im", [128, 4096])
        Cpre_in = kb.din("Cpad_re", [128, 4096]); Cpim_in = kb.din("Cpad_im", [128, 4096])
        tau_in = kb.din("tau", [1, 512])
        s5dT_in = kb.din("s5dT", [128, 8])
        glu_w_in = kb.din("glu_w", [1024, 1024]); glubT_in = kb.din("glubT", [128, 8])
        with contextlib.ExitStack() as ph:
            Wre, Wre_b = kb.sb(ph, "Wre", [128, 4096], BF16)
            Wim, Wim_b = kb.sb(ph, "Wim", [128, 4096], BF16)
            Cre, Cre_b = kb.sb(ph, "Cre", [128, 4096], BF16, dma=True)
            Cim, Cim_b = kb.sb(ph, "Cim", [128, 4096], BF16, dma=True)
            S.dma("pool", Cre[:].rearrange("p (a b) -> p a b", b=1024), Cpre_in.rearrange("p (a b) -> p a b", b=1024), [], Cre_b)
            S.dma("pool", Cim[:].rearrange("p (a b) -> p a b", b=1024), Cpim_in.rearrange("p (a b) -> p a b", b=1024), [], Cim_b)
            tau, tau_b = kb.sb(ph, "tau_sb", [128, 512], F32, dma=True)
            S.dma("sp", tau[:], tau_in[0:1, :].partition_broadcast(128), [], tau_b)
            thT, thT_b = kb.sb(ph, "thT", [128, 32], F32)
            rT, rT_b = kb.sb(ph, "rT", [128, 32], F32)
            cQ, cQ_b = kb.sb(ph, "cQ", [128, 32], F32)
            sQ, sQ_b = kb.sb(ph, "sQ", [128, 32], F32)
            nsQ, nsQ_b = kb.sb(ph, "nsQ", [128, 32], F32)
            s5d, s5d_b = kb.sb(ph, "s5d", [128, 8], F32, dma=True)
            glub, glub_b = kb.sb(ph, "glub", [128, 8], F32, dma=True)
            S.dma("sp", s5d[:], s5dT_in[:, :], [], s5d_b)
            S.dma("sp", glub[:], glubT_in[:, :], [], glub_b)

            sc_cache = {}

            def sincos(ctx, A, Ab, N, nm):
                if nm not in sc_cache:
                    sc_cache[nm] = [kb.sb(ctx, nm + "ki", [128, N], mybir.dt.int32), kb.sb(ctx, nm + "kf", [128, N], F32),
                                    kb.sb(ctx, nm + "r", [128, N], F32), kb.sb(ctx, nm + "m", [128, N], F32),
                                    kb.sb(ctx, nm + "S", [128, N], F32), kb.sb(ctx, nm + "C", [128, N], F32)]
                (ki, ki_b), (kf, kf_b), (r, r_b), (mm, mm_b), (Sx, Sx_b), (Cx, Cx_b) = sc_cache[nm]
                S.op("dve", lambda e: e.tensor_scalar(out=ki[:], in0=A, scalar1=1.0 / TWO_PI, scalar2=None, op0=ALU.mult),
                     reads=[Ab], writes=[ki_b])
                S.op("dve", lambda e: e.tensor_copy(out=kf[:], in_=ki[:]), reads=[ki_b], writes=[kf_b])
                S.op("dve", lambda e: e.scalar_tensor_tensor(out=r[:], in0=kf[:], scalar=-TWO_PI, in1=A, op0=ALU.mult, op1=ALU.add),
                     reads=[kf_b, Ab], writes=[r_b])
                S.op("dve", lambda e: e.tensor_scalar(out=mm[:], in0=r[:], scalar1=math.pi, scalar2=-TWO_PI, op0=ALU.is_gt, op1=ALU.mult),
                     reads=[r_b], writes=[mm_b])
                S.op("dve", lambda e: e.tensor_tensor(out=r[:], in0=r[:], in1=mm[:], op=ALU.add), reads=[r_b, mm_b], writes=[r_b])
                S.op("dve", lambda e: e.tensor_scalar(out=mm[:], in0=r[:], scalar1=-math.pi, scalar2=TWO_PI, op0=ALU.is_lt, op1=ALU.mult),
                     reads=[r_b], writes=[mm_b])
                S.op("dve", lambda e: e.tensor_tensor(out=r[:], in0=r[:], in1=mm[:], op=ALU.add), reads=[r_b, mm_b], writes=[r_b])
                S.op("act", lambda e: e.activation(out=Sx[:], in_=r[:], func=AF.Sin), reads=[r_b], writes=[Sx_b])
                S.op("act", lambda e: e.activation(out=mm[:], in_=r[:], func=AF.Abs), reads=[r_b], writes=[mm_b])
                S.op("act", lambda e: e.activation(out=Cx[:], in_=mm[:], func=AF.Sin, scale=-1.0, bias=halfpi[:, 0:1]),
                     reads=[mm_b, halfpi_b], writes=[Cx_b])
                return Sx, Sx_b, Cx, Cx_b

            halfpi, halfpi_b = kb.sb(ph, "halfpi", [128, 1], F32)
            S.op("dve", lambda e: e.memset(halfpi[:], math.pi / 2), writes=[halfpi_b])

            with contextlib.ExitStack() as pp:
                lreT, lreT_b = kb.sb(pp, "lreT_sb", [128, 32], F32, dma=True)
                limT, limT_b = kb.sb(pp, "limT_sb", [128, 32], F32, dma=True)
                lstT, lstT_b = kb.sb(pp, "lstT_sb", [128, 32], F32, dma=True)
                aQ, aQ_b = kb.sb(pp, "aQ", [128, 32], F32)
                S.dma("sp", lreT[:], lreT_in[:, :], [], lreT_b)
                S.dma("sp", limT[:], limT_in[:, :], [], limT_b)
                S.dma("sp", lstT[:], lstT_in[:, :], [], lstT_b)
                S.op("act", lambda e: e.activation(out=lstT[:], in_=lstT[:], func=AF.Exp), reads=[lstT_b], writes=[lstT_b])
                S.op("dve", lambda e: e.tensor_tensor(out=thT[:], in0=limT[:], in1=lstT[:], op=ALU.mult), reads=[limT_b, lstT_b], writes=[thT_b])
                S.op("dve", lambda e: e.tensor_tensor(out=rT[:], in0=lreT[:], in1=lstT[:], op=ALU.mult), reads=[lreT_b, lstT_b], writes=[rT_b])
                S.op("act", lambda e: e.activation(out=rT[:], in_=rT[:], func=AF.Exp), reads=[rT_b], writes=[rT_b])
                S.op("dve", lambda e: e.tensor_scalar(out=aQ[:], in0=thT[:], scalar1=512.0, scalar2=None, op0=ALU.mult), reads=[thT_b], writes=[aQ_b])
                Sx, Sx_b, Cx, Cx_b = sincos(pp, aQ[:], aQ_b, 32, "q_")
                S.op("dve", lambda e: e.tensor_copy(out=sQ[:], in_=Sx[:]), reads=[Sx_b], writes=[sQ_b])
                S.op("dve", lambda e: e.tensor_copy(out=cQ[:], in_=Cx[:]), reads=[Cx_b], writes=[cQ_b])
                S.op("dve", lambda e: e.tensor_scalar(out=nsQ[:], in0=Sx[:], scalar1=-1.0, scalar2=None, op0=ALU.mult), reads=[Sx_b], writes=[nsQ_b])
                kb.end(pp)

            for pg in range(4):
                with contextlib.ExitStack() as pp:
                    NN = 1024
                    csl = slice(pg * NN, (pg + 1) * NN)
                    def T(nm, dma=False):
                        return kb.sb(pp, f"{nm}{pg}", [128, NN], F32, dma=dma)
                    lre, lre_b = T("lre", True); lim, lim_b = T("lim", True); st, st_b = T("st", True)
                    S.dma("sp", lre[:], lre_f[0:1, csl].partition_broadcast(128), [], lre_b)
                    S.dma("sp", lim[:], lim_f[0:1, csl].partition_broadcast(128), [], lim_b)
                    S.dma("sp", st[:], lst_f[0:1, csl].partition_broadcast(128), [], st_b)
                    bre, bre_b = T("bpre", True); bim, bim_b = T("bpim", True)
                    S.dma("act", bre[:], Bpre_in[:, csl], [], bre_b)
                    S.dma("act", bim[:], Bpim_in[:, csl], [], bim_b)
                    ar, ar_b = T("ar"); ai, ai_b = T("ai"); mag, mag_b = T("mag")
                    S.op("act", lambda e: e.activation(out=st[:], in_=st[:], func=AF.Exp), reads=[st_b], writes=[st_b])
                    S.op("dve", lambda e: e.tensor_tensor(out=ar[:], in0=lre[:], in1=st[:], op=ALU.mult), reads=[lre_b, st_b], writes=[ar_b])
                    S.op("dve", lambda e: e.tensor_tensor(out=ai[:], in0=lim[:], in1=st[:], op=ALU.mult), reads=[lim_b, st_b], writes=[ai_b])
                    S.op("act", lambda e: e.activation(out=mag[:], in_=ar[:], func=AF.Exp), reads=[ar_b], writes=[mag_b])
                    Sx, Sx_b, Cx, Cx_b = sincos(pp, ai[:], ai_b, NN, f"p{pg}_")
                    S.op("dve", lambda e: e.tensor_tensor(out=Cx[:], in0=Cx[:], in1=mag[:], op=ALU.mult), reads=[Cx_b, mag_b], writes=[Cx_b])
                    S.op("dve", lambda e: e.tensor_tensor(out=Sx[:], in0=Sx[:], in1=mag[:], op=ALU.mult), reads=[Sx_b, mag_b], writes=[Sx_b])
                    S.op("dve", lambda e: e.tensor_scalar(out=Cx[:], in0=Cx[:], scalar1=-1.0, scalar2=None, op0=ALU.add), reads=[Cx_b], writes=[Cx_b])
                    S.op("dve", lambda e: e.tensor_tensor(out=mag[:], in0=lre[:], in1=lre[:], op=ALU.mult), reads=[lre_b], writes=[mag_b])
                    S.op("dve", lambda e: e.tensor_tensor(out=ar[:], in0=lim[:], in1=lim[:], op=ALU.mult), reads=[lim_b], writes=[ar_b])
                    S.op("dve", lambda e: e.tensor_tensor(out=mag[:], in0=mag[:], in1=ar[:], op=ALU.add), reads=[mag_b, ar_b], writes=[mag_b])
                    S.op("dve", lambda e: e.reciprocal(out=mag[:], in_=mag[:]), reads=[mag_b], writes=[mag_b])
                    S.op("dve", lambda e: e.tensor_tensor(out=ar[:], in0=Cx[:], in1=lre[:], op=ALU.mult), reads=[Cx_b, lre_b], writes=[ar_b])
                    S.op("dve", lambda e: e.tensor_tensor(out=st[:], in0=Sx[:], in1=lim[:], op=ALU.mult), reads=[Sx_b, lim_b], writes=[st_b])
                    S.op("dve", lambda e: e.tensor_tensor(out=ar[:], in0=ar[:], in1=st[:], op=ALU.add), reads=[ar_b, st_b], writes=[ar_b])
                    S.op("dve", lambda e: e.tensor_tensor(out=ar[:], in0=ar[:], in1=mag[:], op=ALU.mult), reads=[ar_b, mag_b], writes=[ar_b])
                    S.op("dve", lambda e: e.tensor_tensor(out=ai[:], in0=Sx[:], in1=lre[:], op=ALU.mult), reads=[Sx_b, lre_b], writes=[ai_b])
                    S.op("dve", lambda e: e.tensor_tensor(out=st[:], in0=Cx[:], in1=lim[:], op=ALU.mult), reads=[Cx_b, lim_b], writes=[st_b])
                    S.op("dve", lambda e: e.tensor_tensor(out=ai[:], in0=ai[:], in1=st[:], op=ALU.subtract), reads=[ai_b, st_b], writes=[ai_b])
                    S.op("dve", lambda e: e.tensor_tensor(out=ai[:], in0=ai[:], in1=mag[:], op=ALU.mult), reads=[ai_b, mag_b], writes=[ai_b])
                    S.op("dve", lambda e: e.tensor_tensor(out=st[:], in0=ar[:], in1=bre[:], op=ALU.mult), reads=[ar_b, bre_b], writes=[st_b])
                    S.op("dve", lambda e: e.tensor_tensor(out=mag[:], in0=ai[:], in1=bim[:], op=ALU.mult), reads=[ai_b, bim_b], writes=[mag_b])
                    S.op("dve", lambda e: e.tensor_tensor(out=Wre[:, csl], in0=st[:], in1=mag[:], op=ALU.subtract), reads=[st_b, mag_b], writes=[Wre_b])
                    S.op("dve", lambda e: e.tensor_tensor(out=st[:], in0=ar[:], in1=bim[:], op=ALU.mult), reads=[ar_b, bim_b], writes=[st_b])
                    S.op("dve", lambda e: e.tensor_tensor(out=mag[:], in0=ai[:], in1=bre[:], op=ALU.mult), reads=[ai_b, bre_b], writes=[mag_b])
                    S.op("dve", lambda e: e.tensor_tensor(out=Wim[:, csl], in0=st[:], in1=mag[:], op=ALU.add), reads=[st_b, mag_b], writes=[Wim_b])
                    kb.end(pp)

            ang, ang_b = kb.sb(ph, "ang", [128, 512], F32)
            rt, rt_b = kb.sb(ph, "rt", [128, 512], F32)
            ones512, ones512_b = kb.sb(ph, "ones512", [128, 512], F32)
            S.op("dve", lambda e: e.memset(ones512[:], 1.0), writes=[ones512_b])
            uTs = [kb.sb(ph, f"uTs{i}", [128, TALL], BF16, dma=True) for i in range(2)]
            xre, xre_b = kb.sb(ph, "xre", [128, 4, TOWN], BF16)
            nxim, nxim_b = kb.sb(ph, "nxim", [128, 4, TOWN], BF16)
            yg, yg_b = kb.sb(ph, "yg", [128, 8, TOWN], BF16)
            wk = {n: kb.sb(ph, "wk_" + n, [128, 512], F32) for n in ("t1", "t2", "t3", "t4", "bre", "bim", "o1", "o2", "o3", "o4", "ysb")}
            wre2 = [kb.sb(ph, f"wre{i}", [128, 512], F32) for i in range(2)]
            wim2 = [kb.sb(ph, f"wim{i}", [128, 512], F32) for i in range(2)]
            ini_re, ini_re_b = kb.sb(ph, "ini_re", [128, 1], F32)
            ini_im, ini_im_b = kb.sb(ph, "ini_im", [128, 1], F32)
            itmp, itmp_b = kb.sb(ph, "itmp", [128, 2], F32)
            with contextlib.ExitStack() as sc_ctx:
                for cc in range(8):
                    uT, uT_sbb = uTs[cc % 2]
                    S.dma("sp", uT[:], uT_d[cc * 128:(cc + 1) * 128, :], [uT_b], uT_sbb)
                    for pl in range(4):
                        pr = cc * 4 + pl
                        if True:
                            pctx = ph
                            S.op("dve", lambda e, pr=pr: e.tensor_scalar(out=ang[:], in0=tau[:], scalar1=thT[:, pr:pr + 1], scalar2=None, op0=ALU.mult),
                                 reads=[tau_b, thT_b], writes=[ang_b])
                            Sx, Sx_b, Cx, Cx_b = sincos(pctx, ang[:], ang_b, 512, "tmain_")
                            S.op("dve", lambda e, pr=pr: e.tensor_scalar(out=rt[:], in0=ones512[:], scalar1=rT[:, pr:pr + 1], scalar2=None, op0=ALU.mult),
                                 reads=[ones512_b, rT_b], writes=[rt_b])
                            for j in range(8):
                                ps_re, ps_re_b = kb.psum()
                                ps_im, ps_im_b = kb.psum()
                                S.op("pe", lambda e, ps_re=ps_re, j=j, pr=pr: e.matmul(
                                    ps_re[:, :], lhsT=Wre[:, pr * 128:(pr + 1) * 128], rhs=uT[:, j * 512:(j + 1) * 512], start=True, stop=True),
                                    reads=[Wre_b, uT_sbb], writes=[ps_re_b])
                                S.op("pe", lambda e, ps_im=ps_im, j=j, pr=pr: e.matmul(
                                    ps_im[:, :], lhsT=Wim[:, pr * 128:(pr + 1) * 128], rhs=uT[:, j * 512:(j + 1) * 512], start=True, stop=True),
                                    reads=[Wim_b, uT_sbb], writes=[ps_im_b])
                                t1, t1b = wk["t1"]; t2, t2b = wk["t2"]; t3, t3b = wk["t3"]; t4, t4b = wk["t4"]
                                bre_, breb = wk["bre"]; bim_, bimb = wk["bim"]
                                S.op("dve", lambda e, ps_re=ps_re: e.tensor_tensor(out=t1[:], in0=ps_re[:, :], in1=Cx[:], op=ALU.mult), reads=[ps_re_b, Cx_b], writes=[t1b])
                                S.op("dve", lambda e, ps_im=ps_im: e.tensor_tensor(out=t2[:], in0=ps_im[:, :], in1=Sx[:], op=ALU.mult), reads=[ps_im_b, Sx_b], writes=[t2b])
                                S.op("pool", lambda e: e.tensor_tensor(out=bre_[:], in0=t1[:], in1=t2[:], op=ALU.add), reads=[t1b, t2b], writes=[breb])
                                S.op("dve", lambda e, ps_im=ps_im: e.tensor_tensor(out=t3[:], in0=ps_im[:, :], in1=Cx[:], op=ALU.mult), reads=[ps_im_b, Cx_b], writes=[t3b])
                                S.op("dve", lambda e, ps_re=ps_re: e.tensor_tensor(out=t4[:], in0=ps_re[:, :], in1=Sx[:], op=ALU.mult), reads=[ps_re_b, Sx_b], writes=[t4b])
                                S.op("pool", lambda e: e.tensor_tensor(out=bim_[:], in0=t3[:], in1=t4[:], op=ALU.subtract), reads=[t3b, t4b], writes=[bimb])
                                wre, wreb = wre2[j % 2]
                                wim, wimb = wim2[j % 2]
                                if j == 0:
                                    S.op("dve", lambda e, wre=wre: e.tensor_tensor_scan(out=wre[:], data0=rt[:], data1=bre_[:], initial=0.0, op0=ALU.mult, op1=ALU.add),
                                         reads=[rt_b, breb], writes=[wreb])
                                    S.op("dve", lambda e, wim=wim: e.tensor_tensor_scan(out=wim[:], data0=rt[:], data1=bim_[:], initial=0.0, op0=ALU.mult, op1=ALU.add),
                                         reads=[rt_b, bimb], writes=[wimb])
                                else:
                                    S.op("dve", lambda e, wre=wre: e.tensor_tensor_scan(out=wre[:], data0=rt[:], data1=bre_[:], initial=ini_re[:, 0:1], op0=ALU.mult, op1=ALU.add),
                                         reads=[rt_b, breb, ini_re_b], writes=[wreb])
                                    S.op("dve", lambda e, wim=wim: e.tensor_tensor_scan(out=wim[:], data0=rt[:], data1=bim_[:], initial=ini_im[:, 0:1], op0=ALU.mult, op1=ALU.add),
                                         reads=[rt_b, bimb, ini_im_b], writes=[wimb])
                                if j < 7:
                                    S.op("dve", lambda e, wre=wre, pr=pr: e.tensor_scalar(out=itmp[:, 0:1], in0=wre[:, 511:512], scalar1=cQ[:, pr:pr + 1], scalar2=None, op0=ALU.mult),
                                         reads=[wreb, cQ_b], writes=[itmp_b])
                                    S.op("dve", lambda e, wre=wre, pr=pr: e.tensor_scalar(out=itmp[:, 1:2], in0=wre[:, 511:512], scalar1=sQ[:, pr:pr + 1], scalar2=None, op0=ALU.mult),
                                         reads=[wreb, sQ_b], writes=[itmp_b])
                                    S.op("dve", lambda e, wim=wim, pr=pr: e.scalar_tensor_tensor(out=ini_re[:], in0=wim[:, 511:512], scalar=nsQ[:, pr:pr + 1], in1=itmp[:, 0:1], op0=ALU.mult, op1=ALU.add),
                                         reads=[wimb, nsQ_b, itmp_b], writes=[ini_re_b])
                                    S.op("dve", lambda e, wim=wim, pr=pr: e.scalar_tensor_tensor(out=ini_im[:], in0=wim[:, 511:512], scalar=cQ[:, pr:pr + 1], in1=itmp[:, 1:2], op0=ALU.mult, op1=ALU.add),
                                         reads=[wimb, cQ_b, itmp_b], writes=[ini_im_b])
                                if j >= 4:
                                    tsl = slice((j - 4) * 512, (j - 3) * 512)
                                    o1, o1b = wk["o1"]; o2, o2b = wk["o2"]; o3, o3b = wk["o3"]; o4, o4b = wk["o4"]
                                    S.op("dve", lambda e, wre=wre: e.tensor_tensor(out=o1[:], in0=wre[:], in1=Cx[:], op=ALU.mult), reads=[wreb, Cx_b], writes=[o1b])
                                    S.op("pool", lambda e, wim=wim: e.tensor_tensor(out=o2[:], in0=wim[:], in1=Sx[:], op=ALU.mult), reads=[wimb, Sx_b], writes=[o2b])
                                    S.op("pool", lambda e, tsl=tsl, pl=pl: e.tensor_tensor(out=xre[:, pl, tsl], in0=o1[:], in1=o2[:], op=ALU.subtract), reads=[o1b, o2b], writes=[xre_b])
                                    S.op("dve", lambda e, wre=wre: e.tensor_tensor(out=o3[:], in0=wre[:], in1=Sx[:], op=ALU.mult), reads=[wreb, Sx_b], writes=[o3b])
                                    S.op("pool", lambda e, wim=wim: e.tensor_tensor(out=o4[:], in0=wim[:], in1=Cx[:], op=ALU.mult), reads=[wimb, Cx_b], writes=[o4b])
                                    S.op("dve", lambda e, tsl=tsl, pl=pl: e.scalar_tensor_tensor(out=nxim[:, pl, tsl], in0=o3[:], scalar=-1.0, in1=o4[:], op0=ALU.mult, op1=ALU.subtract),
                                         reads=[o3b, o4b], writes=[nxim_b])
                    for tt in range(4):
                        tsl = slice(tt * 512, (tt + 1) * 512)
                        pst, psb = kb.psum()
                        for pl in range(4):
                            pr = cc * 4 + pl
                            S.op("pe", lambda e, pl=pl, pr=pr, pst=pst, tsl=tsl: e.matmul(pst[:, :], lhsT=Cre[:, pr * 128:(pr + 1) * 128], rhs=xre[:, pl, tsl],
                                                                                         start=(pl == 0), stop=False), reads=[Cre_b, xre_b], writes=[psb])
                            S.op("pe", lambda e, pl=pl, pr=pr, pst=pst, tsl=tsl: e.matmul(pst[:, :], lhsT=Cim[:, pr * 128:(pr + 1) * 128], rhs=nxim[:, pl, tsl],
                                                                                         start=False, stop=(pl == 3)), reads=[Cim_b, nxim_b], writes=[psb])
                        ysb, ysbb = wk["ysb"]
                        S.op("dve", lambda e, pst=pst, tt=tt, cc=cc, uT=uT: e.scalar_tensor_tensor(
                            out=ysb[:], in0=uT[:, TALL - TOWN + tt * 512:TALL - TOWN + (tt + 1) * 512], scalar=s5d[:, cc:cc + 1], in1=pst[:, :],
                            op0=ALU.mult, op1=ALU.add), reads=[uT_sbb, s5d_b, psb], writes=[ysbb])
                        S.op("act", lambda e, cc=cc, tsl=tsl: e.activation(out=yg[:, cc, tsl], in_=ysb[:], func=AF.Gelu_apprx_tanh),
                             reads=[ysbb], writes=[yg_b])
            S.barrier()
            with contextlib.ExitStack() as pg_:
                gw, gw_b = kb.sb(pg_, "gw", [128, 8, 1024], BF16, dma=True)
                S.dma("pool", gw[:], glu_w_in.rearrange("(c p) o -> p c o", p=128), [], gw_b)
                sg = [kb.sb(pg_, f"sg{i}", [128, 512], BF16) for i in range(2)]
                y2 = [kb.sb(pg_, f"y2_{i}", [128, 512], BF16) for i in range(2)]
                n = 0
                for co in range(8):
                    for tt in range(4):
                        tsl = slice(tt * 512, (tt + 1) * 512)
                        pst, psb = kb.psum()
                        for cc in range(8):
                            S.op("pe", lambda e, cc=cc, co=co, pst=pst, tsl=tsl: e.matmul(pst[:, :], lhsT=gw[:, cc, co * 128:(co + 1) * 128], rhs=yg[:, cc, tsl],
                                                                                         start=(cc == 0), stop=(cc == 7)), reads=[gw_b, yg_b], writes=[psb])
                        sgt, sgb = sg[n % 2]
                        y2t, y2b = y2[n % 2]
                        n += 1
                        S.op("act", lambda e, pst=pst, sgt=sgt, co=co: e.activation(out=sgt[:], in_=pst[:, :], func=AF.Sigmoid, bias=glub[:, co:co + 1], scale=1.0),
                             reads=[psb, glub_b], writes=[sgb])
                        S.op("dve", lambda e, sgt=sgt, y2t=y2t, co=co, tsl=tsl: e.tensor_tensor(out=y2t[:], in0=sgt[:], in1=yg[:, co, tsl], op=ALU.mult),
                             reads=[sgb, yg_b], writes=[y2b])
                        S.dma("sp", y2T_d[co * 128:(co + 1) * 128, tsl], y2t[:], [y2b], y2T_b)
                kb.end(pg_)
            kb.end(ph)


    x1T_d, x1T_b = kb.dscratch("x1T_d", [D, TOWN], F32)
    x2T_d, x2T_b = kb.dscratch("x2T_d", [D, TOWN], F32)
    if stages >= 5 and 5 not in skip:
        wua_in = kb.din("w_up_attn", [1024, D]); wus_in = kb.din("w_up_ssm", [1024, D]); wout_in = kb.din("w_out", [D, D])
        with contextlib.ExitStack() as ph:
            wua, wua_b = kb.sb(ph, "wua", [128, 8, D], BF16, dma=True)
            wus, wus_b = kb.sb(ph, "wus", [128, 8, D], BF16, dma=True)
            for c4 in range(4):
                S.dma("pool", wua[:, :, c4 * 512:(c4 + 1) * 512], wua_in.rearrange("(c p) o -> p c o", p=128)[:, :, c4 * 512:(c4 + 1) * 512], [], wua_b)
                S.dma("pool", wus[:, :, c4 * 512:(c4 + 1) * 512], wus_in.rearrange("(c p) o -> p c o", p=128)[:, :, c4 * 512:(c4 + 1) * 512], [], wus_b)
            wo2 = [kb.sb(ph, f"wo{i}", [128, KC, 128], BF16, dma=True) for i in range(2)]
            gts, gts_b = kb.sb(ph, "gts", [128, 32, 512], BF16, dma=True)
            oTt, oTt_b = kb.sb(ph, "oTt", [128, 8, 512], BF16, dma=True)
            yTt, yTt_b = kb.sb(ph, "yTt", [128, 8, 512], BF16, dma=True)
            mixed, mixed_b = kb.sb(ph, "mixed", [128, KC, 512], BF16)
            xt2 = [kb.sb(ph, f"xres{i}", [128, 512], F32, dma=True) for i in range(2)]
            x1t2 = [kb.sb(ph, f"x1t{i}", [128, 512], F32) for i in range(2)]
            mt1 = [kb.sb(ph, f"mt1_{i}", [128, 512], F32) for i in range(2)]
            mt2 = [kb.sb(ph, f"mt2_{i}", [128, 512], F32) for i in range(2)]
            woutv = wout_in.rearrange("(c p) o -> p c o", p=128)
            xTv_own = xT.rearrange("(k p) t -> p k t", p=128)
            n = 0
            for tt in range(4):
                tsl = slice(tt * 512, (tt + 1) * 512)
                S.dma("sp", gts[:], mgT_d.rearrange("(c p) t -> p c t", p=128)[:, :, tsl], [mgT_b], gts_b)
                S.dma("sp", oTt[:], oT_d.rearrange("(c p) t -> p c t", p=128)[:, :, tsl], [oT_b], oTt_b)
                S.dma("act", yTt[:], y2T_d.rearrange("(c p) t -> p c t", p=128)[:, :, tsl], [y2T_b], yTt_b)
                for dm in range(KC):
                    psA, psA_b = kb.psum()
                    psB, psB_b = kb.psum()
                    for cc in range(8):
                        S.op("pe", lambda e, cc=cc, dm=dm, psA=psA: e.matmul(psA[:, :], lhsT=wua[:, cc, dm * 128:(dm + 1) * 128], rhs=oTt[:, cc, :],
                                                                           start=(cc == 0), stop=(cc == 7)), reads=[wua_b, oTt_b], writes=[psA_b])
                    for cc in range(8):
                        S.op("pe", lambda e, cc=cc, dm=dm, psB=psB: e.matmul(psB[:, :], lhsT=wus[:, cc, dm * 128:(dm + 1) * 128], rhs=yTt[:, cc, :],
                                                                           start=(cc == 0), stop=(cc == 7)), reads=[wus_b, yTt_b], writes=[psB_b])
                    a1, a1b = mt1[dm % 2]
                    a2, a2b = mt2[dm % 2]
                    S.op("dve", lambda e, psA=psA, a1=a1, dm=dm: e.tensor_tensor(out=a1[:], in0=psA[:, :], in1=gts[:, dm, :], op=ALU.mult),
                         reads=[psA_b, gts_b], writes=[a1b])
                    S.op("dve", lambda e, psB=psB, a2=a2, dm=dm: e.tensor_tensor(out=a2[:], in0=psB[:, :], in1=gts[:, 16 + dm, :], op=ALU.mult),
                         reads=[psB_b, gts_b], writes=[a2b])
                    S.op("pool", lambda e, a1=a1, a2=a2, dm=dm: e.tensor_tensor(out=mixed[:, dm, :], in0=a1[:], in1=a2[:], op=ALU.add),
                         reads=[a1b, a2b], writes=[mixed_b])
                for do in range(KC):
                    wo, wo_b = wo2[n % 2]
                    xr, xr_b = xt2[n % 2]
                    x1t, x1t_b = x1t2[n % 2]
                    n += 1
                    S.dma("pool", wo[:], woutv[:, :, do * 128:(do + 1) * 128], [], wo_b)
                    S.dma("act", xr[:], xTv_own[:, do, TALL - TOWN + tt * 512:TALL - TOWN + (tt + 1) * 512], [], xr_b)
                    pst, psb = kb.psum()
                    for dm in range(KC):
                        S.op("pe", lambda e, dm=dm, pst=pst, wo=wo: e.matmul(pst[:, :], lhsT=wo[:, dm, :], rhs=mixed[:, dm, :],
                                                                           start=(dm == 0), stop=(dm == KC - 1)), reads=[wo_b, mixed_b], writes=[psb])
                    S.op("dve", lambda e, pst=pst, do=do, xr=xr, x1t=x1t: e.scalar_tensor_tensor(
                        out=x1t[:], in0=pst[:, :], scalar=mod[:, G1 + do:G1 + do + 1], in1=xr[:], op0=ALU.mult, op1=ALU.add),
                        reads=[psb, mod_b, xr_b], writes=[x1t_b])
                    S.dma("sp", x1T_d[do * 128:(do + 1) * 128, tsl], x1t[:], [x1t_b], x1T_b)
            kb.end(ph)

    x2src_d, x2src_b = (x1T_d, x1T_b)
    if stages >= 6 and 6 not in skip:
        x2src_d, x2src_b = (x2T_d, x2T_b)
        wq_in = kb.din("peer_w_q", [D, D])
        keysT_in = kb.din("keysT", [128, 16 * 128])
        uT_in = kb.din("peer_uT", [D, 16384])
        v_in = kb.din("peer_v", [16384, D])
        h2T_d, h2T_b = kb.dscratch("h2T_d", [D, TOWN], BF16)
        qpT_d, qpT_b = kb.dscratch("qpT_d", [D, TOWN], BF16)
        G_d, G_b = kb.dscratch("G_d", [16, 128, 16384], BF16)
        uTb_d, uTb_b = kb.dscratch("uTb_d", [32, 128, KC * 512], BF16)
        vb_d, vb_b = kb.dscratch("vb_d", [32, 128, 4 * D], BF16)
        with contextlib.ExitStack() as ph:
            TB = 1024
            hT, hT_b = kb.sb(ph, "p_hT", [128, KC, TB], BF16)
            xbufs = [kb.sb(ph, f"p_xb{i}", [128, KC, 256], F32, dma=True) for i in range(2)]
            tmpbufs = [kb.sb(ph, f"p_ntmp{i}", [128, 256], F32) for i in range(2)]
            sqbuf = kb.sb(ph, "p_sqb", [128, KC, 256], BF16)
            rbuf = kb.sb(ph, "p_rstd", [128, 256], F32)
            wbufs = [kb.sb(ph, f"p_wbuf{i}", [128, KC, 512], BF16, dma=True) for i in range(2)]
            obufs = [kb.sb(ph, f"p_obuf{i}", [128, 512], BF16) for i in range(4)]
            x1v = x1T_d.rearrange("(k p) t -> p k t", p=128)
            wqv = wq_in.rearrange("(k p) c -> p k c", p=128)
            n = 0
            for blk in range(TOWN // TB):
                tb0 = blk * TB

                def src_fn(t, nn):
                    return x1v[:, :, t:t + nn]
                norm_block(ph, src_fn, tb0, TB, hT, hT_b, gm2, gm2_b, SH2, xbufs, tmpbufs, sqbuf, rbuf)
                S.dma("act", h2T_d.rearrange("(k p) t -> p k t", p=128)[:, :, tb0:tb0 + TB], hT[:], [hT_b], h2T_b)
                for cg in range(4):
                    wt, wb = wbufs[cg % 2]
                    S.dma("pool", wt[:], wqv[:, :, cg * 512:(cg + 1) * 512], [], wb)
                    for cc in range(4):
                        for tt in range(TB // 512):
                            pst, psb = kb.psum()
                            for k in range(KC):
                                S.op("pe", lambda e, k=k, cc=cc, tt=tt, pst=pst, wt=wt: e.matmul(
                                    pst[:, :], lhsT=wt[:, k, cc * 128:(cc + 1) * 128], rhs=hT[:, k, tt * 512:(tt + 1) * 512],
                                    start=(k == 0), stop=(k == KC - 1)), reads=[wb, hT_b], writes=[psb])
                            ot, ob = obufs[n % 4]
                            n += 1
                            S.op("act", lambda e, pst=pst, ot=ot: e.activation(out=ot[:], in_=pst[:, :], func=AF.Copy), reads=[psb], writes=[ob])
                            r0 = cg * 512 + cc * 128
                            S.dma("sp", qpT_d[r0:r0 + 128, tb0 + tt * 512:tb0 + (tt + 1) * 512], ot[:], [ob], qpT_b)
            kb.end(ph)
        with contextlib.ExitStack() as ph:
            keysT, keysT_b = kb.sb(ph, "keysT_sb", [128, 16, 128], BF16, dma=True)
            S.dma("pool", keysT[:], keysT_in.rearrange("p (a n) -> p a n", n=128), [], keysT_b)
            qts = [kb.sb(ph, f"qts{i}", [128, 16, 128], BF16, dma=True) for i in range(2)]
            s_all, s_all_b = kb.sb(ph, "s_all", [128, 16, 128], F32)
            v16, v16_b = kb.sb(ph, "v16", [128, 16, 16], F32)
            mtmp, mtmp_b = kb.sb(ph, "mtmp", [128, 128], F32)
            cand, cand_b = kb.sb(ph, "cand", [128, 256], F32)
            ctmp, ctmp_b = kb.sb(ph, "ctmp", [128, 256], F32)
            e256, e256_b = kb.sb(ph, "e256", [128, 256], F32)
            c1, c1_b = kb.sb(ph, "c1", [128, 8], F32)
            c2, c2_b = kb.sb(ph, "c2", [128, 8], F32)
            sm, sm_b = kb.sb(ph, "smalls", [128, 8], F32)
            Gs = [kb.sb(ph, f"Gs{i}", [128, 16384], BF16) for i in range(2)]
            Sd2 = [kb.sb(ph, f"Sd{i}", [128, 16, 128], F32) for i in range(4)]
            Ed2 = [kb.sb(ph, f"Ed{i}", [128, 16, 128], BF16) for i in range(4)]
            Md2 = [kb.sb(ph, f"Md{i}", [128, 16, 128], BF16) for i in range(4)]
            thr8, thr8_b = kb.sb(ph, "thr8", [128, 8], F32)
            nb8, nb8_b = kb.sb(ph, "nb8", [128, 8], F32)
            identg, identg_b = kb.sb(ph, "identg", [128, 128], BF16, dma=True)
            ident_src2 = kb.inputs["ident"] if "ident" in kb.inputs else kb.din("ident", [128, 128])
            S.dma("pool", identg[:], ident_src2[:, :], [], identg_b)
            qpv = qpT_d.rearrange("(a p) t -> p a t", p=128)
            nn = 0
            for ts in range(16):
                qt, qtb = qts[ts % 2]
                S.dma("sp", qt[:], qpv[:, :, ts * 128:(ts + 1) * 128], [qpT_b], qtb)
                for b4 in range(4):
                    pst, psb = kb.psum()
                    for a in range(4):
                        hc = b4 * 4 + a
                        S.op("pe", lambda e, hc=hc, a=a, pst=pst, qt=qt: e.matmul(pst[:, a * 128:(a + 1) * 128], lhsT=qt[:, hc, :], rhs=keysT[:, hc, :],
                                                                                 start=True, stop=True), reads=[qtb, keysT_b], writes=[psb])
                    S.op("act", lambda e, b4=b4, pst=pst: e.activation(out=s_all[:, b4 * 4:(b4 + 1) * 4, :].rearrange("p a n -> p (a n)"), in_=pst[:, :], func=AF.Copy),
                         reads=[psb], writes=[s_all_b])
                for hc in range(16):
                    S.op("dve", lambda e, hc=hc: e.max(out=v16[:, hc, 0:8], in_=s_all[:, hc, :]), reads=[s_all_b], writes=[v16_b])
                    S.op("dve", lambda e, hc=hc: e.match_replace(out=mtmp[:], in_to_replace=v16[:, hc, 0:8], in_values=s_all[:, hc, :], imm_value=-1.0e30),
                         reads=[s_all_b, v16_b], writes=[mtmp_b])
                    S.op("dve", lambda e, hc=hc: e.max(out=v16[:, hc, 8:16], in_=mtmp[:]), reads=[mtmp_b], writes=[v16_b])
                G, G_sb = Gs[ts % 2]
                for h in range(8):
                    S.op("dve", lambda e, h=h: e.tensor_tensor(
                        out=cand[:].rearrange("p (a b) -> p a b", a=16),
                        in0=v16[:, 2 * h, :].unsqueeze(2).to_broadcast([128, 16, 16]),
                        in1=v16[:, 2 * h + 1, :].unsqueeze(1).to_broadcast([128, 16, 16]), op=ALU.add),
                        reads=[v16_b], writes=[cand_b])
                    S.op("dve", lambda e: e.max(out=c1[:], in_=cand[:]), reads=[cand_b], writes=[c1_b])
                    S.op("dve", lambda e: e.match_replace(out=ctmp[:], in_to_replace=c1[:], in_values=cand[:], imm_value=-1.0e30),
                         reads=[cand_b, c1_b], writes=[ctmp_b])
                    S.op("dve", lambda e: e.max(out=c2[:], in_=ctmp[:]), reads=[ctmp_b], writes=[c2_b])
                    S.op("dve", lambda e, h=h: e.tensor_copy(out=thr8[:, h:h + 1], in_=c2[:, 7:8]), reads=[c2_b], writes=[thr8_b])
                    S.op("dve", lambda e: e.tensor_scalar(out=sm[:, 0:1], in0=c1[:, 0:1], scalar1=-1.0, scalar2=None, op0=ALU.mult), reads=[c1_b], writes=[sm_b])
                    S.op("act", lambda e: e.activation(out=e256[:], in_=cand[:], func=AF.Exp, bias=sm[:, 0:1], scale=1.0), reads=[cand_b, sm_b], writes=[e256_b])
                    S.op("dve", lambda e: e.scalar_tensor_tensor(out=ctmp[:], in0=cand[:], scalar=c2[:, 7:8], in1=e256[:], op0=ALU.is_ge, op1=ALU.mult),
                         reads=[cand_b, c2_b, e256_b], writes=[ctmp_b])
                    S.op("dve", lambda e: e.tensor_reduce(out=sm[:, 1:2], in_=ctmp[:], axis=AX.X, op=ALU.add), reads=[ctmp_b], writes=[sm_b])
                    S.op("act", lambda e: e.activation(out=sm[:, 2:3], in_=sm[:, 1:2], func=AF.Ln), reads=[sm_b], writes=[sm_b])
                    S.op("dve", lambda e, h=h: e.tensor_tensor(out=nb8[:, h:h + 1], in0=sm[:, 0:1], in1=sm[:, 2:3], op=ALU.subtract), reads=[sm_b], writes=[nb8_b])
                for ib in range(8):
                    for h in range(8):
                        Sd, Sd_b = Sd2[nn % 4]
                        Ed, Ed_b = Ed2[nn % 4]
                        Md, Md_b = Md2[nn % 4]
                        nn += 1
                        S.op("pool", lambda e, h=h, ib=ib, Sd=Sd: e.tensor_tensor(
                            out=Sd[:], in0=s_all[:, 2 * h, ib * 16:(ib + 1) * 16].unsqueeze(2).to_broadcast([128, 16, 128]),
                            in1=s_all[:, 2 * h + 1, :].unsqueeze(1).to_broadcast([128, 16, 128]), op=ALU.add),
                            reads=[s_all_b], writes=[Sd_b])
                        S.op("act", lambda e, Sd=Sd, Ed=Ed, h=h: e.activation(out=Ed[:], in_=Sd[:], func=AF.Exp, bias=nb8[:, h:h + 1], scale=1.0),
                             reads=[Sd_b, nb8_b], writes=[Ed_b])
                        S.op("dve", lambda e, Sd=Sd, Ed=Ed, Md=Md, h=h: e.scalar_tensor_tensor(
                            out=Md[:].rearrange("p a b -> p (a b)"), in0=Sd[:].rearrange("p a b -> p (a b)"), scalar=thr8[:, h:h + 1],
                            in1=Ed[:].rearrange("p a b -> p (a b)"), op0=ALU.is_ge, op1=ALU.mult), reads=[Sd_b, Ed_b, thr8_b], writes=[Md_b])
                        for q4 in range(4):
                            S.op("pe", lambda e, q4=q4, Md=Md, h=h: e.matmul(kb.ps[q4][0][:, :], lhsT=identg[:], rhs=Md[:].rearrange("p a b -> p (a b)")[:, q4 * 512:(q4 + 1) * 512],
                                                                            start=(h == 0), stop=(h == 7)), reads=[identg_b, Md_b], writes=[kb.ps[q4][1]])
                    for q4 in range(4):
                        S.op("act", lambda e, q4=q4, ib=ib, G=G: e.activation(out=G[:, ib * 2048 + q4 * 512:ib * 2048 + (q4 + 1) * 512], in_=kb.ps[q4][0][:, :], func=AF.Copy),
                             reads=[kb.ps[q4][1]], writes=[G_sb])
                S.dma("act", G_d[ts], G[:], [G_sb], G_b)
            kb.end(ph)
        with contextlib.ExitStack() as ph:
            identp, identp_b = kb.sb(ph, "identp", [128, 128], BF16, dma=True)
            ident_src = kb.inputs["ident"] if "ident" in kb.inputs else kb.din("ident", [128, 128])
            S.dma("pool", identp[:], ident_src[:, :], [], identp_b)
            h2t, h2t_b = kb.sb(ph, "h2t", [128, KC, 512], BF16, dma=True)
            uts = [kb.sb(ph, f"uts{i}", [128, KC, 512], BF16, dma=True) for i in range(2)]
            vts = [kb.sb(ph, f"vts{i}", [128, 4, D], BF16, dma=True) for i in range(2)]
            gss = [kb.sb(ph, f"gss{i}", [128, 4, 512], BF16, dma=True) for i in range(2)]
            gas = [kb.sb(ph, f"gas{i}", [128, 512], BF16) for i in range(2)]
            Wts = [kb.sb(ph, f"Wts{i}", [128, 512], BF16) for i in range(4)]
            WT, WT_b = kb.sb(ph, "WT", [128, 4, 512], BF16)
            acc, acc_b = kb.sb(ph, "pacc", [128, KC, 512], F32)
            x1t2 = [kb.sb(ph, f"px1{i}", [128, 512], F32, dma=True) for i in range(2)]
            x2t2 = [kb.sb(ph, f"px2{i}", [128, 512], F32) for i in range(2)]
            pbanks = [(kb.psbf[0][:], kb.psbf[1]), (kb.ps[6][0][:].bitcast(BF16), kb.ps[6][1])]
            uTv = uT_in.rearrange("(k p) e -> p k e", p=128)
            vv = v_in.rearrange("(c p) d -> p c d", p=128)
            h2v = h2T_d.rearrange("(k p) t -> p k t", p=128)
            nW = 0
            nT = 0
            for T in range(4):
                tsl = slice(T * 512, (T + 1) * 512)
                S.dma("sp", h2t[:], h2v[:, :, tsl], [h2T_b], h2t_b)
                for et in range(32):
                    ut, utb = uts[et % 2]
                    vt, vtb = vts[et % 2]
                    gs, gsb = gss[et % 2]
                    if T == 0:
                        S.dma("pool", ut[:], uTv[:, :, et * 512:(et + 1) * 512], [], utb)
                        S.dma("pool", vt[:].rearrange("p c (a b) -> p c a b", b=1024), vv[:, et * 4:(et + 1) * 4, :].rearrange("p c (a b) -> p c a b", b=1024), [], vtb)
                        S.dma("act", uTb_d[et], ut[:].rearrange("p k e -> p (k e)"), [utb], uTb_b)
                        S.dma("act", vb_d[et], vt[:].rearrange("p c d -> p (c d)"), [vtb], vb_b)
                    else:
                        S.dma("sp", ut[:].rearrange("p k e -> p (k e)"), uTb_d[et], [uTb_b], utb)
                        S.dma("act", vt[:].rearrange("p c d -> p (c d)"), vb_d[et], [vb_b], vtb)
                    S.dma("sp", gs[:], G_d[4 * T:4 * T + 4, :, et * 512:(et + 1) * 512].rearrange("a t e -> t a e"), [G_b], gsb)
                    wl = []
                    for a in range(4):
                        pst, psb = kb.psum()
                        for k in range(KC):
                            S.op("pe", lambda e, k=k, a=a, pst=pst, ut=ut: e.matmul(pst[:, :], lhsT=h2t[:, k, a * 128:(a + 1) * 128], rhs=ut[:, k, :],
                                                                                  start=(k == 0), stop=(k == KC - 1)), reads=[h2t_b, utb], writes=[psb])
                        ga, gab = gas[a % 2]
                        S.op("act", lambda e, pst=pst, ga=ga: e.activation(out=ga[:], in_=pst[:, :], func=AF.Gelu_apprx_tanh), reads=[psb], writes=[gab])
                        Wt, Wtb = Wts[nW % 4]
                        nW += 1
                        S.op("dve", lambda e, ga=ga, gs=gs, a=a, Wt=Wt: e.tensor_tensor(out=Wt[:], in0=ga[:], in1=gs[:, a, :], op=ALU.mult),
                             reads=[gab, gsb], writes=[Wtb])
                        wl.append((Wt, Wtb))
                    for c in range(4):
                        pbt, hb = pbanks[nT % 2]
                        nT += 1
                        for a in range(4):
                            S.op("pe", lambda e, a=a, c=c, pbt=pbt, wl=wl: e.transpose(out=pbt[:, a * 128:(a + 1) * 128], in_=wl[a][0][:, c * 128:(c + 1) * 128],
                                                                                      identity=identp[:]), reads=[wl[a][1], identp_b], writes=[hb])
                        S.op("act", lambda e, c=c, pbt=pbt: e.activation(out=WT[:, c, :], in_=pbt[:, 0:512], func=AF.Copy), reads=[hb], writes=[WT_b])
                    for dk in range(KC):
                        pst, psb = kb.psum()
                        for c in range(4):
                            S.op("pe", lambda e, c=c, dk=dk, pst=pst, vt=vt: e.matmul(pst[:, :], lhsT=vt[:, c, dk * 128:(dk + 1) * 128], rhs=WT[:, c, :],
                                                                                    start=(c == 0), stop=(c == 3)), reads=[vtb, WT_b], writes=[psb])
                        if et == 0:
                            S.op("dve", lambda e, dk=dk, pst=pst: e.tensor_copy(out=acc[:, dk, :], in_=pst[:, :]), reads=[psb], writes=[acc_b])
                        else:
                            S.op("dve", lambda e, dk=dk, pst=pst: e.tensor_tensor(out=acc[:, dk, :], in0=pst[:, :], in1=acc[:, dk, :], op=ALU.add),
                                 reads=[psb, acc_b], writes=[acc_b])
                for dk in range(KC):
                    x1t, x1tb = x1t2[dk % 2]
                    x2t, x2tb = x2t2[dk % 2]
                    S.dma("sp", x1t[:], x1T_d[dk * 128:(dk + 1) * 128, tsl], [x1T_b], x1tb)
                    S.op("dve", lambda e, dk=dk, x1t=x1t, x2t=x2t: e.scalar_tensor_tensor(
                        out=x2t[:], in0=acc[:, dk, :], scalar=mod[:, G2 + dk:G2 + dk + 1], in1=x1t[:], op0=ALU.mult, op1=ALU.add),
                        reads=[acc_b, mod_b, x1tb], writes=[x2tb])
                    S.dma("act", x2T_d[dk * 128:(dk + 1) * 128, tsl], x2t[:], [x2tb], x2T_b)
                if T == 0:
                    S.barrier()
                    S.retire([b_ for (_, b_) in uts + vts])
                    for (_, b_) in uts + vts:
                        b_.dma = True
            kb.end(ph)

    if stages >= 7:
        with contextlib.ExitStack() as ph:
            TT = 256
            xb2 = [kb.sb(ph, f"fx{i}", [128, KC, TT], F32, dma=True) for i in range(2)]
            sq, sqb = kb.sb(ph, "fsq", [128, KC, TT], BF16)
            rt_, rb_ = kb.sb(ph, "frstd", [128, TT], F32)
            ob2 = [kb.sb(ph, f"fo{i}", [128, KC, TT], F32) for i in range(2)]
            srcv = x2src_d.rearrange("(k p) t -> p k t", p=128)
            outv = outT.rearrange("(k p) t -> p k t", p=128)
            for ti in range(TOWN // TT):
                xt, xb = xb2[ti % 2]
                ot, ob = ob2[ti % 2]
                S.dma("sp", xt[:], srcv[:, :, ti * TT:(ti + 1) * TT], [x2src_b], xb)
                S.op("act", lambda e, xt=xt: e.activation(out=sq[:], in_=xt[:], func=AF.Square), reads=[xb], writes=[sqb])
                pst, psb = kb.psum()
                for k in range(KC):
                    S.op("pe", lambda e, k=k, pst=pst: e.matmul(pst[:, 0:TT], lhsT=ones_bf[:], rhs=sq[:, k, :], start=(k == 0), stop=(k == KC - 1)),
                         reads=[ones_bf_b, sqb], writes=[psb])
                S.op("act", lambda e, pst=pst: e.activation(out=rt_[:], in_=pst[:, 0:TT], func=AF.Sqrt, bias=eps_t[:, 0:1], scale=1.0 / D),
                     reads=[psb, eps_b], writes=[rb_])
                S.op("dve", lambda e: e.reciprocal(out=rt_[:], in_=rt_[:]), reads=[rb_], writes=[rb_])
                for k in range(KC):
                    S.op("dve", lambda e, k=k, xt=xt, ot=ot: e.scalar_tensor_tensor(out=ot[:, k, :], in0=xt[:, k, :], scalar=gfin[:, k:k + 1], in1=rt_[:],
                                                                                  op0=ALU.mult, op1=ALU.mult), reads=[xb, gfin_b, rb_], writes=[ob])
                S.dma("act", outv[:, :, ti * TT:(ti + 1) * TT], ot[:], [ob], outT_b)
            kb.end(ph)

    for name in debug:
        if name == "mod":
            continue
        src = {"qT_d": (qT_d, qT_b), "vs_d": (vs_d, vs_b), "uT_d": (uT_d, uT_b), "mgT_d": (mgT_d, mgT_b),
               "kcT_d": (kcT_d, kcT_b), "gT_d": (gT_d, gT_b), "oT_d": (oT_d, oT_b), "y2T_d": (y2T_d, y2T_b), "x1T_d": (x1T_d, x1T_b), "x2T_d": (x2T_d, x2T_b)}[name]
        o = nc.dram_tensor("dbg_" + name, list(src[0].shape), src[0].dtype, kind="ExternalOutput").ap()
        ob = S.buf("dbg_" + name, dma=True)
        S.dma("sp", o[:, :], src[0][:, :], [src[1]], ob)
    if "mod" in debug:
        o = nc.dram_tensor("dbg_mod", [128, 96], F32, kind="ExternalOutput").ap()
        ob = S.buf("dbg_mod", dma=True)
        S.dma("sp", o[:, :], mod[:], [mod_b], ob)

    S.barrier()
    es.close()
    return nc, kb


def pcol(v, chunks):
    return np.ascontiguousarray(np.asarray(v, np.float32).reshape(chunks, 128).T)


def t5_bucket_np(dist):
    dist = np.maximum(dist, 0)
    lr = np.log(np.maximum(dist, 1).astype(np.float32) / np.float32(16)) / np.float32(math.log(8.0))
    large = np.minimum(16 + (lr * np.float32(16)).astype(np.int32), 31)
    return np.where(dist < 16, dist, large)


NEG = np.float32(-30000.0)


def attn_tables(half, inp):
    LB, OFFB, LC, OFFC = 4096, 1024, 6400, 2064
    rb = np.asarray(inp["rel_bias"], np.float32)
    m = {}
    d = np.arange(LB) - OFFB
    g = rb[t5_bucket_np(d)].T
    Vb = np.where(d[None, :] >= 0, g, NEG).astype(np.float32)
    Vw = np.where((d[None, :] >= 0) & (d[None, :] < 512), g, NEG).astype(np.float32)
    m["Vb"], m["Vw"] = Vb, Vw
    m["Vwc"] = Vw if half == 1 else np.full_like(Vw, NEG)
    d = np.arange(LC) - OFFC
    g = rb[t5_bucket_np(d)].T
    Vc = np.where(d[None, :] >= 0, g, NEG).astype(np.float32)
    m["Vc"] = Vc
    m["Vc0"] = Vc if half == 1 else np.full_like(Vc, NEG)
    p = np.arange(128)[:, None, None]
    ts = np.arange(16)[None, :, None]
    j = np.arange(64)[None, None, :]
    t_abs = half * TOWN + ts * 128 + p
    cur = t_abs // 64
    ja = j - 32 * (1 - half)
    valid = (ja >= 0) & (ja <= cur)
    forced = (ja == 0) | (ja == cur) | (ja == cur - 1)
    m["selbias"] = np.where(valid, np.float32(1e4) * forced, np.float32(-1e30)).astype(np.float32).reshape(128, 1024)
    m["selinv"] = (~valid).astype(np.float32).reshape(128, 1024)
    pp = np.arange(128)[:, None]
    nn = 128 * np.arange(2)[None, :] + 127 - pp
    cs = nn[:, :, None] * 16
    ss = np.arange(64)[None, None, :] * 64
    ov = np.clip(np.minimum(cs + 32, ss + 64) - np.maximum(cs, ss), 0, None) / 32.0
    ov = np.where(nn[:, :, None] >= 255, 0.0, ov)
    maug = np.concatenate([ov, np.ones((128, 2, 1))], axis=2).astype(np.float32)
    m["maug"] = maug.reshape(128, 130)
    sl = 128 * np.arange(32)[None, :, None] + 127 - np.arange(128)[None, None, :]
    ne = np.where((sl // 64) == np.arange(64)[:, None, None], NEG, np.float32(0)).astype(np.float32).reshape(64, 4096)
    m["negE"] = np.concatenate([ne, ne], axis=0)
    k = np.arange(48)
    sg_ = np.repeat((k[:, None] == k[None, :]).astype(np.float32)[:, :, None], 64, axis=2).reshape(48, 3072)
    m["selg"] = np.concatenate([sg_, np.zeros((16, 3072), np.float32)], axis=0)
    m["ident"] = np.eye(128, dtype=np.float32)
    m["jmat"] = np.eye(128, dtype=np.float32)[::-1].copy()
    m["rel_bias"] = rb
    m["cmp_w1"] = np.ascontiguousarray(inp["cmp_w1"][0])
    m["cmp_posT"] = np.ascontiguousarray(np.transpose(inp["cmp_pos"][0], (0, 2, 1)))
    m["cmp_b1T"] = np.stack([pcol(inp["cmp_b1"][0, jj], 2) for jj in range(2)])
    m["cmp_w2"] = np.ascontiguousarray(inp["cmp_w2"][0])
    m["cmp_b2"] = np.ascontiguousarray(inp["cmp_b2"][0])
    return m


def s5_tables(inp):
    m = {}
    lre = np.asarray(inp["s5_lam_re"][0], np.float32)
    lim = np.asarray(inp["s5_lam_im"][0], np.float32)
    lst = np.asarray(inp["s5_log_step"][0], np.float32)
    def qp(a):
        return a.reshape(32, 2, 64).reshape(32, 128)
    lst2 = np.repeat(lst[:, None], 64, axis=1)
    m["lre_f"] = qp(lre).reshape(1, 4096).copy()
    m["lim_f"] = qp(lim).reshape(1, 4096).copy()
    m["lst_f"] = qp(lst2).reshape(1, 4096).copy()
    m["lreT"] = np.ascontiguousarray(qp(lre).T)
    m["limT"] = np.ascontiguousarray(qp(lim).T)
    m["lstT"] = np.ascontiguousarray(qp(lst2).T)
    bre = np.asarray(inp["s5_b_re"][0], np.float32)
    bim = np.asarray(inp["s5_b_im"][0], np.float32)
    cre = np.asarray(inp["s5_c_re"][0], np.float32)
    cim = np.asarray(inp["s5_c_im"][0], np.float32)
    Bre = np.zeros((128, 32, 128), np.float32); Bim = np.zeros_like(Bre)
    Cre = np.zeros((128, 32, 128), np.float32); Cim = np.zeros_like(Cre)
    for pr in range(32):
        for gg in range(2):
            g = 2 * pr + gg
            lg = g % 8
            Bre[lg * 16:(lg + 1) * 16, pr, gg * 64:(gg + 1) * 64] = bre[g].T
            Bim[lg * 16:(lg + 1) * 16, pr, gg * 64:(gg + 1) * 64] = bim[g].T
            Cre[gg * 64:(gg + 1) * 64, pr, lg * 16:(lg + 1) * 16] = cre[g].T
            Cim[gg * 64:(gg + 1) * 64, pr, lg * 16:(lg + 1) * 16] = cim[g].T
    m["Bpad_re"] = Bre.reshape(128, 4096); m["Bpad_im"] = Bim.reshape(128, 4096)
    m["Cpad_re"] = Cre.reshape(128, 4096); m["Cpad_im"] = Cim.reshape(128, 4096)
    m["tau"] = np.arange(512, dtype=np.float32).reshape(1, 512)
    m["s5dT"] = pcol(inp["s5_d"][0], 8)
    m["glu_w"] = np.ascontiguousarray(inp["glu_w"][0])
    m["glubT"] = pcol(inp["glu_b"][0], 8)
    return m


SHARED = {}


def prep_shared(inp):
    SHARED["peer_uT"] = np.ascontiguousarray(np.asarray(inp["peer_u"][0]).T)
    SHARED["peer_v"] = np.ascontiguousarray(inp["peer_v"][0])


def prep_inputs(core, inp):
    if "peer_uT" not in SHARED:
        prep_shared(inp)
    b, half = core // 2, core % 2
    x = inp["x"]
    own = x[b, half * TOWN:(half + 1) * TOWN]
    ctx = x[b, 0:TOWN] if half == 1 else np.zeros_like(own)
    xT = np.ascontiguousarray(np.concatenate([ctx, own], 0).T)
    m = {
        "xT": xT,
        "xTr": np.ascontiguousarray(xT.reshape(D, TALL // 128, 128)[:, :, ::-1].reshape(D, TALL)),
        "cT": pcol(inp["c"][b], KC),
        "ada_w": np.ascontiguousarray(inp["ada_w"][0]),
        "ada_bT": pcol(inp["ada_b"][0], 96),
        "gmixT": pcol(inp["norm_mix_g"][0], KC),
        "gffnT": pcol(inp["norm_ffn_g"][0], KC),
        "gfinT": pcol(inp["final_g"], KC),
        "w_in": np.ascontiguousarray(inp["w_in"][0]),
        "ctxflag": np.full((128, 1), float(half), np.float32),
    }
    m.update(attn_tables(half, inp))
    m.update(s5_tables(inp))
    m["w_up_attn"] = np.ascontiguousarray(inp["w_up_attn"][0])
    m["w_up_ssm"] = np.ascontiguousarray(inp["w_up_ssm"][0])
    m["w_out"] = np.ascontiguousarray(inp["w_out"][0])
    m["peer_w_q"] = np.ascontiguousarray(inp["peer_w_q"][0])
    sk = np.asarray(inp["peer_sub_keys"][0], np.float32)
    m["keysT"] = np.ascontiguousarray(np.transpose(sk.reshape(16, 128, 128), (2, 0, 1)).reshape(128, 2048))
    m["peer_uT"] = SHARED["peer_uT"]
    m["peer_v"] = SHARED["peer_v"]
    return m


def kernel(**inputs):
    inp = {k: np.asarray(v) for k, v in inputs.items()}
    SHARED.clear()
    nc, kb = build()
    in_maps = []
    for core in range(8):
        m = prep_inputs(core, inp)
        in_maps.append({k: m[k] for k in kb.inputs})
    res = run_bass_kernel_spmd(nc, in_maps, core_ids=list(range(8)))
    out = np.zeros((4, 4096, D), np.float32)
    for core in range(8):
        b, half = core // 2, core % 2
        out[b, half * TOWN:(half + 1) * TOWN] = res.results[core]["outT"].T
    return out
```
